# Optimizing a Trainium2 kernel written in Bass

```python
import math
import jax
import jax.numpy as jnp
from jax import lax
import numpy as np

D_MODEL = 2048
BATCH = 4
SEQ = 4096
DEPTH = 2

CHUNK = 64
QBLOCK = 128
EPS = 1e-6

POOL_WINDOWS = (2, 4, 8, 16)
N_POOL = 4
POOL_WIDTH = D_MODEL // 2
POOL_GROUP = POOL_WIDTH // N_POOL

DN_HEADS = 8
DN_HEAD_DIM = 128
DN_WIDTH = DN_HEADS * DN_HEAD_DIM
CONV_K = 4

FOX_HEADS = 8
FOX_HEAD_DIM = 128
FOX_WIDTH = FOX_HEADS * FOX_HEAD_DIM

N_BRANCH = 3

IN_SPLITS = (POOL_WIDTH, 3 * DN_WIDTH, DN_HEADS, DN_HEADS, DN_WIDTH, 3 * FOX_WIDTH, FOX_HEADS, N_BRANCH * D_MODEL)
IN_WIDTH = sum(IN_SPLITS)

PEER_HEADS = 8
PEER_KEY_DIM = 256
PEER_HALF = PEER_KEY_DIM // 2
N_SUBKEYS = 128
N_EXPERTS = N_SUBKEYS * N_SUBKEYS
PEER_TOPK = 16
PEER_TOKEN_BLOCK = 128

kernel_name = 'hybrid_pool_deltanet_fox_peer_adaln'


def rms_norm(x, w):
    xf = x.astype(jnp.float32)
    y = xf * lax.rsqrt(jnp.mean(xf * xf, axis=-1, keepdims=True) + EPS)
    return (y * w.astype(jnp.float32)).astype(x.dtype)


def l2_norm(x):
    return x * lax.rsqrt(jnp.sum(x * x, axis=-1, keepdims=True) + EPS)


def split_points():
    pts, acc = [], 0
    for w in IN_SPLITS[:-1]:
        acc += w
        pts.append(acc)
    return pts


def pool_mixer(p, pool_w, pool_scale):
    B, S, _ = p.shape
    pf = p.astype(jnp.float32).reshape(B, S, N_POOL, POOL_GROUP)
    cs = jnp.pad(jnp.cumsum(pf, axis=1), ((0, 0), (1, 0), (0, 0), (0, 0)))
    pos = jnp.arange(S)
    outs = []
    for g, w in enumerate(POOL_WINDOWS):
        csg = cs[:, :, g]
        lagged = jnp.pad(csg[:, :S + 1 - w], ((0, 0), (w - 1, 0), (0, 0)))
        cnt = jnp.minimum(pos + 1, w).astype(jnp.float32)[None, :, None]
        outs.append((csg[:, 1:] - lagged) / cnt - pf[:, :, g])
    pooled = jnp.stack(outs, axis=2).astype(p.dtype)
    y = jnp.einsum('bsgc,gcd->bsgd', pooled, pool_w)
    return y.reshape(B, S, POOL_WIDTH) * pool_scale


def causal_dwconv(x, w):
    return lax.conv_general_dilated(
        x, w[:, None, :].astype(x.dtype), window_strides=(1,), padding=((CONV_K - 1, 0),),
        dimension_numbers=('NWC', 'WIO', 'NWC'), feature_group_count=x.shape[-1])


def gated_delta_rule(q, k, v, g, beta):
    B, H, S, Dk = q.shape
    Dv = v.shape[-1]
    n = S // CHUNK
    q = q * Dk ** -0.5
    kb = k * beta[..., None]
    vb = v * beta[..., None]
    rs = lambda a: a.reshape((B, H, n, CHUNK) + a.shape[3:])
    q, k, kb, vb, g = rs(q), rs(k), rs(kb), rs(vb), rs(g)
    g = jnp.cumsum(g, axis=-1)
    idx = jnp.arange(CHUNK)
    incl = idx[:, None] >= idx[None, :]
    strict = idx[:, None] > idx[None, :]
    decay = jnp.exp(jnp.where(incl, g[..., :, None] - g[..., None, :], -jnp.inf))
    lower = jnp.where(strict, jnp.einsum('bhnid,bhnjd->bhnij', kb, k) * decay, 0.0)
    eye = jnp.eye(CHUNK, dtype=jnp.float32)
    t_mat = lax.linalg.triangular_solve(lower + eye, jnp.broadcast_to(eye, lower.shape),
                                        left_side=True, lower=True, unit_diagonal=True)
    u = jnp.einsum('bhnij,bhnjd->bhnid', t_mat, vb)
    wk = jnp.einsum('bhnij,bhnjd->bhnid', t_mat, kb * jnp.exp(g)[..., None])
    a_intra = jnp.where(incl, jnp.einsum('bhnid,bhnjd->bhnij', q, k) * decay, 0.0)
    q_dec = q * jnp.exp(g)[..., None]
    k_dec = k * jnp.exp(g[..., -1:] - g)[..., None]
    g_last = jnp.exp(g[..., -1])

    def step(state, inp):
        qd_i, kd_i, u_i, wk_i, a_i, gl_i = inp
        v_new = u_i - jnp.einsum('bhcd,bhde->bhce', wk_i, state)
        o = jnp.einsum('bhcd,bhde->bhce', qd_i, state) + jnp.einsum('bhij,bhje->bhie', a_i, v_new)
        state = state * gl_i[..., None, None] + jnp.einsum('bhcd,bhce->bhde', kd_i, v_new)
        return state, o

    mv = lambda a: jnp.moveaxis(a, 2, 0)
    state0 = jnp.zeros((B, H, Dk, Dv), jnp.float32)
    _, o = lax.scan(step, state0, (mv(q_dec), mv(k_dec), mv(u), mv(wk), mv(a_intra), mv(g_last)))
    return jnp.moveaxis(o, 0, 2).reshape(B, H, S, Dv)


def deltanet_branch(qkv, b_logit, a_logit, gate, conv_w, a_log, dt_bias, onorm_w):
    B, S, _ = qkv.shape
    qkv = jax.nn.silu(causal_dwconv(qkv, conv_w)).astype(jnp.float32)
    q, k, v = jnp.split(qkv, 3, axis=-1)
    heads = lambda a: a.reshape(B, S, DN_HEADS, DN_HEAD_DIM).transpose(0, 2, 1, 3)
    q, k, v = l2_norm(heads(q)), l2_norm(heads(k)), heads(v)
    beta = jax.nn.sigmoid(b_logit.astype(jnp.float32)).transpose(0, 2, 1)
    g = -(jnp.exp(a_log.astype(jnp.float32)) *
          jax.nn.softplus(a_logit.astype(jnp.float32) + dt_bias.astype(jnp.float32))).transpose(0, 2, 1)
    o = gated_delta_rule(q, k, v, g, beta).transpose(0, 2, 1, 3)
    o = o * lax.rsqrt(jnp.mean(o * o, axis=-1, keepdims=True) + EPS) * onorm_w.astype(jnp.float32)
    o = o * jax.nn.silu(gate.astype(jnp.float32).reshape(B, S, DN_HEADS, DN_HEAD_DIM))
    return o.reshape(B, S, DN_WIDTH).astype(qkv.dtype if False else gate.dtype)


def fox_branch(qkv, f_logit, f_bias):
    B, S, _ = qkv.shape
    q, k, v = jnp.split(qkv, 3, axis=-1)
    heads = lambda a: a.reshape(B, S, FOX_HEADS, FOX_HEAD_DIM).transpose(0, 2, 1, 3)
    q, k, v = heads(q), heads(k), heads(v)
    log_f = jax.nn.log_sigmoid(f_logit.astype(jnp.float32) + f_bias.astype(jnp.float32))
    cum_f = jnp.cumsum(log_f.transpose(0, 2, 1), axis=-1)
    scale = FOX_HEAD_DIM ** -0.5
    outs = []
    for i in range(S // QBLOCK):
        lo, hi = i * QBLOCK, (i + 1) * QBLOCK
        s = jnp.einsum('bhqd,bhkd->bhqk', q[:, :, lo:hi], k[:, :, :hi]).astype(jnp.float32) * scale
        s = s + cum_f[:, :, lo:hi, None] - cum_f[:, :, None, :hi]
        mask = (lo + jnp.arange(QBLOCK))[:, None] >= jnp.arange(hi)[None, :]
        p = jax.nn.softmax(jnp.where(mask, s, -jnp.inf), axis=-1)
        outs.append(jnp.einsum('bhqk,bhkd->bhqd', p.astype(v.dtype), v[:, :, :hi]))
    o = jnp.concatenate(outs, axis=2)
    return o.transpose(0, 2, 1, 3).reshape(B, S, FOX_WIDTH)


def hybrid_mixer(h, w_in, pool_w, pool_scale, dn_conv_w, dn_a_log, dn_dt_bias, dn_onorm_w,
                 fox_f_bias, w_branch_pool, w_branch_dn, w_branch_fox, w_out):
    B, S, D = h.shape
    proj = h @ w_in
    pool_in, dn_qkv, dn_b, dn_a, dn_g, fox_qkv, fox_f, gate_logits = jnp.split(proj, split_points(), axis=-1)
    y_pool = pool_mixer(pool_in, pool_w, pool_scale)
    y_dn = deltanet_branch(dn_qkv, dn_b, dn_a, dn_g, dn_conv_w, dn_a_log, dn_dt_bias, dn_onorm_w)
    y_fox = fox_branch(fox_qkv, fox_f, fox_f_bias)
    gates = jax.nn.sigmoid(gate_logits.astype(jnp.float32)).astype(h.dtype).reshape(B, S, N_BRANCH, D)
    merged = (gates[:, :, 0] * (y_pool @ w_branch_pool)
              + gates[:, :, 1] * (y_dn @ w_branch_dn)
              + gates[:, :, 2] * (y_fox @ w_branch_fox))
    return merged @ w_out


def peer_ffn(h, w_q, sub_keys, expert_u, expert_v):
    B, S, D = h.shape
    T = B * S
    hf = h.reshape(T, D)
    q = (hf @ w_q).reshape(T, PEER_HEADS, 2, PEER_HALF)
    scores = jnp.einsum('thpd,hpnd->thpn', q, sub_keys).astype(jnp.float32)
    top_s, top_i = lax.top_k(scores, PEER_TOPK)
    cand_s = top_s[:, :, 0, :, None] + top_s[:, :, 1, None, :]
    cand_id = top_i[:, :, 0, :, None] * N_SUBKEYS + top_i[:, :, 1, None, :]
    kk = PEER_TOPK * PEER_TOPK
    best_s, best_pos = lax.top_k(cand_s.reshape(T, PEER_HEADS, kk), PEER_TOPK)
    ids = jnp.take_along_axis(cand_id.reshape(T, PEER_HEADS, kk), best_pos, axis=-1)
    wts = jax.nn.softmax(best_s, axis=-1)
    n_slots = PEER_HEADS * PEER_TOPK
    nb = T // PEER_TOKEN_BLOCK

    def block(args):
        xb, idb, wb = args
        act = jax.nn.gelu(jnp.einsum('td,ted->te', xb, expert_u[idb]), approximate=False)
        coef = (act.astype(jnp.float32) * wb).astype(xb.dtype)
        return jnp.einsum('te,ted->td', coef, expert_v[idb])

    y = lax.map(block, (hf.reshape(nb, PEER_TOKEN_BLOCK, D),
                        ids.reshape(nb, PEER_TOKEN_BLOCK, n_slots),
                        wts.reshape(nb, PEER_TOKEN_BLOCK, n_slots)))
    return y.reshape(B, S, D)


def setup_inputs(seed: int = 0) -> dict:
    key = jax.random.key(seed)
    ks = jax.random.split(key, 24)
    f32 = jnp.float32
    nrm = lambda k, shape, s: jax.random.normal(k, shape, f32) * s
    L = DEPTH
    x = nrm(ks[0], (BATCH, SEQ, D_MODEL), 1.0)
    c = nrm(ks[1], (BATCH, D_MODEL), 1.0)
    ada_w = nrm(ks[2], (L, D_MODEL, 6 * D_MODEL), 0.5 * D_MODEL ** -0.5)
    ada_b = nrm(ks[3], (L, 6 * D_MODEL), 0.02)
    norm_mix_w = 1.0 + nrm(ks[4], (L, D_MODEL), 0.05)
    w_in = nrm(ks[5], (L, D_MODEL, IN_WIDTH), D_MODEL ** -0.5)
    pool_w = nrm(ks[6], (L, N_POOL, POOL_GROUP, POOL_GROUP), POOL_GROUP ** -0.5)
    pool_scale = jax.random.uniform(ks[7], (L, POOL_WIDTH), f32, 0.5, 1.5)
    dn_conv_w = nrm(ks[8], (L, CONV_K, 3 * DN_WIDTH), CONV_K ** -0.5)
    dn_a_log = jnp.log(jax.random.uniform(ks[9], (L, DN_HEADS), f32, 1.0, 16.0))
    dt = jnp.exp(jax.random.uniform(ks[10], (L, DN_HEADS), f32, math.log(1e-3), math.log(1e-1)))
    dn_dt_bias = dt + jnp.log(-jnp.expm1(-dt))
    dn_onorm_w = 1.0 + nrm(ks[11], (L, DN_HEAD_DIM), 0.05)
    fox_f_bias = jax.random.uniform(ks[12], (L, FOX_HEADS), f32, 1.0, 5.0)
    w_branch_pool = nrm(ks[13], (L, POOL_WIDTH, D_MODEL), POOL_WIDTH ** -0.5)
    w_branch_dn = nrm(ks[14], (L, DN_WIDTH, D_MODEL), DN_WIDTH ** -0.5)
    w_branch_fox = nrm(ks[15], (L, FOX_WIDTH, D_MODEL), FOX_WIDTH ** -0.5)
    w_out = nrm(ks[16], (L, D_MODEL, D_MODEL), D_MODEL ** -0.5)
    norm_ffn_w = 1.0 + nrm(ks[17], (L, D_MODEL), 0.05)
    peer_w_q = nrm(ks[18], (L, D_MODEL, PEER_HEADS * PEER_KEY_DIM), D_MODEL ** -0.5)
    peer_sub_keys = nrm(ks[19], (L, PEER_HEADS, 2, N_SUBKEYS, PEER_HALF), PEER_HALF ** -0.5)
    peer_u = nrm(ks[20], (L, N_EXPERTS, D_MODEL), D_MODEL ** -0.5)
    peer_v = nrm(ks[21], (L, N_EXPERTS, D_MODEL), PEER_HEADS ** -0.5)
    final_norm_w = 1.0 + nrm(ks[22], (D_MODEL,), 0.05)
    return {'x': x, 'c': c, 'ada_w': ada_w, 'ada_b': ada_b, 'norm_mix_w': norm_mix_w, 'w_in': w_in,
            'pool_w': pool_w, 'pool_scale': pool_scale, 'dn_conv_w': dn_conv_w, 'dn_a_log': dn_a_log,
            'dn_dt_bias': dn_dt_bias, 'dn_onorm_w': dn_onorm_w, 'fox_f_bias': fox_f_bias,
            'w_branch_pool': w_branch_pool, 'w_branch_dn': w_branch_dn, 'w_branch_fox': w_branch_fox,
            'w_out': w_out, 'norm_ffn_w': norm_ffn_w, 'peer_w_q': peer_w_q, 'peer_sub_keys': peer_sub_keys,
            'peer_u': peer_u, 'peer_v': peer_v, 'final_norm_w': final_norm_w}


def reference(x, c, ada_w, ada_b, norm_mix_w, w_in, pool_w, pool_scale, dn_conv_w, dn_a_log,
              dn_dt_bias, dn_onorm_w, fox_f_bias, w_branch_pool, w_branch_dn, w_branch_fox,
              w_out, norm_ffn_w, peer_w_q, peer_sub_keys, peer_u, peer_v, final_norm_w):
    for l in range(DEPTH):
        mod = (jax.nn.silu(c) @ ada_w[l] + ada_b[l])[:, None, :]
        sh_m, sc_m, g_m, sh_f, sc_f, g_f = jnp.split(mod, 6, axis=-1)
        h = rms_norm(x, norm_mix_w[l]) * (1 + sc_m) + sh_m
        x = x + g_m * hybrid_mixer(h, w_in[l], pool_w[l], pool_scale[l], dn_conv_w[l], dn_a_log[l],
                                   dn_dt_bias[l], dn_onorm_w[l], fox_f_bias[l], w_branch_pool[l],
                                   w_branch_dn[l], w_branch_fox[l], w_out[l])
        h = rms_norm(x, norm_ffn_w[l]) * (1 + sc_f) + sh_f
        x = x + g_f * peer_ffn(h, peer_w_q[l], peer_sub_keys[l], peer_u[l], peer_v[l])
    return rms_norm(x, final_norm_w)
```

```python
import numpy as np
from contextlib import ExitStack
import concourse.bass as bass
import concourse.mybir as mybir
from concourse.bass_utils import run_bass_kernel_spmd

F32 = mybir.dt.float32
BF16 = mybir.dt.bfloat16
I32 = mybir.dt.int32
U32 = mybir.dt.uint32
ALU = mybir.AluOpType
AF = mybir.ActivationFunctionType
AX = mybir.AxisListType

ENGS = ("pe", "act", "dve", "pool", "sp")


class Prog:
    def __init__(self, nc, dma_sems=None):
        self.nc = nc
        self.streams = {e: [] for e in ENGS}
        self.count = {}
        self.waited = {e: {} for e in ENGS}
        self.res_w = {}
        self.res_r = {}
        self.dma_sems = dma_sems or {"sp": 8, "act": 6, "pool": 12}
        self.dma_rr = {q: 0 for q in self.dma_sems}
        self.es = ExitStack()
        self.n_ops = 0
        self.sb_bytes = 0
        self.nbank = 0
        self.limit = None

    def sb(self, name, shape, dt):
        n = 1
        for s in shape[1:]:
            n *= s
        self.sb_bytes += n * (2 if dt == BF16 else 4)
        return self.es.enter_context(self.nc.sbuf_tensor(name, list(shape), dt))

    def ps(self, name, shape, dt):
        return self.es.enter_context(self.nc.psum_tensor(name, list(shape), dt))

    def _deps(self, reads, writes):
        deps = {}

        def add(k, v):
            if deps.get(k, 0) < v:
                deps[k] = v
        for r in reads:
            if r in self.res_w:
                add(*self.res_w[r])
        for w in writes:
            if w in self.res_w:
                add(*self.res_w[w])
            for k, v in self.res_r.get(w, {}).items():
                add(k, v)
        return deps

    def _emit_waits(self, eng, deps):
        for k, v in deps.items():
            if k == eng and eng == "pe":
                continue
            if self.waited[eng].get(k, 0) >= v:
                continue
            self.streams[eng].append(("wait", k, v))
            self.waited[eng][k] = v

    def _mark(self, key, val, reads, writes):
        for r in reads:
            d = self.res_r.setdefault(r, {})
            if d.get(key, 0) < val:
                d[key] = val
        for w in writes:
            self.res_w[w] = (key, val)
            self.res_r[w] = {}

    def op(self, eng, fn, reads=(), writes=()):
        if self.limit is not None and self.n_ops >= self.limit:
            return
        self._emit_waits(eng, self._deps(reads, writes))
        v = self.count.get(eng, 0) + 1
        self.count[eng] = v
        self.streams[eng].append(("op", fn, eng, 1))
        self._mark(eng, v, reads, writes)
        self.n_ops += 1

    def pe(self, fn, r=(), w=()):
        self.op("pe", fn, r, w)

    def act(self, fn, r=(), w=()):
        self.op("act", fn, r, w)

    def dve(self, fn, r=(), w=()):
        self.op("dve", fn, r, w)

    def pool(self, fn, r=(), w=()):
        self.op("pool", fn, r, w)

    def dma(self, q, fn, reads=(), writes=()):
        if self.limit is not None and self.n_ops >= self.limit:
            return
        deps = self._deps(reads, writes)
        k = self.dma_rr[q] % self.dma_sems[q]
        self.dma_rr[q] += 1
        key = ("dma", q, k)
        prev = self.count.get(key, 0)
        if prev:
            deps[key] = max(deps.get(key, 0), prev)
        self._emit_waits(q, deps)
        v = prev + 16
        self.count[key] = v
        self.streams[q].append(("op", fn, key, 16))
        self._mark(key, v, reads, writes)
        self.n_ops += 1

    def barrier(self):
        for e in ENGS:
            for k, v in self.count.items():
                if k == e and e == "pe":
                    continue
                if self.waited[e].get(k, 0) < v:
                    self.streams[e].append(("wait", k, v))
                    self.waited[e][k] = v

    def finish(self):
        for k, v in self.count.items():
            if self.waited["sp"].get(k, 0) < v:
                self.streams["sp"].append(("wait", k, v))
                self.waited["sp"][k] = v

    def emit(self):
        nc = self.nc
        sems = {}
        for k in self.count:
            nm = k if isinstance(k, str) else "d_%s_%d" % (k[1], k[2])
            sems[k] = self.es.enter_context(nc.semaphore("s_" + nm))
        block = self.es.enter_context(nc.Block())

        def run(ename):
            def f(e):
                for it in self.streams[ename]:
                    if it[0] == "wait":
                        e.wait_ge(sems[it[1]], it[2])
                    else:
                        it[1](e).then_inc(sems[it[2]], it[3])
            return f
        block.tensor(run("pe"))
        block.scalar(run("act"))
        block.vector(run("dve"))
        block.gpsimd(run("pool"))
        block.sync(run("sp"))
        self.es.close()


class Arena:
    def __init__(self, t, nwords):
        self.t, self.n, self.off, self.gen = t, nwords, 0, 0

    def reset(self):
        self.off = 0
        self.gen += 1

    def alloc(self, name, free_shape, dt):
        n = 1
        for d_ in free_shape:
            n *= d_
        words = n if dt in (F32, I32, U32) else (n + 1) // 2
        words = (words + 3) // 4 * 4
        assert self.off + words <= self.n, ("arena overflow", name, self.off, words, self.n)
        ap = self.t[:, self.off:self.off + words]
        self.off += words
        if dt != F32:
            ap = ap.bitcast(dt)
        ap = ap[:, 0:n]
        if len(free_shape) > 1:
            names = ["a%d" % i for i in range(len(free_shape))]
            pat = "p (%s) -> p %s" % (" ".join(names), " ".join(names))
            ap = ap.rearrange(pat, **{nm: d_ for nm, d_ in zip(names[1:], free_shape[1:])})
        return ap


S = 4096
D = 2048
TG = 512
NG = S // TG
KC = 16
NL = 2
EPS = 1e-6
NW = 14360
NEXP = 16384
OFF_P, OFF_D, OFF_GD, OFF_F, OFF_SBA, OFF_V, OFF_G, OFF_SF = 0, 1024, 4096, 5120, 7168, 7184, 8208, 14352
HALF_A = 7184

C_NAMES = ["ident", "ones", "tri128", "triblk", "strictU", "blkones", "selA", "selB", "selrow0"]
C_OFF = {n: i * 128 for i, n in enumerate(C_NAMES)}
C_MASKA = len(C_NAMES) * 128
C_MASKB = C_MASKA + 1
C_IOTA16 = C_MASKB + 1
C_INVCNT = C_IOTA16 + 16
C_TOTAL = C_INVCNT + 64


def make_consts():
    c = np.zeros((128, C_TOTAL), np.float32)
    m = np.arange(128)[:, None]
    i = np.arange(128)[None, :]
    same = (m // 64) == (i // 64)
    mats = {
        "ident": (m == i), "ones": np.ones((128, 128), bool), "tri128": (m <= i),
        "triblk": (m <= i) & same, "strictU": (m > i) & same, "blkones": same,
        "selA": (m < 64) & (i >= 0), "selB": (m >= 64) & (i >= 0), "selrow0": (m == 0) & (i >= 0),
    }
    for n in C_NAMES:
        c[:, C_OFF[n]:C_OFF[n] + 128] = mats[n].astype(np.float32)
    c[:, C_MASKA] = (np.arange(128) < 64)
    c[:, C_MASKB] = (np.arange(128) >= 64)
    c[:, C_IOTA16:C_IOTA16 + 16] = np.arange(16)[None, :]
    for gi, w in enumerate((2, 4, 8, 16)):
        c[:, C_INVCNT + gi * 16:C_INVCNT + gi * 16 + 16] = 1.0 / np.minimum(np.arange(16) + 1, w)[None, :]
    return c


def build(cfg):
    nc = bass.Bass("TRN2", target_bir_lowering=False)
    P = Prog(nc)
    P.limit = cfg.get("max_ops")
    dbg = cfg.get("debug", {})
    phases = cfg.get("phases", "all")
    layers = cfg.get("layers", [0, 1])

    shrink = cfg.get("shrink", ())

    def din(name, shape, dt=F32):
        if name in shrink:
            return None
        return nc.dram_tensor(name, list(shape), dt, kind="ExternalInput").ap()

    feed = cfg.get("feed", ())

    def dscr(name, shape, dt):
        kind = "ExternalInput" if name in feed else "Internal"
        return nc.dram_tensor(name, list(shape), dt, kind=kind).ap()

    def dout(name, shape, dt=F32):
        return nc.dram_tensor(name, list(shape), dt, kind="ExternalOutput").ap()

    x_in = din("x", [S, D])
    cT_in = din("cT", [128, KC])
    ada_w = din("ada_w", [NL, D, 6 * D])
    ada_b = din("ada_b", [NL, 6 * D])
    norm_mix_w = din("norm_mix_w", [NL, D])
    norm_ffn_w = din("norm_ffn_w", [NL, D])
    final_norm_w = din("final_norm_w", [1, D])
    w_in = din("w_in", [NL, D, NW])
    pool_w = din("pool_w", [NL, 4, 256, 256])
    pool_scale_t = din("pool_scale_t", [NL, 128, 8])
    conv_t = din("conv_t", [NL, 128, 24 * 4])
    a_log_bc = din("a_log_bc", [NL, 128, 8])
    dt_bias_bc = din("dt_bias_bc", [NL, 128, 8])
    f_bias_bc = din("f_bias_bc", [NL, 128, 8])
    onorm_bc = din("onorm_bc", [NL, 128, 128])
    w_br = [din("w_branch_pool", [NL, 1024, D]), din("w_branch_dn", [NL, 1024, D]), din("w_branch_fox", [NL, 1024, D])]
    if cT_in is None:
        cT_in = din("cT_dummy", [128, KC])
    w_out = din("w_out", [NL, D, D])
    peer_w_q = din("peer_w_q", [NL, D, D])
    skT = din("skT", [NL, 16, 128, 128])
    peer_u = din("peer_u", [NL, NEXP, D])
    peer_v = din("peer_v", [NL, NEXP, D])
    consts_in = din("consts", [128, C_TOTAL])
    y_out = dout("y", [S, D])

    winb = dscr("winb", [D, NW], BF16)
    wbrb = dscr("wbrb", [3, 1024, D], BF16)
    woutb = dscr("woutb", [D, D], BF16)
    xres = dscr("xres", [S, D], F32)
    for i_ in range(S // 128):
        if "xres" in feed:
            P.res_w.pop("xres%d" % i_, None)
    Hs = dscr("Hs", [KC, 128, S], BF16)
    Yp = dscr("Yp", [8, 128, S], BF16)
    Yd = dscr("Yd", [8, 128, S], BF16)
    Yf = dscr("Yf", [8, 128, S], BF16)
    DNq = dscr("DNq", [8, 128, S], F32)
    DNk = dscr("DNk", [8, 128, S], F32)
    DNv = dscr("DNv", [8, 128, S], F32)
    Gdn = dscr("Gdn", [S, 1024], BF16)
    Qc = dscr("Qc", [8, 128, S], BF16)
    Kc = dscr("Kc", [8, 128, S], BF16)
    Vc = dscr("Vc", [128, S // 128, 1024], BF16)

    NBANK = cfg.get("nbank", 8)
    banks = [P.ps("bank%d" % i, [128, 512], F32) for i in range(8)]

    P.bank_lo, P.bank_n = 0, NBANK

    def bank():
        i = P.bank_lo + P.nbank % P.bank_n
        P.nbank += 1
        return banks[i], "bank%d" % i

    cst = P.sb("cst", [128, C_TOTAL], F32)
    P.dma("sp", lambda e: e.dma_start(out=cst[:], in_=consts_in[:, :]), [], ["cst"])

    def cm(name):
        return cst[:, C_OFF[name]:C_OFF[name] + 128]
    ident_bf = P.sb("ident_bf", [128, 128], BF16)
    ones_bf = P.sb("ones_bf", [128, 128], BF16)
    P.dve(lambda e: e.tensor_copy(ident_bf[:], cm("ident")), ["cst"], ["ident_bf"])
    P.dve(lambda e: e.tensor_copy(ones_bf[:], cm("ones")), ["cst"], ["ones_bf"])

    ct = P.sb("ct", [128, KC], F32)
    csbc = P.sb("csbc", [128, KC, 128], F32)
    P.dma("sp", lambda e: e.dma_start(out=ct[:], in_=cT_in[:, :]), [], ["ct"])
    P.act(lambda e: e.activation(out=ct[:], in_=ct[:], func=AF.Silu), ["ct"], ["ct"])
    P.dve(lambda e: e.tensor_copy(csbc[:], ct[:].unsqueeze(2).to_broadcast([128, KC, 128])), ["ct"], ["csbc"])

    stg = P.sb("stg", [128, 4096], F32)
    modt = [P.sb("mod%d" % i, [128, D], F32) for i in range(3)]
    stat = P.sb("stat", [128, 8], F32)
    sm = P.sb("sm", [128, 128], F32)
    sg = P.sb("sg", [128, S // 128, 24], F32)
    if "sg_in" in feed:
        sg_in = din("sg_in", [128, S // 128, 24])
        P.dma("sp", lambda e: e.dma_start(out=sg[:], in_=sg_in[:, :, :]), [], ["sg"])
    ARW = cfg.get("arw", 36864)
    arena_t = P.sb("arena", [128, ARW], F32)
    AR = Arena(arena_t, ARW)

    def new_phase():
        P.barrier()
        AR.reset()

    def mod_vector(l, j, dst, dst_key, plus_one=False, mul_row=None):
        stv = stg[:, 0:KC * 256].rearrange("p (k c) -> p k c", k=KC)
        for cb in range(8):
            c0 = j * D + cb * 256
            P.dma("sp", lambda e, c0=c0: e.dma_start(
                out=stv, in_=ada_w[l, :, c0:c0 + 256].rearrange("(k p) c -> p k c", p=128)), [], ["stg"])
            bk, bkk = bank()
            for k in range(KC):
                P.pe(lambda e, bk=bk, k=k: e.matmul(bk[:, 0:256], lhsT=csbc[:, k, :], rhs=stv[:, k, :],
                                                    start=(k == 0), stop=(k == KC - 1)), ["csbc", "stg"], [bkk])
            P.act(lambda e, bk=bk, cb=cb: e.copy(out=dst[:, cb * 256:(cb + 1) * 256], in_=bk[:, 0:256]), [bkk], [dst_key])
        P.dma("sp", lambda e: e.dma_start(out=stg[:, 0:D], in_=ada_b[l:l + 1, j * D:(j + 1) * D].to_broadcast([128, D])),
              [], ["stg"])
        P.dve(lambda e: e.tensor_add(dst[:], dst[:], stg[:, 0:D]), ["stg", dst_key], [dst_key])
        if plus_one:
            P.dma("sp", lambda e: e.dma_start(out=stg[:, D:2 * D], in_=mul_row.to_broadcast([128, D])), [], ["stg"])
            P.dve(lambda e: e.scalar_tensor_tensor(out=dst[:], in0=dst[:], scalar=1.0, in1=stg[:, D:2 * D],
                                                   op0=ALU.add, op1=ALU.mult), ["stg", dst_key], [dst_key])

    def cast_rows(src2d, dst2d, ncols, segs, key_dst, pst, pstb):
        rows = src2d.shape[0]
        for r0 in range(0, rows, 128):
            P.dma("sp", lambda e, r0=r0: e.dma_start(out=pst[:, 0:ncols], in_=src2d[r0:r0 + 128, :]), [], ["pst"])
            for si, (ov, iv) in enumerate(segs):
                eng = ("act", "dve", "pool")[si % 3]
                if eng == "act":
                    P.act(lambda e, ov=ov, iv=iv: e.copy(out=ov(pstb), in_=iv(pst)), ["pst"], ["pstb"])
                elif eng == "dve":
                    P.dve(lambda e, ov=ov, iv=iv: e.tensor_copy(ov(pstb), iv(pst)), ["pst"], ["pstb"])
                else:
                    P.pool(lambda e, ov=ov, iv=iv: e.tensor_copy(ov(pstb), iv(pst)), ["pst"], ["pstb"])
            P.dma("sp", lambda e, r0=r0: e.dma_start(out=dst2d[r0:r0 + 128, :], in_=pstb[:, 0:ncols]), ["pstb"], [key_dst])

    def prepass(l):
        new_phase()
        pst = AR.alloc("pst", [HALF_A], F32)
        pstb = AR.alloc("pstb", [HALF_A], BF16)
        segsA = [
            (lambda o: o[:, 0:1024], lambda i: i[:, 0:1024]),
            (lambda o: o[:, OFF_D:OFF_D + 3072].rearrange("p (h t c) -> p h t c", h=8, t=3),
             lambda i: i[:, 1024:4096].rearrange("p (t h c) -> p h t c", t=3, h=8)),
            (lambda o: o[:, OFF_GD:OFF_GD + 1024], lambda i: i[:, 4112:5136]),
            (lambda o: o[:, OFF_F:OFF_F + 2048].rearrange("p (h t c) -> p h t c", h=8, t=2),
             lambda i: i[:, 5136:7184].rearrange("p (t h c) -> p h t c", t=2, h=8)),
            (lambda o: o[:, OFF_SBA:OFF_SBA + 16], lambda i: i[:, 4096:4112]),
        ]
        cast_rows(w_in[l, :, 0:HALF_A], winb[:, 0:HALF_A], HALF_A, segsA, "winb", pst, pstb)
        nB = NW - HALF_A
        segsB = [
            (lambda o: o[:, 0:1024], lambda i: i[:, 0:1024]),
            (lambda o: o[:, 1024:1024 + 6144].rearrange("p (j b c) -> p j b c", j=16, b=3),
             lambda i: i[:, 1032:1032 + 6144].rearrange("p (b j c) -> p j b c", b=3, j=16)),
            (lambda o: o[:, 7168:7176], lambda i: i[:, 1024:1032]),
        ]
        cast_rows(w_in[l, :, HALF_A:NW], winb[:, HALF_A:NW], nB, segsB, "winb", pst, pstb)
        plain = [(lambda o: o[:, 0:1024], lambda i: i[:, 0:1024]), (lambda o: o[:, 1024:2048], lambda i: i[:, 1024:2048])]
        for b in range(3):
            cast_rows(w_br[b][l], wbrb[b], 2048, plain, "wbrb", pst, pstb)
        cast_rows(w_out[l], woutb, 2048, plain, "woutb", pst, pstb)

    def load_wt(wts, src2d, c0, n, srckey, pref="wt"):
        i = P.wt_rr % len(wts)
        P.wt_rr += 1
        t, k = wts[i], "%s%d" % (pref, i)
        P.dma("sp", lambda e: e.dma_start(out=t[:, :, 0:n], in_=src2d[:, c0:c0 + n].rearrange("(k p) c -> p k c", p=128)),
              [srckey], [k])
        return t, k
    P.wt_rr = 0

    def rmsnorm_tile(xt, xtk, gain, gaink, shift, shiftk, junk):
        P.act(lambda e: e.activation(out=junk[:], in_=xt[:], func=AF.Square, accum_out=stat[:, 0:1]), [xtk], ["hb", "pjunk", "stat"])
        P.act(lambda e: e.activation(out=stat[:, 1:2], in_=stat[:, 0:1], func=AF.Sqrt, scale=1.0 / D, bias=EPS), ["stat"], ["stat"])
        P.dve(lambda e: e.reciprocal(out=stat[:, 2:3], in_=stat[:, 1:2]), ["stat"], ["stat"])
        P.dve(lambda e: e.scalar_tensor_tensor(out=xt[:], in0=xt[:], scalar=stat[:, 2:3], in1=gain[:],
                                               op0=ALU.mult, op1=ALU.mult), [xtk, "stat", gaink], [xtk])
        if shift is not None:
            P.dve(lambda e: e.tensor_add(xt[:], xt[:], shift[:]), [xtk, shiftk], [xtk])

    HT = {}

    def proj_fm(wt, wtk, c0, n=TG):
        hT = HT["hT"]
        bk, bkk = bank()
        for k in range(KC):
            P.pe(lambda e, k=k: e.matmul(bk[:, 0:n], lhsT=wt[:, k, c0:c0 + 128], rhs=hT[:, k, 0:n],
                                         start=(k == 0), stop=(k == KC - 1)), [wtk, "hT"], [bkk])
        return bk, bkk

    def proj_tm(wt, wtk, t, c0, n, bk=None, bkk=None, o0=0):
        hT = HT["hT"]
        if bk is None:
            bk, bkk = bank()
        for k in range(KC):
            P.pe(lambda e, k=k: e.matmul(bk[:, o0:o0 + n], lhsT=hT[:, k, t * 128:(t + 1) * 128], rhs=wt[:, k, c0:c0 + n],
                                         start=(k == 0), stop=(k == KC - 1)), [wtk, "hT"], [bkk])
        return bk, bkk

    def phase1(l, xsrc, xsrc_key):
        new_phase()
        hT = AR.alloc("hT", [KC, TG], BF16)
        HT["hT"] = hT
        wts = [AR.alloc("wt%d" % i, [KC, 512], BF16) for i in range(2)]
        xts = [AR.alloc("xt%d" % i, [D], F32) for i in range(2)]
        hb = AR.alloc("hb", [D], BF16)
        pin = AR.alloc("pin", [8, 15 + TG], F32)
        pacc = [AR.alloc("pacc%d" % i, [15 + TG], F32) for i in range(2)]
        pooled = AR.alloc("pooled", [8, TG], BF16)
        ypT = AR.alloc("ypT", [8, TG], BF16)
        pwb = AR.alloc("pwb", [8, 256], BF16)
        psc = AR.alloc("psc", [8], F32)
        cvw = AR.alloc("cvw", [24 * 4], F32)
        cin = AR.alloc("cin", [3, 3 + TG], F32)
        ctail = AR.alloc("ctail", [24, 3], F32)
        cacc = AR.alloc("cacc", [3, TG], F32)
        csq = AR.alloc("csq", [TG], F32)
        crn = AR.alloc("crn", [TG], F32)
        tm_bf = AR.alloc("tm_bf", [4, 1024], BF16)
        fqk = AR.alloc("fqk", [2, TG], BF16)
        junk = sm
        P.dma("sp", lambda e: e.dma_start(out=stg[:, 0:2048].rearrange("p (a c) -> p a c", a=8),
                                          in_=pool_w[l].rearrange("g (cc p) d -> p (g cc) d", p=128)), [], ["stg"])
        P.dve(lambda e: e.tensor_copy(pwb[:], stg[:, 0:2048].rearrange("p (a c) -> p a c", a=8)), ["stg"], ["pwb"])
        P.dma("sp", lambda e: e.dma_start(out=psc[:], in_=pool_scale_t[l]), [], ["psc"])
        P.dma("sp", lambda e: e.dma_start(out=cvw[:], in_=conv_t[l]), [], ["cvw"])
        P.pool(lambda e: e.memset(pin[:], 0.0), [], ["pin"])
        P.pool(lambda e: e.memset(ctail[:], 0.0), [], ["ctail"])
        mod_vector(l, 1, modt[0], "mod0", plus_one=True, mul_row=norm_mix_w[l:l + 1, :])
        mod_vector(l, 0, modt[1], "mod1")
        for g in range(NG):
            for t in range(4):
                xt, xtk = xts[t % 2], "xt%d" % (t % 2)
                r0 = g * TG + t * 128
                P.dma("sp", lambda e, xt=xt, r0=r0: e.dma_start(out=xt[:], in_=xsrc[r0:r0 + 128, :]), [xsrc_key + str(g * 4 + t)], [xtk])
                rmsnorm_tile(xt, xtk, modt[0], "mod0", modt[1], "mod1", hb)
                P.pool(lambda e, xt=xt: e.tensor_copy(hb[:], xt[:]), [xtk], ["hb"])
                for half in range(2):
                    bk, bkk = bank()
                    bkb = bk[:, :].bitcast(BF16)
                    for kk in range(8):
                        k = half * 8 + kk
                        P.pe(lambda e, bkb=bkb, kk=kk, k=k: e.transpose(out=bkb[:, kk * 128:(kk + 1) * 128],
                                                                        in_=hb[:, k * 128:(k + 1) * 128], identity=ident_bf[:]),
                             ["hb", "ident_bf"], [bkk])
                    P.act(lambda e, bkb=bkb, half=half, t=t: e.copy(
                        out=hT[:, half * 8:half * 8 + 8, t * 128:(t + 1) * 128],
                        in_=bkb[:, 0:1024].rearrange("p (k c) -> p k c", k=8)), [bkk], ["hT"])
            P.dma("sp", lambda e, g=g: e.dma_start(out=Hs[:, :, g * TG:(g + 1) * TG].rearrange("k p t -> p k t"), in_=hT[:]),
                  ["hT"], ["Hs"])
            for wi in range(2):
                wt, wtk = load_wt(wts, winb, OFF_P + wi * 512, 512, "winb")
                for cc in range(4):
                    ch = wi * 4 + cc
                    bk, bkk = proj_fm(wt, wtk, cc * 128)
                    P.act(lambda e, bk=bk, ch=ch: e.copy(out=pin[:, ch, 15:15 + TG], in_=bk[:, :]), [bkk], ["pin"])
            for ch in range(8):
                gi = ch // 2
                w = (2, 4, 8, 16)[gi]
                src = pin[:, ch, :]
                srck = "pin"
                sh = 1
                for lvl in range(gi + 1):
                    dst, dstk = pacc[lvl % 2], "pacc%d" % (lvl % 2)
                    n = 15 + TG - sh
                    P.pool(lambda e, dst=dst, src=src, sh=sh, n=n: e.tensor_tensor(
                        out=dst[:, sh:sh + n], in0=src[:, sh:sh + n], in1=src[:, 0:n], op=ALU.add), [srck], [dstk])
                    src, srck = dst[:, :], dstk
                    sh *= 2
                P.dve(lambda e, src=src, ch=ch, w=w: e.scalar_tensor_tensor(
                    out=pooled[:, ch, :], in0=src[:, 15:15 + TG], scalar=1.0 / w, in1=pin[:, ch, 15:15 + TG],
                    op0=ALU.mult, op1=ALU.subtract), [srck, "pin"], ["pooled"])
                if g == 0:
                    P.dve(lambda e, src=src, gi=gi: e.tensor_tensor(
                        out=junk[:, 0:16], in0=src[:, 15:31], in1=cst[:, C_INVCNT + gi * 16:C_INVCNT + gi * 16 + 16],
                        op=ALU.mult), [srck, "cst"], ["junk"])
                    P.dve(lambda e, ch=ch: e.tensor_sub(pooled[:, ch, 0:16], junk[:, 0:16], pin[:, ch, 15:31]),
                          ["junk", "pin"], ["pooled"])
            P.dve(lambda e: e.tensor_copy(junk[:, 0:120].rearrange("p (a c) -> p a c", a=8), pin[:, :, TG:TG + 15]), ["pin"], ["junk"])
            P.dve(lambda e: e.tensor_copy(pin[:, :, 0:15], junk[:, 0:120].rearrange("p (a c) -> p a c", a=8)), ["junk"], ["pin"])
            for gi in range(4):
                for dc in range(2):
                    bk, bkk = bank()
                    for cc in range(2):
                        P.pe(lambda e, bk=bk, gi=gi, dc=dc, cc=cc: e.matmul(
                            bk[:, :], lhsT=pwb[:, gi * 2 + cc, dc * 128:(dc + 1) * 128], rhs=pooled[:, gi * 2 + cc, :],
                            start=(cc == 0), stop=(cc == 1)), ["pwb", "pooled"], [bkk])
                    P.act(lambda e, bk=bk, gi=gi, dc=dc: e.activation(
                        out=ypT[:, gi * 2 + dc, :], in_=bk[:, :], func=AF.Copy, scale=psc[:, gi * 2 + dc:gi * 2 + dc + 1]),
                        [bkk, "psc"], ["ypT"])
            P.dma("sp", lambda e, g=g: e.dma_start(out=Yp[:, :, g * TG:(g + 1) * TG].rearrange("k p t -> p k t"), in_=ypT[:]),
                  ["ypT"], ["Yp"])
            for h in range(8):
                wt, wtk = load_wt(wts, winb, OFF_D + h * 384, 384, "winb")
                P.dve(lambda e, h=h: e.tensor_copy(cin[:, :, 0:3], ctail[:, h * 3:h * 3 + 3, :]), ["ctail"], ["cin"])
                for i3 in range(3):
                    bk, bkk = proj_fm(wt, wtk, i3 * 128)
                    P.act(lambda e, bk=bk, i3=i3: e.copy(out=cin[:, i3, 3:3 + TG], in_=bk[:, :]), [bkk], ["cin"])
                P.dve(lambda e, h=h: e.tensor_copy(ctail[:, h * 3:h * 3 + 3, :], cin[:, :, TG:TG + 3]), ["cin"], ["ctail"])
                for i3 in range(3):
                    ch = i3 * 8 + h
                    for j in range(4):
                        wcol = cvw[:, ch * 4 + j:ch * 4 + j + 1]
                        if j == 0:
                            P.dve(lambda e, i3=i3, wcol=wcol: e.tensor_scalar(cacc[:, i3, :], cin[:, i3, 0:TG], wcol, None, ALU.mult),
                                  ["cin", "cvw"], ["cacc"])
                        else:
                            P.dve(lambda e, i3=i3, j=j, wcol=wcol: e.scalar_tensor_tensor(
                                out=cacc[:, i3, :], in0=cin[:, i3, j:j + TG], scalar=wcol, in1=cacc[:, i3, :],
                                op0=ALU.mult, op1=ALU.add), ["cin", "cvw", "cacc"], ["cacc"])
                    P.act(lambda e, i3=i3: e.activation(out=cacc[:, i3, :], in_=cacc[:, i3, :], func=AF.Silu), ["cacc"], ["cacc"])
                    if i3 < 2:
                        P.act(lambda e, i3=i3: e.activation(out=csq[:], in_=cacc[:, i3, :], func=AF.Square), ["cacc"], ["csq"])
                        bk, bkk = bank()
                        P.pe(lambda e, bk=bk: e.matmul(bk[:, :], lhsT=cm("ones"), rhs=csq[:], start=True, stop=True), ["cst", "csq"], [bkk])
                        P.act(lambda e, bk=bk: e.activation(out=crn[:], in_=bk[:, :], func=AF.Sqrt, bias=EPS), [bkk], ["crn"])
                        P.dve(lambda e: e.reciprocal(out=crn[:], in_=crn[:]), ["crn"], ["crn"])
                        sc_ = (128.0 ** -0.5) if i3 == 0 else 1.0
                        P.dve(lambda e, i3=i3, sc_=sc_: e.scalar_tensor_tensor(
                            out=cacc[:, i3, :], in0=cacc[:, i3, :], scalar=sc_, in1=crn[:], op0=ALU.mult, op1=ALU.mult),
                            ["cacc", "crn"], ["cacc"])
                    dstT = (DNq, DNk, DNv)[i3]
                    P.dma("sp", lambda e, dstT=dstT, h=h, g=g, i3=i3: e.dma_start(
                        out=dstT[h, :, g * TG:(g + 1) * TG], in_=cacc[:, i3, :]), ["cacc"], ["DN%d" % i3])
            for blk, off in ((0, OFF_GD), (1, OFF_V)):
                for wi in range(2):
                    wt, wtk = load_wt(wts, winb, off + wi * 512, 512, "winb")
                    for t in range(4):
                        bk, bkk = proj_tm(wt, wtk, t, 0, 512)
                        if blk == 0:
                            P.act(lambda e, bk=bk, t=t, wi=wi: e.activation(out=tm_bf[:, t, wi * 512:(wi + 1) * 512], in_=bk[:, :], func=AF.Silu),
                                  [bkk], ["tm_bf"])
                        else:
                            P.act(lambda e, bk=bk, t=t, wi=wi: e.copy(out=tm_bf[:, t, wi * 512:(wi + 1) * 512], in_=bk[:, :]), [bkk], ["tm_bf"])
                if blk == 0:
                    P.dma("sp", lambda e, g=g: e.dma_start(out=Gdn[g * TG:(g + 1) * TG, :].rearrange("(t p) c -> p t c", p=128), in_=tm_bf[:]),
                          ["tm_bf"], ["Gdn"])
                else:
                    P.dma("sp", lambda e, g=g: e.dma_start(out=Vc[:, g * 4:(g + 1) * 4, :], in_=tm_bf[:]), ["tm_bf"], ["Vc"])
            wtA, wtAk = load_wt(wts, winb, OFF_SBA, 16, "winb")
            wtB, wtBk = load_wt(wts, winb, OFF_SF, 8, "winb")
            for t in range(4):
                bk, bkk = proj_tm(wtA, wtAk, t, 0, 16)
                proj_tm(wtB, wtBk, t, 0, 8, bk=bk, bkk=bkk, o0=16)
                P.act(lambda e, bk=bk, g=g, t=t: e.copy(out=sg[:, g * 4 + t, :], in_=bk[:, 0:24]), [bkk], ["sg"])
            for h in range(8):
                if h % 2 == 0:
                    wt, wtk = load_wt(wts, winb, OFF_F + h * 256, 512, "winb")
                for i2 in range(2):
                    bk, bkk = proj_fm(wt, wtk, (h % 2) * 256 + i2 * 128)
                    P.act(lambda e, bk=bk, i2=i2: e.copy(out=fqk[:, i2, :], in_=bk[:, :]), [bkk], ["fqk"])
                    dstT = (Qc, Kc)[i2]
                    P.dma("sp", lambda e, dstT=dstT, h=h, g=g, i2=i2: e.dma_start(
                        out=dstT[h, :, g * TG:(g + 1) * TG], in_=fqk[:, i2, :]), ["fqk"], ["QK%d" % i2])


    def bc_last(ap2, n):
        return ap2.unsqueeze(2).to_broadcast([128, ap2.shape[1], n])

    def bc_mid(ap2, a):
        return ap2.unsqueeze(1).to_broadcast([128, a, ap2.shape[1]])

    def v4(bk):
        return bk[:, :].rearrange("p (a c) -> p a c", a=4)

    def phase_dn(l):
        new_phase()
        NT = S // 128
        A = lambda n, sh, dt=F32: AR.alloc(n, sh, dt)
        beta, gg, gc, glt, eg, sdA, sdB, sbg, nbeta = [A(n, [NT, 8]) for n in
                                                       ("beta", "gg", "gc", "glt", "eg", "sdA", "sdB", "sbg", "nbeta")]
        glrep = A("glrep", [NT, 2, 8])
        dtb, alg = A("dtb", [8]), A("alg", [8])
        onb = A("onb", [128])
        qT, kT, vT = A("qT", [S]), A("kT", [S]), A("vT", [S])
        gtm = A("gtm", [NT, 128], BF16)
        yT = A("yT", [S], BF16)
        Sst = A("Sst", [128])
        vn = A("vn", [128])
        o1 = A("o1", [128])
        M = {n: A(n, [4, 128]) for n in ("rhsG", "Dm", "DTm", "tmpN", "Na", "Nb", "Pa", "Pb", "R", "aT", "kbg", "kdA",
                                         "kdB", "vb", "u", "wkT", "o4", "sq")}
        yb = A("yb", [4, 128], BF16)
        ssum = A("ssum", [8])
        G = ["dn_gates"]
        P.dma("sp", lambda e: e.dma_start(out=dtb[:], in_=dt_bias_bc[l]), [], ["dtb"])
        P.dma("sp", lambda e: e.dma_start(out=alg[:], in_=a_log_bc[l]), [], ["alg"])
        P.dma("sp", lambda e: e.dma_start(out=onb[:], in_=onorm_bc[l]), [], ["onb"])
        P.act(lambda e: e.activation(out=beta[:], in_=sg[:, :, 0:8], func=AF.Sigmoid), ["sg"], G)
        P.dve(lambda e: e.tensor_tensor(out=gg[:], in0=sg[:, :, 8:16], in1=bc_mid(dtb[:], NT), op=ALU.add), ["sg", "dtb"], G)
        P.act(lambda e: e.activation(out=gg[:], in_=gg[:], func=AF.Exp), G, G)
        P.act(lambda e: e.activation(out=gg[:], in_=gg[:], func=AF.Ln, bias=1.0), G, G)
        P.act(lambda e: e.activation(out=alg[:], in_=alg[:], func=AF.Exp), ["alg"], ["alg"])
        P.dve(lambda e: e.tensor_scalar(alg[:], alg[:], -1.0, None, ALU.mult), ["alg"], ["alg"])
        P.dve(lambda e: e.tensor_tensor(out=gg[:], in0=gg[:], in1=bc_mid(alg[:], NT), op=ALU.mult), G + ["alg"], G)
        gg2 = gg[:].rearrange("p a b -> p (a b)")
        for dst, cname in ((gc, "triblk"), (glt, "blkones")):
            bk, bkk = bank()
            P.pe(lambda e, bk=bk, cname=cname: e.matmul(bk[:, 0:256], lhsT=cm(cname), rhs=gg2, start=True, stop=True), ["cst"] + G, [bkk])
            P.act(lambda e, bk=bk, dst=dst: e.copy(out=dst[:].rearrange("p a b -> p (a b)"), in_=bk[:, 0:256]), [bkk], G)
        for bi, cname in ((0, "selA"), (1, "selB")):
            bk, bkk = bank()
            P.pe(lambda e, bk=bk, cname=cname: e.matmul(bk[:, 0:256], lhsT=cm(cname), rhs=gg2, start=True, stop=True), ["cst"] + G, [bkk])
            P.act(lambda e, bk=bk, bi=bi: e.activation(out=glrep[:, :, bi, :], in_=bk[:, 0:256].rearrange("p (a b) -> p a b", a=NT),
                                                       func=AF.Exp), [bkk], G)
        P.act(lambda e: e.activation(out=eg[:], in_=gc[:], func=AF.Exp), G, G)
        P.dve(lambda e: e.tensor_sub(glt[:], glt[:], gc[:]), G, G)
        P.act(lambda e: e.activation(out=glt[:], in_=glt[:], func=AF.Exp), G, G)
        P.dve(lambda e: e.tensor_scalar(sdA[:], glt[:], cst[:, C_MASKA:C_MASKA + 1], None, ALU.mult), G + ["cst"], G)
        P.dve(lambda e: e.tensor_scalar(sdB[:], glt[:], cst[:, C_MASKB:C_MASKB + 1], None, ALU.mult), G + ["cst"], G)
        P.dve(lambda e: e.tensor_mul(sbg[:], beta[:], eg[:]), G, G)
        P.dve(lambda e: e.tensor_scalar(nbeta[:], beta[:], -1.0, None, ALU.mult), G, G)
        P.pool(lambda e: e.memset(vn[:], 0.0), [], ["vn"])
        identf = cm("ident")

        for h in range(cfg.get("dn_heads", 8)):
            P.dma("sp", lambda e, h=h: e.dma_start(out=qT[:], in_=DNq[h]), ["DN0"], ["qT"])
            P.dma("sp", lambda e, h=h: e.dma_start(out=kT[:], in_=DNk[h]), ["DN1"], ["kT"])
            P.dma("sp", lambda e, h=h: e.dma_start(out=vT[:], in_=DNv[h]), ["DN2"], ["vT"])
            P.dma("sp", lambda e, h=h: e.dma_start(out=gtm[:], in_=Gdn[:, h * 128:(h + 1) * 128].rearrange("(t p) c -> p t c", p=128)),
                  ["Gdn"], ["gtm"])
            P.pool(lambda e: e.memset(Sst[:], 0.0), [], ["Sst"])
            for bt in range(cfg.get("dn_batches", S // 512)):
                t0 = bt * 4
                dn_stage = cfg.get("dn_stage", 9)
                tk = lambda i: slice((t0 + i) * 128, (t0 + i + 1) * 128)

                def mm4(lhs_fn, rhs_fn, reads, transpose=False):
                    bk, bkk = bank()
                    for i in range(4):
                        la = lhs_fn(i)
                        if transpose:
                            P.pe(lambda e, bk=bk, i=i, la=la: e.matmul(bk[:, i * 128:(i + 1) * 128], lhsT=la, rhs=identf,
                                                                       start=True, stop=True), reads + ["cst"], [bkk])
                        else:
                            ra = rhs_fn(i)
                            P.pe(lambda e, bk=bk, i=i, la=la, ra=ra: e.matmul(bk[:, i * 128:(i + 1) * 128], lhsT=la, rhs=ra,
                                                                              start=True, stop=True), reads, [bkk])
                    return bk, bkk
                if dn_stage < 1:
                    continue
                sub = cfg.get("dn_sub", 99)
                if sub < 99:
                    bKK, kKK = mm4(lambda i: kT[:, tk(i)], lambda i: kT[:, tk(i)], ["kT"])
                    if sub >= 1:
                        P.dve(lambda e, h=h, t0=t0: e.tensor_tensor(out=M["rhsG"][:], in0=bc_mid(cm("strictU"), 4),
                                                                    in1=bc_last(gg[:, t0:t0 + 4, h], 128), op=ALU.mult), ["cst"] + G, ["rhsG"])
                    if sub >= 2:
                        bDd, kDd = mm4(lambda i: cm("triblk"), lambda i: M["rhsG"][:, i, :], ["cst", "rhsG"])
                    if sub >= 3:
                        P.act(lambda e, bk=bDd: e.activation(out=M["Dm"][:], in_=v4(bk), func=AF.Exp), [kDd], ["Dm"])
                    if sub >= 4:
                        P.dve(lambda e, bk=bKK: e.tensor_tensor(out=M["Na"][:], in0=v4(bk), in1=M["Dm"][:], op=ALU.mult), [kKK, "Dm"], ["Na"])
                    if sub >= 5:
                        bP, kP = mm4(lambda i: M["Na"][:, i, :], None, ["Na"], transpose=True)
                    if sub >= 6:
                        P.act(lambda e, bk=bP: e.copy(out=M["Pa"][:], in_=v4(bk)), [kP], ["Pa"])
                    continue
                bKK, kKK = mm4(lambda i: kT[:, tk(i)], lambda i: kT[:, tk(i)], ["kT"])
                bKQ, kKQ = mm4(lambda i: kT[:, tk(i)], lambda i: qT[:, tk(i)], ["kT", "qT"])
                P.dve(lambda e, h=h, t0=t0: e.tensor_tensor(out=M["rhsG"][:], in0=bc_mid(cm("strictU"), 4),
                                                            in1=bc_last(gg[:, t0:t0 + 4, h], 128), op=ALU.mult), ["cst"] + G, ["rhsG"])
                bDd, kDd = mm4(lambda i: cm("triblk"), lambda i: M["rhsG"][:, i, :], ["cst", "rhsG"])
                P.act(lambda e, bk=bDd: e.activation(out=M["Dm"][:], in_=v4(bk), func=AF.Exp), [kDd], ["Dm"])
                bDT, kDT = mm4(lambda i: M["Dm"][:, i, :], None, ["Dm"], transpose=True)
                P.dve(lambda e, bk=bDT: e.tensor_tensor(out=M["DTm"][:], in0=v4(bk), in1=bc_mid(cm("triblk"), 4), op=ALU.mult), [kDT, "cst"], ["DTm"])
                P.dve(lambda e: e.tensor_tensor(out=M["Dm"][:], in0=M["Dm"][:], in1=bc_mid(cm("strictU"), 4), op=ALU.mult), ["Dm", "cst"], ["Dm"])
                P.dve(lambda e, h=h, t0=t0: e.tensor_tensor(out=M["tmpN"][:], in0=M["Dm"][:], in1=bc_last(nbeta[:, t0:t0 + 4, h], 128),
                                                            op=ALU.mult), ["Dm"] + G, ["tmpN"])
                P.dve(lambda e, bk=bKK: e.tensor_tensor(out=M["Na"][:], in0=v4(bk), in1=M["tmpN"][:], op=ALU.mult), [kKK, "tmpN"], ["Na"])
                P.dve(lambda e, bk=bKQ: e.tensor_tensor(out=M["aT"][:], in0=v4(bk), in1=M["DTm"][:], op=ALU.mult), [kKQ, "DTm"], ["aT"])
                bP, kP = mm4(lambda i: M["Na"][:, i, :], None, ["Na"], transpose=True)
                P.act(lambda e, bk=bP: e.copy(out=M["Pa"][:], in_=v4(bk)), [kP], ["Pa"])
                P.dve(lambda e: e.tensor_tensor(out=M["R"][:], in0=M["Pa"][:], in1=bc_mid(identf, 4), op=ALU.add), ["Pa", "cst"], ["R"])
                N1, P1, N2, P2 = "Na", "Pa", "Nb", "Pb"
                for lev in range(5):
                    bN, kN = mm4(lambda i, P1=P1: M[P1][:, i, :], lambda i, N1=N1: M[N1][:, i, :], [N1, P1])
                    P.act(lambda e, bk=bN, N2=N2: e.copy(out=M[N2][:], in_=v4(bk)), [kN], [N2])
                    if lev < 4:
                        bQ, kQ = mm4(lambda i, N1=N1: M[N1][:, i, :], lambda i, P1=P1: M[P1][:, i, :], [N1, P1])
                        P.act(lambda e, bk=bQ, P2=P2: e.copy(out=M[P2][:], in_=v4(bk)), [kQ], [P2])
                    bR, kR = mm4(lambda i, N2=N2: M[N2][:, i, :], lambda i: M["R"][:, i, :], [N2, "R"])
                    P.dve(lambda e, bk=bR: e.tensor_tensor(out=M["R"][:], in0=v4(bk), in1=M["R"][:], op=ALU.add), [kR, "R"], ["R"])
                    N1, P1, N2, P2 = N2, P2, N1, P1
                bKt, kKt = mm4(lambda i: kT[:, tk(i)], None, ["kT"], transpose=True)
                bVt, kVt = mm4(lambda i: vT[:, tk(i)], None, ["vT"], transpose=True)
                for dstn, scal in (("kbg", sbg), ("kdA", sdA), ("kdB", sdB)):
                    P.dve(lambda e, bk=bKt, dstn=dstn, scal=scal, h=h, t0=t0: e.tensor_tensor(
                        out=M[dstn][:], in0=v4(bk), in1=bc_last(scal[:, t0:t0 + 4, h], 128), op=ALU.mult), [kKt] + G, [dstn])
                P.dve(lambda e, bk=bVt, h=h, t0=t0: e.tensor_tensor(out=M["vb"][:], in0=v4(bk), in1=bc_last(beta[:, t0:t0 + 4, h], 128),
                                                                    op=ALU.mult), [kVt] + G, ["vb"])
                bU, kU = mm4(lambda i: M["R"][:, i, :], lambda i: M["vb"][:, i, :], ["R", "vb"])
                P.act(lambda e, bk=bU: e.copy(out=M["u"][:], in_=v4(bk)), [kU], ["u"])
                bW, kW = mm4(lambda i: M["kbg"][:, i, :], lambda i: M["R"][:, i, :], ["R", "kbg"])
                P.act(lambda e, bk=bW: e.copy(out=M["wkT"][:], in_=v4(bk)), [kW], ["wkT"])
                if cfg.get("dn_dump") and bt == 0 and h == 0:
                    lim = P.limit
                    P.limit = None
                    for nm in ("Na", "Pa", "Nb", "Pb", "R", "Dm", "DTm", "aT", "rhsG", "kbg", "kdA", "kdB", "vb", "u", "wkT"):
                        o = dout("dump_" + nm, [128, 4, 128])
                        P.dma("sp", lambda e, o=o, nm=nm: e.dma_start(out=o[:, :, :], in_=M[nm][:]), [nm], [])
                    for nm, t_ in (("gg", gg), ("beta", beta), ("gc", gc), ("eg", eg), ("sdA", sdA), ("sbg", sbg)):
                        o = dout("dump_" + nm, [128, NT, 8])
                        P.dma("sp", lambda e, o=o, t_=t_: e.dma_start(out=o[:, :, :], in_=t_[:]), G, [])
                    o = dout("dump_glrep", [128, NT, 2, 8])
                    P.dma("sp", lambda e, o=o: e.dma_start(out=o[:, :, :, :], in_=glrep[:]), G, [])
                    P.limit = lim
                for i in range(4 if dn_stage >= 2 else 0):
                    t = t0 + i
                    for half, (lo, hi), kd in ((0, (0, 64), "kdA"), (1, (64, 128), "kdB")):
                        bk, bkk = bank()
                        P.pe(lambda e, bk=bk, i=i: e.matmul(bk[:, 0:128], lhsT=M["wkT"][:, i, :], rhs=Sst[:], start=True, stop=True),
                             ["wkT", "Sst"], [bkk])
                        qsl = qT[:, tk(i)]
                        P.pe(lambda e, bk=bk, qsl=qsl: e.matmul(bk[:, 128:256], lhsT=qsl, rhs=Sst[:], start=True, stop=True),
                             ["qT", "Sst"], [bkk])
                        P.dve(lambda e, bk=bk, i=i, lo=lo, hi=hi: e.tensor_sub(vn[lo:hi, :], M["u"][lo:hi, i, :], bk[lo:hi, 0:128]),
                              ["u", bkk], ["vn"])
                        P.dve(lambda e, bk=bk, lo=lo, hi=hi, t=t, h=h: e.tensor_scalar(o1[lo:hi, :], bk[lo:hi, 128:256], eg[lo:hi, t, h:h + 1],
                                                                                     None, ALU.mult), [bkk] + G, ["o1"])
                        bk2, bkk2 = bank()
                        P.pe(lambda e, bk2=bk2, i=i, kd=kd: e.matmul(bk2[:, 0:128], lhsT=M[kd][:, i, :], rhs=vn[:], start=True, stop=True),
                             [kd, "vn"], [bkk2])
                        P.dve(lambda e, bk2=bk2, t=t, half=half, h=h: e.scalar_tensor_tensor(
                            out=Sst[:], in0=Sst[:], scalar=glrep[:, t, half, h:h + 1], in1=bk2[:, 0:128], op0=ALU.mult, op1=ALU.add),
                            [bkk2, "Sst"] + G, ["Sst"])
                    bk3, bkk3 = bank()
                    P.pe(lambda e, bk3=bk3, i=i: e.matmul(bk3[:, 0:128], lhsT=M["aT"][:, i, :], rhs=vn[:], start=True, stop=True),
                         ["aT", "vn"], [bkk3])
                    P.dve(lambda e, bk3=bk3, i=i: e.tensor_add(M["o4"][:, i, :], o1[:], bk3[:, 0:128]), [bkk3, "o1"], ["o4"])
                if cfg.get("dn_dump") and dn_stage >= 2 and bt == 0 and h == 0:
                    o = dout("dump_o4", [128, 4, 128])
                    P.dma("sp", lambda e, o=o: e.dma_start(out=o[:, :, :], in_=M["o4"][:]), ["o4"], [])
                    o = dout("dump_S", [128, 128])
                    P.dma("sp", lambda e, o=o: e.dma_start(out=o[:, :], in_=Sst[:]), ["Sst"], [])
                if dn_stage < 3:
                    continue
                P.pool(lambda e: e.tensor_tensor(out=M["sq"][:], in0=M["o4"][:], in1=M["o4"][:], op=ALU.mult), ["o4"], ["sq"])
                P.dve(lambda e: e.tensor_reduce(out=ssum[:, 0:4], in_=M["sq"][:], axis=AX.X, op=ALU.add), ["sq"], ["ssum"])
                P.act(lambda e: e.activation(out=ssum[:, 4:8], in_=ssum[:, 0:4], func=AF.Sqrt, scale=1.0 / 128, bias=EPS), ["ssum"], ["ssum"])
                P.dve(lambda e: e.reciprocal(out=ssum[:, 4:8], in_=ssum[:, 4:8]), ["ssum"], ["ssum"])
                P.dve(lambda e: e.tensor_tensor(out=M["o4"][:], in0=M["o4"][:], in1=bc_last(ssum[:, 4:8], 128), op=ALU.mult), ["o4", "ssum"], ["o4"])
                P.dve(lambda e: e.tensor_tensor(out=M["o4"][:], in0=M["o4"][:], in1=bc_mid(onb[:], 4), op=ALU.mult), ["o4", "onb"], ["o4"])
                P.dve(lambda e, t0=t0: e.tensor_tensor(out=yb[:], in0=M["o4"][:], in1=gtm[:, t0:t0 + 4, :], op=ALU.mult), ["o4", "gtm"], ["yb"])
                if cfg.get("dn_dump") and bt == 0 and h == 0:
                    o = dout("dump_yb", [128, 4, 128], BF16)
                    P.dma("sp", lambda e, o=o: e.dma_start(out=o[:, :, :], in_=yb[:]), ["yb"], [])
                    o = dout("dump_ssum", [128, 8])
                    P.dma("sp", lambda e, o=o: e.dma_start(out=o[:, :], in_=ssum[:]), ["ssum"], [])
                bk, bkk = bank()
                bkb = bk[:, :].bitcast(BF16)
                for i in range(4):
                    P.pe(lambda e, bkb=bkb, i=i: e.transpose(out=bkb[:, i * 128:(i + 1) * 128], in_=yb[:, i, :], identity=ident_bf[:]),
                         ["yb", "ident_bf"], [bkk])
                P.act(lambda e, bkb=bkb, bt=bt: e.copy(out=yT[:, bt * 512:(bt + 1) * 512], in_=bkb[:, 0:512]), [bkk], ["yT"])
            if cfg.get("dn_stage", 9) >= 3:
                P.dma("sp", lambda e, h=h: e.dma_start(out=Yd[h], in_=yT[:]), ["yT"], ["Yd"])


    FILL = {}

    def fill_reg(e):
        if "r" not in FILL:
            FILL["r"] = e.to_reg(-30000.0)
        return FILL["r"]

    def phase_fox(l):
        new_phase()
        NT = S // 128
        A = lambda n, sh, dt=F32: AR.alloc(n, sh, dt)
        lf, cf, tot, incl = A("lf", [NT, 8]), A("cf", [NT, 8]), A("tot", [NT, 8]), A("incl", [NT, 8])
        fbb = A("fbb", [8])
        rrep = A("rrep", [8, 8])
        sel8 = A("sel8", [8, 128])
        cfT = A("cfT", [S])
        rowv = A("rowv", [512])
        cfq = A("cfq", [512])
        bkg = A("bkg", [NT])
        qT, kT = A("fqT", [S], BF16), A("fkT", [S], BF16)
        Vh = A("Vh", [NT, 128], BF16)
        yT = A("fyT", [S], BF16)
        tmps = [A("ftmp%d" % i, [512]) for i in range(2)]
        pTs = [A("fpT%d" % i, [512], BF16) for i in range(2)]
        rinv = A("rinv", [512])
        F = ["fox_gates"]
        P.pool(lambda e: e.memset(cfT[:], 0.0), [], ["cfT"])
        P.dma("sp", lambda e: e.dma_start(out=fbb[:], in_=f_bias_bc[l]), [], ["fbb"])
        P.dve(lambda e: e.tensor_tensor(out=lf[:], in0=sg[:, :, 16:24], in1=bc_mid(fbb[:], NT), op=ALU.add), ["sg", "fbb"], F)
        P.act(lambda e: e.activation(out=lf[:], in_=lf[:], func=AF.Exp, scale=-1.0), F, F)
        P.act(lambda e: e.activation(out=lf[:], in_=lf[:], func=AF.Ln, bias=1.0), F, F)
        P.dve(lambda e: e.tensor_scalar(lf[:], lf[:], -1.0, None, ALU.mult), F, F)
        lf2 = lf[:].rearrange("p a b -> p (a b)")
        for dst, cname in ((cf, "tri128"), (tot, "ones")):
            bk, bkk = bank()
            P.pe(lambda e, bk=bk, cname=cname: e.matmul(bk[:, 0:256], lhsT=cm(cname), rhs=lf2, start=True, stop=True), ["cst"] + F, [bkk])
            P.act(lambda e, bk=bk, dst=dst: e.copy(out=dst[:].rearrange("p a b -> p (a b)"), in_=bk[:, 0:256]), [bkk], F)
        for h in range(8):
            P.dve(lambda e, h=h: e.tensor_tensor_scan(out=incl[:, :, h], data0=cm("ones")[:, 0:NT], data1=tot[:, :, h], initial=0.0,
                                                     op0=ALU.mult, op1=ALU.add), ["cst"] + F, F)
        P.dve(lambda e: e.tensor_sub(incl[:], incl[:], tot[:]), F, F)
        P.dve(lambda e: e.tensor_add(cf[:], cf[:], incl[:]), F, F)
        bk, bkk = bank()
        cf4 = cf[:].rearrange("p (g f) h -> p g f h", f=4)[:, :, 0, :]
        P.pe(lambda e, bk=bk: e.matmul(bk[:, 0:64].rearrange("p (g h) -> p g h", g=8), lhsT=cm("selrow0"), rhs=cf4, start=True, stop=True),
             ["cst"] + F, [bkk])
        P.act(lambda e, bk=bk: e.copy(out=rrep[:].rearrange("p g h -> p (g h)"), in_=bk[:, 0:64]), [bkk], F)
        for q4 in range(8):
            bk, bkk = bank()
            for i in range(4):
                t = q4 * 4 + i
                P.pe(lambda e, bk=bk, i=i, t=t: e.matmul(bk[0:8, i * 128:(i + 1) * 128], lhsT=cf[:, t, :], rhs=cm("ident"), start=True, stop=True),
                     ["cst"] + F, [bkk])
            P.act(lambda e, bk=bk, q4=q4: e.copy(out=cfT[0:8, q4 * 512:(q4 + 1) * 512], in_=bk[0:8, :]), [bkk], ["cfT"])
        P.pool(lambda e: e.memset(sel8[:], 0.0), [], ["sel8"])
        P.pool(lambda e: e.memset(rowv[:], 0.0), [], ["rowv"])
        P.dve(lambda e: e.tensor_copy(sel8[0:8, :, :], cm("ident")[0:8, 0:8].unsqueeze(2).to_broadcast([8, 8, 128])), ["cst", "sel8"], ["sel8"])
        scale = 128.0 ** -0.5
        nheads = cfg.get("fox_heads", 8)
        for h in range(nheads):
            P.dma("sp", lambda e, h=h: e.dma_start(out=qT[:], in_=Qc[h]), ["QK0"], ["fqT"])
            P.dma("sp", lambda e, h=h: e.dma_start(out=kT[:], in_=Kc[h]), ["QK1"], ["fkT"])
            P.dma("sp", lambda e, h=h: e.dma_start(out=Vh[:], in_=Vc[:, :, h * 128:(h + 1) * 128]), ["Vc"], ["Vh"])
            for g in range(cfg.get("fox_groups", NG)):
                gs = slice(g * 512, (g + 1) * 512)
                nj = 4 * g + 4
                P.dve(lambda e, g=g, gs=gs: e.tensor_scalar(rowv[0:8, :], cfT[0:8, gs], cfT[0:8, g * 512:g * 512 + 1], None,
                                                            ALU.subtract), ["cfT"], ["rowv"])
                P.bank_lo, P.bank_n = 0, 4
                bk, bkk = bank()
                P.pe(lambda e, bk=bk, h=h: e.matmul(bk[:, :], lhsT=sel8[0:8, h, :], rhs=rowv[0:8, :], start=True, stop=True), ["sel8", "rowv"], [bkk])
                P.act(lambda e, bk=bk: e.copy(out=cfq[:], in_=bk[:, :]), [bkk], ["cfq"])
                P.dve(lambda e, g=g, h=h, nj=nj: e.tensor_scalar(bkg[:, 0:nj], cf[:, 0:nj, h], -1.0, rrep[:, g, h:h + 1], ALU.mult, ALU.add),
                      F, ["bkg"])
                if cfg.get("fox_dump") and g == 0 and h == 0:
                    for nm, t_, shp in (("cfq", cfq, [128, 512]), ("rowv", rowv, [128, 512]), ("cfT", cfT, [128, S]), ("cf", cf, [128, NT, 8]),
                                        ("rrep", rrep, [128, 8, 8]), ("sel8", sel8, [128, 8, 128]), ("bkg", bkg, [128, NT])):
                        o = dout("dump_" + nm, shp)
                        P.dma("sp", lambda e, o=o, t_=t_: e.dma_start(out=o, in_=t_[:]), ["cfq", "rowv", "cfT", "sel8", "bkg"] + F, [])
                    continue
                ai = 4 + 2 * (g % 2)
                accO, kO, accS, kS = banks[ai], "bank%d" % ai, banks[ai + 1], "bank%d" % (ai + 1)

                def qk(j):
                    c0 = 0
                    n = 512
                    bs, bsk = bank()
                    ka = kT[:, j * 128:(j + 1) * 128]
                    qa = qT[:, g * 512 + c0:(g + 1) * 512]
                    P.pe(lambda e: e.matmul(bs[:, 0:n], lhsT=ka, rhs=qa, start=True, stop=True), ["fkT", "fqT"], [bsk])
                    return bs, bsk, c0, n
                nxt = qk(0)
                for j in range(nj):
                    bs, bsk, c0, n = nxt
                    tmp, tmpk = tmps[j % 2], "ftmp%d" % (j % 2)
                    pT, pTk = pTs[j % 2], "fpT%d" % (j % 2)
                    P.dve(lambda e, bs=bs, tmp=tmp, c0=c0, n=n: e.scalar_tensor_tensor(
                        out=tmp[:, 0:n], in0=bs[:, 0:n], scalar=scale, in1=cfq[:, c0:512], op0=ALU.mult, op1=ALU.add), [bsk, "cfq"], [tmpk])
                    if j >= 4 * g:
                        jj = j - 4 * g
                        P.pool(lambda e, tmp=tmp, jj=jj: e.affine_select(out=tmp[:, :], in_=tmp[:, :], pattern=[[1, 512]], compare_op=ALU.is_ge,
                                                                         fill=fill_reg(e), base=-128 * jj, channel_multiplier=-1), [tmpk], [tmpk])
                    P.act(lambda e, tmp=tmp, pT=pT, n=n, j=j: e.activation(out=pT[:, 0:n], in_=tmp[:, 0:n], func=AF.Exp, bias=bkg[:, j:j + 1]),
                          [tmpk, "bkg"], [pTk])
                    if j + 1 < nj:
                        nxt = qk(j + 1)
                    va = Vh[:, j, :]
                    P.pe(lambda e, pT=pT, va=va, c0=c0, n=n, j=j, accO=accO, nj=nj: e.matmul(accO[:, c0:512], lhsT=va, rhs=pT[:, 0:n], start=(j == 0), stop=(j == nj - 1)),
                         ["Vh", pTk], [kO])
                    P.pe(lambda e, pT=pT, c0=c0, n=n, j=j, accS=accS, nj=nj: e.matmul(accS[:, c0:512], lhsT=ones_bf[:], rhs=pT[:, 0:n], start=(j == 0), stop=(j == nj - 1)),
                         ["ones_bf", pTk], [kS])
                P.dve(lambda e, accS=accS: e.reciprocal(out=rinv[:], in_=accS[:, :]), [kS], ["rinv"])
                P.dve(lambda e, gs=gs, accO=accO: e.tensor_tensor(out=yT[:, gs], in0=accO[:, :], in1=rinv[:], op=ALU.mult), [kO, "rinv"], ["fyT"])
            P.bank_lo, P.bank_n = 0, NBANK
            if cfg.get("fox_groups", NG) == NG:
                P.dma("sp", lambda e, h=h: e.dma_start(out=Yf[h], in_=yT[:]), ["fyT"], ["Yf"])


    def phase_merge(l, xsrc, xkey):
        new_phase()
        A = lambda n, sh, dt=F32: AR.alloc(n, sh, dt)
        hT = A("hT", [KC, TG], BF16)
        HT["hT"] = hT
        Ys = [A("Y%d" % b, [8, TG], BF16) for b in range(3)]
        wts = [A("wt%d" % i, [KC, 384], BF16) for i in range(2)]
        wbt = [A("wbt%d" % i, [3, 8, 128], BF16) for i in range(2)]
        gsb = A("gsb", [3, TG])
        macc, mtmp = A("macc", [TG]), A("mtmp", [TG])
        mergedT = A("mergedT", [KC, TG], BF16)
        xts = [A("xt0", [D])] * 2
        wos = [A("wo%d" % i, [KC, 512], BF16) for i in range(2)]
        rtmp = A("rtmp", [512])
        mod_vector(l, 2, modt[2], "mod2")
        Ysrc = (Yp, Yd, Yf)
        Ykey = ("Yp", "Yd", "Yf")
        for g in range(cfg.get("mg_groups", NG)):
            gs = slice(g * TG, (g + 1) * TG)
            P.dma("sp", lambda e, gs=gs: e.dma_start(out=hT[:], in_=Hs[:, :, gs].rearrange("k p t -> p k t")), ["Hs"], ["hT"])
            for b in range(3):
                P.dma("sp", lambda e, gs=gs, b=b: e.dma_start(out=Ys[b][:], in_=Ysrc[b][:, :, gs].rearrange("k p t -> p k t")), [Ykey[b]], ["Y%d" % b])
            for j in range(KC):
                wt, wtk = load_wt(wts, winb, OFF_G + j * 384, 384, "winb")
                wb, wbk = wbt[j % 2], "wbt%d" % (j % 2)
                for b in range(3):
                    P.dma("sp", lambda e, wb=wb, b=b, j=j: e.dma_start(
                        out=wb[:, b, :, :], in_=wbrb[b][:, j * 128:(j + 1) * 128].rearrange("(k p) c -> p k c", p=128)), ["wbrb"], [wbk])
                for b in range(3):
                    bk, bkk = proj_fm(wt, wtk, b * 128)
                    P.act(lambda e, bk=bk, b=b: e.activation(out=gsb[:, b, :], in_=bk[:, :], func=AF.Sigmoid), [bkk], ["gsb"])
                for b in range(3):
                    bz, bzk = bank()
                    for k in range(8):
                        P.pe(lambda e, bz=bz, wb=wb, b=b, k=k: e.matmul(bz[:, :], lhsT=wb[:, b, k, :], rhs=Ys[b][:, k, :],
                                                                        start=(k == 0), stop=(k == 7)), [wbk, "Y%d" % b], [bzk])
                    if b == 0:
                        P.dve(lambda e, bz=bz: e.tensor_tensor(out=macc[:], in0=bz[:, :], in1=gsb[:, 0, :], op=ALU.mult), [bzk, "gsb"], ["macc"])
                    else:
                        P.dve(lambda e, bz=bz, b=b: e.tensor_tensor(out=mtmp[:], in0=bz[:, :], in1=gsb[:, b, :], op=ALU.mult), [bzk, "gsb"], ["mtmp"])
                        if b == 1:
                            P.pool(lambda e: e.tensor_tensor(out=macc[:], in0=macc[:], in1=mtmp[:], op=ALU.add), ["macc", "mtmp"], ["macc"])
                        else:
                            P.pool(lambda e, j=j: e.tensor_tensor(out=mergedT[:, j, :], in0=macc[:], in1=mtmp[:], op=ALU.add),
                                   ["macc", "mtmp"], ["mergedT"])
            for t in range(4):
                ti = g * 4 + t
                xt, xtk = xts[0], "xt0"
                r0 = ti * 128
                P.dma("sp", lambda e, xt=xt, r0=r0: e.dma_start(out=xt[:], in_=xsrc[r0:r0 + 128, :]), [xkey + str(ti)], [xtk])
                for cb in range(4):
                    wo, wok = load_wt(wos, woutb, cb * 512, 512, "woutb", pref="wo")
                    bk, bkk = bank()
                    for k in range(KC):
                        P.pe(lambda e, bk=bk, wo=wo, k=k, t=t: e.matmul(bk[:, :], lhsT=mergedT[:, k, t * 128:(t + 1) * 128], rhs=wo[:, k, :],
                                                                        start=(k == 0), stop=(k == KC - 1)), [wok, "mergedT"], [bkk])
                    P.dve(lambda e, bk=bk, cb=cb: e.tensor_tensor(out=rtmp[:], in0=bk[:, :], in1=modt[2][:, cb * 512:(cb + 1) * 512], op=ALU.mult),
                          [bkk, "mod2"], ["rtmp"])
                    P.dve(lambda e, xt=xt, cb=cb: e.tensor_add(xt[:, cb * 512:(cb + 1) * 512], xt[:, cb * 512:(cb + 1) * 512], rtmp[:]),
                          ["rtmp", xtk], [xtk])
                P.dma("sp", lambda e, xt=xt, r0=r0: e.dma_start(out=xres[r0:r0 + 128, :], in_=xt[:]), [xtk], ["xres" + str(ti)])


    peer_u_flat = peer_u.rearrange("l e d -> (l e) d") if peer_u is not None else None
    peer_v_flat = peer_v.rearrange("l e d -> (l e) d") if peer_v is not None else None

    def phase_peer(l, last):
        new_phase()
        A = lambda n, sh, dt=F32: AR.alloc(n, sh, dt)
        hf = [A("hf%d" % i, [D]) for i in range(2)]
        hfT = A("hfT", [KC, 256])
        wq = A("wq", [KC, 128])
        qTc = A("qTc", [256])
        skt = A("skt", [16, 128])
        scs = A("scs", [2, 16, 128])
        cw = A("cw", [2048])
        cand = A("cand", [8, 16, 16])
        work = A("work", [2048])
        tv, tif = A("tv", [16, 16]), A("tif", [16, 16])
        ti = A("ti", [16, 16], U32)
        bs, bpf, i1, i2, sel1, sel2, wsm = [A(n, [8, 16]) for n in ("bs", "bpf", "i1", "i2", "sel1", "sel2", "wsm")]
        bp = A("bp", [8, 16], U32)
        idf, apre, coef = A("idf", [128]), A("apre", [128]), A("coef", [128])
        idi = A("idi", [128], I32)
        wsum = A("wsum", [8])
        thr = A("thr", [16])
        NGB = 4
        Gs = [A("G%d" % i, [D]) for i in range(NGB)]
        junk = A("pjunk", [D], BF16)
        acc = A("acc", [D])
        cw16 = cw[:, :].rearrange("p (c n) -> p c n", c=16)
        cw8 = cw[:, :].rearrange("p (h n) -> p h n", h=8)
        tv4 = tv[:].rearrange("p (h t) k -> p h t k", t=2)
        tif4 = tif[:].rearrange("p (h t) k -> p h t k", t=2)
        iota16 = cst[:, C_IOTA16:C_IOTA16 + 16]
        mod_vector(l, 4, modt[0], "mod0", plus_one=True, mul_row=norm_ffn_w[l:l + 1, :])
        mod_vector(l, 3, modt[1], "mod1")
        mod_vector(l, 5, modt[2], "mod2")
        P.dma("sp", lambda e: e.dma_start(out=skt[:], in_=skT[l].rearrange("c d n -> d c n")), [], ["skt"])
        P.dve(lambda e: e.tensor_scalar(thr[:, 0:15], iota16[:, 1:16], 16.0, None, ALU.mult), ["cst"], ["thr"])
        identf = cm("ident")
        ngrp = cfg.get("peer_groups", S // 256)
        for g in range(ngrp):
            for t in range(2):
                ti_ = g * 2 + t
                r0 = ti_ * 128
                hk = "hf%d" % t
                P.dma("sp", lambda e, t=t, r0=r0: e.dma_start(out=hf[t][:], in_=xres[r0:r0 + 128, :]), ["xres%d" % ti_], [hk])
                rmsnorm_tile(hf[t], hk, modt[0], "mod0", modt[1], "mod1", junk)
                for q4 in range(4):
                    bk, bkk = bank()
                    for i in range(4):
                        k = q4 * 4 + i
                        P.pe(lambda e, bk=bk, i=i, k=k, t=t: e.matmul(bk[:, i * 128:(i + 1) * 128], lhsT=hf[t][:, k * 128:(k + 1) * 128], rhs=identf,
                                                                      start=True, stop=True), [hk, "cst"], [bkk])
                    P.act(lambda e, bk=bk, q4=q4, t=t: e.copy(out=hfT[:, q4 * 4:q4 * 4 + 4, t * 128:(t + 1) * 128], in_=v4(bk)), [bkk], ["hfT"])
            for c in range(16):
                P.dma("sp", lambda e, c=c: e.dma_start(out=wq[:], in_=peer_w_q[l, :, c * 128:(c + 1) * 128].rearrange("(k p) c -> p k c", p=128)),
                      [], ["wq"])
                bk, bkk = bank()
                for k in range(KC):
                    P.pe(lambda e, bk=bk, k=k: e.matmul(bk[:, 0:256], lhsT=wq[:, k, :], rhs=hfT[:, k, :], start=(k == 0), stop=(k == KC - 1)),
                         ["wq", "hfT"], [bkk])
                P.act(lambda e, bk=bk: e.copy(out=qTc[:], in_=bk[:, 0:256]), [bkk], ["qTc"])
                bk2, bkk2 = bank()
                for t in range(2):
                    P.pe(lambda e, bk2=bk2, t=t, c=c: e.matmul(bk2[:, t * 128:(t + 1) * 128], lhsT=qTc[:, t * 128:(t + 1) * 128], rhs=skt[:, c, :],
                                                               start=True, stop=True), ["qTc", "skt"], [bkk2])
                P.act(lambda e, bk2=bk2, c=c: e.copy(out=scs[:, :, c, :], in_=bk2[:, 0:256].rearrange("p (t n) -> p t n", t=2)), [bkk2], ["scs"])
            for t in range(2):
                ti_ = g * 2 + t
                r0 = ti_ * 128
                hk = "hf%d" % t
                for c in range(16):
                    sv = scs[:, t, c, :]
                    P.dve(lambda e, c=c, sv=sv: e.max(out=tv[:, c, 0:8], in_=sv), ["scs"], ["tv"])
                    P.dve(lambda e, c=c, sv=sv: e.max_index(out=ti[:, c, 0:8], in_max=tv[:, c, 0:8], in_values=sv), ["scs", "tv"], ["ti"])
                    P.dve(lambda e, c=c, sv=sv: e.match_replace(out=cw16[:, c, :], in_to_replace=tv[:, c, 0:8], in_values=sv, imm_value=-1e30),
                          ["scs", "tv"], ["cw"])
                    P.dve(lambda e, c=c: e.max(out=tv[:, c, 8:16], in_=cw16[:, c, :]), ["cw"], ["tv"])
                    P.dve(lambda e, c=c: e.max_index(out=ti[:, c, 8:16], in_max=tv[:, c, 8:16], in_values=cw16[:, c, :]), ["cw", "tv"], ["ti"])
                P.dve(lambda e: e.tensor_copy(tif[:], ti[:]), ["ti"], ["tif"])
                P.dve(lambda e: e.tensor_tensor(out=cand[:], in0=tv4[:, :, 0, :].unsqueeze(3).to_broadcast([128, 8, 16, 16]),
                                                in1=tv4[:, :, 1, :].unsqueeze(2).to_broadcast([128, 8, 16, 16]), op=ALU.add), ["tv"], ["cand"])
                for h in range(8):
                    ch = cand[:, h, :, :].rearrange("p a b -> p (a b)")
                    P.dve(lambda e, h=h, ch=ch: e.max(out=bs[:, h, 0:8], in_=ch), ["cand"], ["bs"])
                    P.dve(lambda e, h=h, ch=ch: e.max_index(out=bp[:, h, 0:8], in_max=bs[:, h, 0:8], in_values=ch), ["cand", "bs"], ["bp"])
                    P.dve(lambda e, h=h, ch=ch: e.match_replace(out=cw8[:, h, :], in_to_replace=bs[:, h, 0:8], in_values=ch, imm_value=-1e30),
                          ["cand", "bs"], ["cw"])
                    P.dve(lambda e, h=h: e.max(out=bs[:, h, 8:16], in_=cw8[:, h, :]), ["cw"], ["bs"])
                    P.dve(lambda e, h=h: e.max_index(out=bp[:, h, 8:16], in_max=bs[:, h, 8:16], in_values=cw8[:, h, :]), ["cw", "bs"], ["bp"])
                P.dve(lambda e: e.tensor_copy(bpf[:], bp[:]), ["bp"], ["bpf"])
                w15 = work[:, 0:1920].rearrange("p (h k m) -> p h k m", h=8, k=16)
                P.dve(lambda e: e.tensor_tensor(out=w15, in0=bpf[:].unsqueeze(3).to_broadcast([128, 8, 16, 15]),
                                                in1=thr[:, 0:15].unsqueeze(1).unsqueeze(1).to_broadcast([128, 8, 16, 15]), op=ALU.is_ge),
                      ["bpf", "thr"], ["work"])
                P.dve(lambda e: e.tensor_reduce(out=i1[:], in_=w15, axis=AX.X, op=ALU.add), ["work"], ["i1"])
                P.dve(lambda e: e.scalar_tensor_tensor(out=i2[:].rearrange("p a b -> p (a b)"), in0=i1[:].rearrange("p a b -> p (a b)"), scalar=-16.0,
                                                       in1=bpf[:].rearrange("p a b -> p (a b)"), op0=ALU.mult, op1=ALU.add), ["i1", "bpf"], ["i2"])
                w16 = work[:, 0:2048].rearrange("p (h k m) -> p h k m", h=8, k=16)
                for ii, src_half, dsel in ((i1, 0, sel1), (i2, 1, sel2)):
                    iik = "i1" if src_half == 0 else "i2"
                    dk_ = "sel1" if src_half == 0 else "sel2"
                    P.dve(lambda e, ii=ii: e.tensor_tensor(out=w16, in0=ii[:].unsqueeze(3).to_broadcast([128, 8, 16, 16]),
                                                           in1=iota16.unsqueeze(1).unsqueeze(1).to_broadcast([128, 8, 16, 16]), op=ALU.is_equal),
                          [iik, "cst"], ["work"])
                    P.dve(lambda e, src_half=src_half: e.tensor_tensor(out=w16, in0=w16, in1=tif4[:, :, src_half, :].unsqueeze(2).to_broadcast([128, 8, 16, 16]),
                                                                       op=ALU.mult), ["work", "tif"], ["work"])
                    P.dve(lambda e, dsel=dsel: e.tensor_reduce(out=dsel[:], in_=w16, axis=AX.X, op=ALU.add), ["work"], [dk_])
                P.dve(lambda e: e.scalar_tensor_tensor(out=idf[:], in0=sel1[:].rearrange("p a b -> p (a b)"), scalar=128.0,
                                                       in1=sel2[:].rearrange("p a b -> p (a b)"), op0=ALU.mult, op1=ALU.add), ["sel1", "sel2"], ["idf"])
                if l > 0:
                    P.dve(lambda e: e.tensor_scalar(idf[:], idf[:], float(l * NEXP), None, ALU.add), ["idf"], ["idf"])
                P.dve(lambda e: e.tensor_copy(idi[:], idf[:]), ["idf"], ["idi"])
                P.dve(lambda e: e.tensor_tensor(out=wsm[:], in0=bs[:], in1=bs[:, :, 0:1].to_broadcast([128, 8, 16]), op=ALU.subtract), ["bs"], ["wsm"])
                P.act(lambda e: e.activation(out=wsm[:], in_=wsm[:], func=AF.Exp), ["wsm"], ["wsm"])
                P.dve(lambda e: e.tensor_reduce(out=wsum[:], in_=wsm[:], axis=AX.X, op=ALU.add), ["wsm"], ["wsum"])
                P.dve(lambda e: e.reciprocal(out=wsum[:], in_=wsum[:]), ["wsum"], ["wsum"])
                P.dve(lambda e: e.tensor_tensor(out=wsm[:], in0=wsm[:], in1=wsum[:].unsqueeze(2).to_broadcast([128, 8, 16]), op=ALU.mult),
                      ["wsm", "wsum"], ["wsm"])
                if cfg.get("peer_dump") and g == 0 and t == 0:
                    for nm, t_, shp, dt_ in (("idi", idi, [128, 128], I32), ("wsm", wsm, [128, 8, 16], F32), ("bs", bs, [128, 8, 16], F32),
                                             ("tv", tv, [128, 16, 16], F32), ("tif", tif, [128, 16, 16], F32)):
                        o = dout("dump_" + nm, shp, dt_)
                        P.dma("sp", lambda e, o=o, t_=t_: e.dma_start(out=o, in_=t_[:]), ["idi", "wsm", "bs", "tv", "tif"], [])
                nslot = cfg.get("peer_slots", 128)
                for s_ in range(nslot):
                    Gb, Gk = Gs[s_ % NGB], "G%d" % (s_ % NGB)
                    P.dma("pool", lambda e, Gb=Gb, s_=s_: e.indirect_dma_start(out=Gb[:], out_offset=None, in_=peer_u_flat,
                                                                               in_offset=bass.IndirectOffsetOnAxis(ap=idi[:, s_:s_ + 1], axis=0)),
                          ["idi"], [Gk])
                    P.dve(lambda e, Gb=Gb, s_=s_, t=t: e.scalar_tensor_tensor(out=junk[:], in0=Gb[:], scalar=1.0, in1=hf[t][:], op0=ALU.mult, op1=ALU.mult,
                                                                              accum_out=apre[:, s_:s_ + 1]), [Gk, hk], ["pjunk", "apre"])
                P.act(lambda e: e.activation(out=coef[:, 0:nslot], in_=apre[:, 0:nslot], func=AF.Gelu), ["apre"], ["coef"])
                P.dve(lambda e: e.tensor_tensor(out=coef[:, 0:nslot], in0=coef[:, 0:nslot], in1=wsm[:].rearrange("p a b -> p (a b)")[:, 0:nslot], op=ALU.mult),
                      ["coef", "wsm"], ["coef"])
                for s_ in range(nslot):
                    Gb, Gk = Gs[s_ % NGB], "G%d" % (s_ % NGB)
                    P.dma("pool", lambda e, Gb=Gb, s_=s_: e.indirect_dma_start(out=Gb[:], out_offset=None, in_=peer_v_flat,
                                                                               in_offset=bass.IndirectOffsetOnAxis(ap=idi[:, s_:s_ + 1], axis=0)),
                          ["idi"], [Gk])
                    if s_ == 0:
                        P.dve(lambda e, Gb=Gb: e.tensor_scalar(acc[:], Gb[:], coef[:, 0:1], None, ALU.mult), [Gk, "coef"], ["acc"])
                    else:
                        P.dve(lambda e, Gb=Gb, s_=s_: e.scalar_tensor_tensor(out=acc[:], in0=Gb[:], scalar=coef[:, s_:s_ + 1], in1=acc[:],
                                                                             op0=ALU.mult, op1=ALU.add), [Gk, "coef", "acc"], ["acc"])
                xt = hf[t]
                P.dma("sp", lambda e, xt=xt, r0=r0: e.dma_start(out=xt[:], in_=xres[r0:r0 + 128, :]), ["xres%d" % ti_], [hk])
                P.dve(lambda e: e.tensor_mul(acc[:], acc[:], modt[2][:]), ["acc", "mod2"], ["acc"])
                P.dve(lambda e, xt=xt: e.tensor_add(xt[:], xt[:], acc[:]), ["acc", hk], [hk])
                if last:
                    fnw = Gs[0]
                    P.dma("sp", lambda e: e.dma_start(out=fnw[:], in_=final_norm_w[0:1, :].to_broadcast([128, D])), [], ["G0"])
                    rmsnorm_tile(xt, hk, fnw, "G0", None, None, junk)
                    P.dma("sp", lambda e, xt=xt, r0=r0: e.dma_start(out=y_out[r0:r0 + 128, :], in_=xt[:]), [hk], ["y_out"])
                else:
                    P.dma("sp", lambda e, xt=xt, r0=r0: e.dma_start(out=xres[r0:r0 + 128, :], in_=xt[:]), [hk], ["xres%d" % ti_])

    dbg_outs = {}
    if phases in ("p1",):
        l = 0
        prepass(l)
        phase1(l, x_in, "x_in")
        for nm, src, shape, dt in (("d_Hs", Hs, [KC, 128, S], BF16), ("d_Yp", Yp, [8, 128, S], BF16),
                                   ("d_DNq", DNq, [8, 128, S], F32), ("d_DNk", DNk, [8, 128, S], F32), ("d_DNv", DNv, [8, 128, S], F32),
                                   ("d_Qc", Qc, [8, 128, S], BF16), ("d_Kc", Kc, [8, 128, S], BF16)):
            o = dout(nm, shape, dt)
            for a in range(shape[0]):
                P.dma("sp", lambda e, o=o, src=src, a=a: e.dma_start(out=o[a], in_=src[a]),
                      ["Hs", "Yp", "DN0", "DN1", "DN2", "QK0", "QK1"], [])
        o = dout("d_Gdn", [S, 1024], BF16)
        P.dma("sp", lambda e, o=o: e.dma_start(out=o[:, :], in_=Gdn[:, :]), ["Gdn"], [])
        o = dout("d_Vc", [128, 32, 1024], BF16)
        P.dma("sp", lambda e, o=o: e.dma_start(out=o[:, :, :], in_=Vc[:, :, :]), ["Vc"], [])
        o = dout("d_sg", [128, 32, 24], F32)
        P.dma("sp", lambda e, o=o: e.dma_start(out=o[:, :, :], in_=sg[:]), ["sg"], [])

    if phases == "dn":
        phase_dn(0)
        o = dout("d_Yd", [8, 128, S], BF16)
        for a in range(cfg.get("dn_heads", 8) if cfg.get("dn_stage", 9) >= 3 else 0):
            P.dma("sp", lambda e, o=o, a=a: e.dma_start(out=o[a], in_=Yd[a]), ["Yd"], [])

    if phases == "fox":
        phase_fox(0)
        o = dout("d_Yf", [8, 128, S], BF16)
        for a in range(cfg.get("fox_heads", 8)):
            P.dma("sp", lambda e, o=o, a=a: e.dma_start(out=o[a], in_=Yf[a]), ["Yf"], [])

    if phases == "merge":
        if "winb" not in feed:
            prepass(0)
        phase_merge(0, x_in, "x_in")
        o = dout("d_xres", [S, D])
        ng = cfg.get("mg_groups", NG)
        P.dma("sp", lambda e, o=o: e.dma_start(out=o[0:ng * TG, :], in_=xres[0:ng * TG, :]), ["xres%d" % i for i in range(ng * 4)], [])

    if phases == "peer":
        phase_peer(0, cfg.get("peer_last", False))
        if not cfg.get("peer_last", False):
            o = dout("d_xres", [S, D])
            ng = cfg.get("peer_groups", S // 256)
            P.dma("sp", lambda e, o=o: e.dma_start(out=o[0:ng * 256, :], in_=xres[0:ng * 256, :]), ["xres%d" % i for i in range(ng * 2)], [])

    if phases == "all":
        for l in layers:
            xsrc, xkey = (x_in, "x_in") if l == 0 else (xres, "xres")
            prepass(l)
            phase1(l, xsrc, xkey)
            phase_dn(l)
            phase_fox(l)
            phase_merge(l, xsrc, xkey)
            phase_peer(l, l == NL - 1)

    P.limit = None
    P.finish()
    print("ops", P.n_ops, "sbuf bytes/partition", P.sb_bytes)
    P.emit()
    return nc


def prep_shared(inputs):
    f = lambda a: np.ascontiguousarray(np.asarray(a, dtype=np.float32))
    m = {}
    for k in ("ada_w", "ada_b", "norm_mix_w", "norm_ffn_w", "w_in", "pool_w", "w_branch_pool", "w_branch_dn",
              "w_branch_fox", "w_out", "peer_w_q", "peer_u", "peer_v"):
        m[k] = f(inputs[k])
    m["final_norm_w"] = f(inputs["final_norm_w"]).reshape(1, D)
    m["pool_scale_t"] = f(np.asarray(inputs["pool_scale"]).reshape(NL, 8, 128).transpose(0, 2, 1))
    cw = np.asarray(inputs["dn_conv_w"]).reshape(NL, 4, 24, 128)
    m["conv_t"] = f(cw.transpose(0, 3, 2, 1).reshape(NL, 128, 96))
    bc = lambda a: f(np.broadcast_to(np.asarray(a)[:, None, :], (NL, 128, np.asarray(a).shape[-1])))
    m["a_log_bc"] = bc(inputs["dn_a_log"])
    m["dt_bias_bc"] = bc(inputs["dn_dt_bias"])
    m["f_bias_bc"] = bc(inputs["fox_f_bias"])
    m["onorm_bc"] = bc(inputs["dn_onorm_w"])
    sk = np.asarray(inputs["peer_sub_keys"])
    m["skT"] = f(sk.reshape(NL, 16, 128, 128).transpose(0, 1, 3, 2))
    m["consts"] = make_consts()
    return m


def prep_core(inputs, shared, b):
    m = dict(shared)
    m["x"] = np.ascontiguousarray(np.asarray(inputs["x"][b], dtype=np.float32))
    m["cT"] = np.ascontiguousarray(np.asarray(inputs["c"][b], dtype=np.float32).reshape(KC, 128).T)
    return m


_NC_CACHE = {}


def kernel(**inputs):
    if "nc" not in _NC_CACHE:
        _NC_CACHE["nc"] = build({"phases": "all"})
    nc = _NC_CACHE["nc"]
    shared = prep_shared(inputs)
    in_maps = [prep_core(inputs, shared, i % 4) for i in range(8)]
    res = run_bass_kernel_spmd(nc, in_maps, core_ids=list(range(8)))
    out = np.stack([np.asarray(res.results[i]["y"], dtype=np.float32) for i in range(4)], axis=0)
    return out
```

```python
import numpy as np
from contextlib import ExitStack
import concourse.bass as bass
import concourse.mybir as mybir
from concourse.bass_utils import run_bass_kernel_spmd

F32 = mybir.dt.float32
BF16 = mybir.dt.bfloat16
I32 = mybir.dt.int32
U32 = mybir.dt.uint32
ALU = mybir.AluOpType
AF = mybir.ActivationFunctionType
AX = mybir.AxisListType

ENGS = ("pe", "act", "dve", "pool", "sp")


class Prog:
    def __init__(self, nc, dma_sems=None):
        self.nc = nc
        self.streams = {e: [] for e in ENGS}
        self.count = {}
        self.waited = {e: {} for e in ENGS}
        self.res_w = {}
        self.res_r = {}
        self.dma_sems = dma_sems or {"sp": 8, "act": 6, "pool": 12}
        self.dma_rr = {q: 0 for q in self.dma_sems}
        self.es = ExitStack()
        self.n_ops = 0
        self.sb_bytes = 0
        self.nbank = 0
        self.limit = None

    def sb(self, name, shape, dt):
        n = 1
        for s in shape[1:]:
            n *= s
        self.sb_bytes += n * (2 if dt == BF16 else 4)
        return self.es.enter_context(self.nc.sbuf_tensor(name, list(shape), dt))

    def ps(self, name, shape, dt):
        return self.es.enter_context(self.nc.psum_tensor(name, list(shape), dt))

    def _deps(self, reads, writes):
        deps = {}

        def add(k, v):
            if deps.get(k, 0) < v:
                deps[k] = v
        for r in reads:
            if r in self.res_w:
                add(*self.res_w[r])
        for w in writes:
            if w in self.res_w:
                add(*self.res_w[w])
            for k, v in self.res_r.get(w, {}).items():
                add(k, v)
        return deps

    def _emit_waits(self, eng, deps):
        for k, v in deps.items():
            if k == eng and eng == "pe":
                continue
            if self.waited[eng].get(k, 0) >= v:
                continue
            self.streams[eng].append(("wait", k, v))
            self.waited[eng][k] = v

    def _mark(self, key, val, reads, writes):
        for r in reads:
            d = self.res_r.setdefault(r, {})
            if d.get(key, 0) < val:
                d[key] = val
        for w in writes:
            self.res_w[w] = (key, val)
            self.res_r[w] = {}

    def op(self, eng, fn, reads=(), writes=()):
        if self.limit is not None and self.n_ops >= self.limit:
            return
        self._emit_waits(eng, self._deps(reads, writes))
        v = self.count.get(eng, 0) + 1
        self.count[eng] = v
        self.streams[eng].append(("op", fn, eng, 1))
        self._mark(eng, v, reads, writes)
        self.n_ops += 1

    def pe(self, fn, r=(), w=()):
        self.op("pe", fn, r, w)

    def act(self, fn, r=(), w=()):
        self.op("act", fn, r, w)

    def dve(self, fn, r=(), w=()):
        self.op("dve", fn, r, w)

    def pool(self, fn, r=(), w=()):
        self.op("pool", fn, r, w)

    def dma(self, q, fn, reads=(), writes=()):
        if self.limit is not None and self.n_ops >= self.limit:
            return
        deps = self._deps(reads, writes)
        k = self.dma_rr[q] % self.dma_sems[q]
        self.dma_rr[q] += 1
        key = ("dma", q, k)
        prev = self.count.get(key, 0)
        if prev:
            deps[key] = max(deps.get(key, 0), prev)
        self._emit_waits(q, deps)
        v = prev + 16
        self.count[key] = v
        self.streams[q].append(("op", fn, key, 16))
        self._mark(key, v, reads, writes)
        self.n_ops += 1

    def barrier(self):
        for e in ENGS:
            for k, v in self.count.items():
                if k == e and e == "pe":
                    continue
                if self.waited[e].get(k, 0) < v:
                    self.streams[e].append(("wait", k, v))
                    self.waited[e][k] = v

    def finish(self):
        for k, v in self.count.items():
            if self.waited["sp"].get(k, 0) < v:
                self.streams["sp"].append(("wait", k, v))
                self.waited["sp"][k] = v

    def emit(self):
        nc = self.nc
        sems = {}
        for k in self.count:
            nm = k if isinstance(k, str) else "d_%s_%d" % (k[1], k[2])
            sems[k] = self.es.enter_context(nc.semaphore("s_" + nm))
        block = self.es.enter_context(nc.Block())

        def run(ename):
            def f(e):
                for it in self.streams[ename]:
                    if it[0] == "wait":
                        e.wait_ge(sems[it[1]], it[2])
                    else:
                        it[1](e).then_inc(sems[it[2]], it[3])
            return f
        block.tensor(run("pe"))
        block.scalar(run("act"))
        block.vector(run("dve"))
        block.gpsimd(run("pool"))
        block.sync(run("sp"))
        self.es.close()


class Arena:
    def __init__(self, t, nwords):
        self.t, self.n, self.off, self.gen = t, nwords, 0, 0

    def reset(self):
        self.off = 0
        self.gen += 1

    def alloc(self, name, free_shape, dt):
        n = 1
        for d_ in free_shape:
            n *= d_
        words = n if dt in (F32, I32, U32) else (n + 1) // 2
        words = (words + 3) // 4 * 4
        assert self.off + words <= self.n, ("arena overflow", name, self.off, words, self.n)
        ap = self.t[:, self.off:self.off + words]
        self.off += words
        if dt != F32:
            ap = ap.bitcast(dt)
        ap = ap[:, 0:n]
        if len(free_shape) > 1:
            names = ["a%d" % i for i in range(len(free_shape))]
            pat = "p (%s) -> p %s" % (" ".join(names), " ".join(names))
            ap = ap.rearrange(pat, **{nm: d_ for nm, d_ in zip(names[1:], free_shape[1:])})
        return ap


S = 4096
D = 2048
TG = 512
NG = S // TG
KC = 16
NL = 2
EPS = 1e-6
NW = 14360
NEXP = 16384
OFF_P, OFF_D, OFF_GD, OFF_F, OFF_SBA, OFF_V, OFF_G, OFF_SF = 0, 1024, 4096, 5120, 7168, 7184, 8208, 14352
HALF_A = 7184

C_NAMES = ["ident", "ones", "tri128", "triblk", "strictU", "blkones", "selA", "selB", "selrow0"]
C_OFF = {n: i * 128 for i, n in enumerate(C_NAMES)}
C_MASKA = len(C_NAMES) * 128
C_MASKB = C_MASKA + 1
C_IOTA16 = C_MASKB + 1
C_INVCNT = C_IOTA16 + 16
C_TOTAL = C_INVCNT + 64


def make_consts():
    c = np.zeros((128, C_TOTAL), np.float32)
    m = np.arange(128)[:, None]
    i = np.arange(128)[None, :]
    same = (m // 64) == (i // 64)
    mats = {
        "ident": (m == i), "ones": np.ones((128, 128), bool), "tri128": (m <= i),
        "triblk": (m <= i) & same, "strictU": (m > i) & same, "blkones": same,
        "selA": (m < 64) & (i >= 0), "selB": (m >= 64) & (i >= 0), "selrow0": (m == 0) & (i >= 0),
    }
    for n in C_NAMES:
        c[:, C_OFF[n]:C_OFF[n] + 128] = mats[n].astype(np.float32)
    c[:, C_MASKA] = (np.arange(128) < 64)
    c[:, C_MASKB] = (np.arange(128) >= 64)
    c[:, C_IOTA16:C_IOTA16 + 16] = np.arange(16)[None, :]
    for gi, w in enumerate((2, 4, 8, 16)):
        c[:, C_INVCNT + gi * 16:C_INVCNT + gi * 16 + 16] = 1.0 / np.minimum(np.arange(16) + 1, w)[None, :]
    return c


def build(cfg):
    nc = bass.Bass("TRN2", target_bir_lowering=False)
    P = Prog(nc)
    P.limit = cfg.get("max_ops")
    dbg = cfg.get("debug", {})
    phases = cfg.get("phases", "all")
    layers = cfg.get("layers", [0, 1])

    shrink = cfg.get("shrink", ())

    def din(name, shape, dt=F32):
        if name in shrink:
            return None
        return nc.dram_tensor(name, list(shape), dt, kind="ExternalInput").ap()

    feed = cfg.get("feed", ())

    def dscr(name, shape, dt):
        kind = "ExternalInput" if name in feed else "Internal"
        return nc.dram_tensor(name, list(shape), dt, kind=kind).ap()

    def dout(name, shape, dt=F32):
        return nc.dram_tensor(name, list(shape), dt, kind="ExternalOutput").ap()

    x_in = din("x", [S, D])
    cT_in = din("cT", [128, KC])
    ada_w = din("ada_w", [NL, D, 6 * D])
    ada_b = din("ada_b", [NL, 6 * D])
    norm_mix_w = din("norm_mix_w", [NL, D])
    norm_ffn_w = din("norm_ffn_w", [NL, D])
    final_norm_w = din("final_norm_w", [1, D])
    w_in = din("w_in", [NL, D, NW])
    pool_w = din("pool_w", [NL, 4, 256, 256])
    pool_scale_t = din("pool_scale_t", [NL, 128, 8])
    conv_t = din("conv_t", [NL, 128, 24 * 4])
    a_log_bc = din("a_log_bc", [NL, 128, 8])
    dt_bias_bc = din("dt_bias_bc", [NL, 128, 8])
    f_bias_bc = din("f_bias_bc", [NL, 128, 8])
    onorm_bc = din("onorm_bc", [NL, 128, 128])
    w_br = [din("w_branch_pool", [NL, 1024, D]), din("w_branch_dn", [NL, 1024, D]), din("w_branch_fox", [NL, 1024, D])]
    if cT_in is None:
        cT_in = din("cT_dummy", [128, KC])
    w_out = din("w_out", [NL, D, D])
    peer_w_q = din("peer_w_q", [NL, D, D])
    skT = din("skT", [NL, 16, 128, 128])
    peer_u = din("peer_u", [NL, NEXP, D])
    peer_v = din("peer_v", [NL, NEXP, D])
    consts_in = din("consts", [128, C_TOTAL])
    rowidx_in = din("rowidx", [128, S // 128], I32)
    SPLIT = cfg.get("split_last", True)
    y_out = dout("y", [S // 2 if SPLIT else S, D])

    winb = dscr("winb", [D, NW], BF16)
    wbrb = dscr("wbrb", [3, 1024, D], BF16)
    woutb = dscr("woutb", [D, D], BF16)
    xres = dscr("xres", [S, D], F32)
    for i_ in range(S // 128):
        if "xres" in feed:
            P.res_w.pop("xres%d" % i_, None)
    Hs = dscr("Hs", [KC, 128, S], BF16)
    Yp = dscr("Yp", [8, 128, S], BF16)
    Yd = dscr("Yd", [8, 128, S], BF16)
    Yf = dscr("Yf", [8, 128, S], BF16)
    DNq = dscr("DNq", [8, 128, S], F32)
    DNk = dscr("DNk", [8, 128, S], F32)
    DNv = dscr("DNv", [8, 128, S], F32)
    Gdn = dscr("Gdn", [S, 1024], BF16)
    Qc = dscr("Qc", [8, 128, S], BF16)
    Kc = dscr("Kc", [8, 128, S], BF16)
    Vc = dscr("Vc", [128, S // 128, 1024], BF16)

    NBANK = cfg.get("nbank", 8)
    banks = [P.ps("bank%d" % i, [128, 512], F32) for i in range(8)]

    P.bank_lo, P.bank_n = 0, NBANK

    def bank():
        i = P.bank_lo + P.nbank % P.bank_n
        P.nbank += 1
        return banks[i], "bank%d" % i

    cst = P.sb("cst", [128, C_TOTAL], F32)
    P.dma("sp", lambda e: e.dma_start(out=cst[:], in_=consts_in[:, :]), [], ["cst"])

    def cm(name):
        return cst[:, C_OFF[name]:C_OFF[name] + 128]
    ridx = P.sb("ridx", [128, S // 128], I32)
    if rowidx_in is not None:
        P.dma("sp", lambda e: e.dma_start(out=ridx[:], in_=rowidx_in[:, :]), [], ["ridx"])
    ident_bf = P.sb("ident_bf", [128, 128], BF16)
    ones_bf = P.sb("ones_bf", [128, 128], BF16)
    P.dve(lambda e: e.tensor_copy(ident_bf[:], cm("ident")), ["cst"], ["ident_bf"])
    P.dve(lambda e: e.tensor_copy(ones_bf[:], cm("ones")), ["cst"], ["ones_bf"])

    ct = P.sb("ct", [128, KC], F32)
    csbc = P.sb("csbc", [128, KC, 128], F32)
    P.dma("sp", lambda e: e.dma_start(out=ct[:], in_=cT_in[:, :]), [], ["ct"])
    P.act(lambda e: e.activation(out=ct[:], in_=ct[:], func=AF.Silu), ["ct"], ["ct"])
    P.dve(lambda e: e.tensor_copy(csbc[:], ct[:].unsqueeze(2).to_broadcast([128, KC, 128])), ["ct"], ["csbc"])

    stg = P.sb("stg", [128, 4096], F32)
    modt = [P.sb("mod%d" % i, [128, D], F32) for i in range(3)]
    stat = P.sb("stat", [128, 8], F32)
    sm = P.sb("sm", [128, 128], F32)
    sg = P.sb("sg", [128, S // 128, 24], F32)
    if "sg_in" in feed:
        sg_in = din("sg_in", [128, S // 128, 24])
        P.dma("sp", lambda e: e.dma_start(out=sg[:], in_=sg_in[:, :, :]), [], ["sg"])
    ARW = cfg.get("arw", 36864)
    arena_t = P.sb("arena", [128, ARW], F32)
    AR = Arena(arena_t, ARW)

    def new_phase():
        P.barrier()
        AR.reset()
        P.bank_lo, P.bank_n = 0, NBANK

    def mod_vector(l, j, dst, dst_key, plus_one=False, mul_row=None):
        stv = stg[:, 0:KC * 256].rearrange("p (k c) -> p k c", k=KC)
        for cb in range(8):
            c0 = j * D + cb * 256
            P.dma("sp", lambda e, c0=c0: e.dma_start(
                out=stv, in_=ada_w[l, :, c0:c0 + 256].rearrange("(k p) c -> p k c", p=128)), [], ["stg"])
            bk, bkk = bank()
            for k in range(KC):
                P.pe(lambda e, bk=bk, k=k: e.matmul(bk[:, 0:256], lhsT=csbc[:, k, :], rhs=stv[:, k, :],
                                                    start=(k == 0), stop=(k == KC - 1)), ["csbc", "stg"], [bkk])
            P.act(lambda e, bk=bk, cb=cb: e.copy(out=dst[:, cb * 256:(cb + 1) * 256], in_=bk[:, 0:256]), [bkk], [dst_key])
        P.dma("sp", lambda e: e.dma_start(out=stg[:, 0:D], in_=ada_b[l:l + 1, j * D:(j + 1) * D].to_broadcast([128, D])),
              [], ["stg"])
        P.dve(lambda e: e.tensor_add(dst[:], dst[:], stg[:, 0:D]), ["stg", dst_key], [dst_key])
        if plus_one:
            P.dma("sp", lambda e: e.dma_start(out=stg[:, D:2 * D], in_=mul_row.to_broadcast([128, D])), [], ["stg"])
            P.dve(lambda e: e.scalar_tensor_tensor(out=dst[:], in0=dst[:], scalar=1.0, in1=stg[:, D:2 * D],
                                                   op0=ALU.add, op1=ALU.mult), ["stg", dst_key], [dst_key])

    def cast_rows(src2d, dst2d, ncols, segs, key_dst, pst, pstb):
        rows = src2d.shape[0]
        for r0 in range(0, rows, 128):
            P.dma("sp", lambda e, r0=r0: e.dma_start(out=pst[:, 0:ncols], in_=src2d[r0:r0 + 128, :]), [], ["pst"])
            for si, (ov, iv) in enumerate(segs):
                eng = ("act", "dve", "pool")[si % 3]
                if eng == "act":
                    P.act(lambda e, ov=ov, iv=iv: e.copy(out=ov(pstb), in_=iv(pst)), ["pst"], ["pstb"])
                elif eng == "dve":
                    P.dve(lambda e, ov=ov, iv=iv: e.tensor_copy(ov(pstb), iv(pst)), ["pst"], ["pstb"])
                else:
                    P.pool(lambda e, ov=ov, iv=iv: e.tensor_copy(ov(pstb), iv(pst)), ["pst"], ["pstb"])
            P.dma("sp", lambda e, r0=r0: e.dma_start(out=dst2d[r0:r0 + 128, :], in_=pstb[:, 0:ncols]), ["pstb"], [key_dst])

    def prepass(l):
        new_phase()
        pst = AR.alloc("pst", [HALF_A], F32)
        pstb = AR.alloc("pstb", [HALF_A], BF16)
        segsA = [
            (lambda o: o[:, 0:1024], lambda i: i[:, 0:1024]),
            (lambda o: o[:, OFF_D:OFF_D + 3072].rearrange("p (h t c) -> p h t c", h=8, t=3),
             lambda i: i[:, 1024:4096].rearrange("p (t h c) -> p h t c", t=3, h=8)),
            (lambda o: o[:, OFF_GD:OFF_GD + 1024], lambda i: i[:, 4112:5136]),
            (lambda o: o[:, OFF_F:OFF_F + 2048].rearrange("p (h t c) -> p h t c", h=8, t=2),
             lambda i: i[:, 5136:7184].rearrange("p (t h c) -> p h t c", t=2, h=8)),
            (lambda o: o[:, OFF_SBA:OFF_SBA + 16], lambda i: i[:, 4096:4112]),
        ]
        cast_rows(w_in[l, :, 0:HALF_A], winb[:, 0:HALF_A], HALF_A, segsA, "winb", pst, pstb)
        nB = NW - HALF_A
        segsB = [
            (lambda o: o[:, 0:1024], lambda i: i[:, 0:1024]),
            (lambda o: o[:, 1024:1024 + 6144].rearrange("p (j b c) -> p j b c", j=16, b=3),
             lambda i: i[:, 1032:1032 + 6144].rearrange("p (b j c) -> p j b c", b=3, j=16)),
            (lambda o: o[:, 7168:7176], lambda i: i[:, 1024:1032]),
        ]
        cast_rows(w_in[l, :, HALF_A:NW], winb[:, HALF_A:NW], nB, segsB, "winb", pst, pstb)
        plain = [(lambda o: o[:, 0:1024], lambda i: i[:, 0:1024]), (lambda o: o[:, 1024:2048], lambda i: i[:, 1024:2048])]
        for b in range(3):
            cast_rows(w_br[b][l], wbrb[b], 2048, plain, "wbrb", pst, pstb)
        cast_rows(w_out[l], woutb, 2048, plain, "woutb", pst, pstb)

    def load_wt(wts, src2d, c0, n, srckey, pref="wt"):
        i = P.wt_rr % len(wts)
        P.wt_rr += 1
        t, k = wts[i], "%s%d" % (pref, i)
        P.dma("sp", lambda e: e.dma_start(out=t[:, :, 0:n], in_=src2d[:, c0:c0 + n].rearrange("(k p) c -> p k c", p=128)),
              [srckey], [k])
        return t, k
    P.wt_rr = 0

    def rmsnorm_tile(xt, xtk, gain, gaink, shift, shiftk, junk):
        P.act(lambda e: e.activation(out=junk[:], in_=xt[:], func=AF.Square, accum_out=stat[:, 0:1]), [xtk], ["hb", "pjunk", "stat"])
        P.act(lambda e: e.activation(out=stat[:, 1:2], in_=stat[:, 0:1], func=AF.Sqrt, scale=1.0 / D, bias=EPS), ["stat"], ["stat"])
        P.dve(lambda e: e.reciprocal(out=stat[:, 2:3], in_=stat[:, 1:2]), ["stat"], ["stat"])
        P.dve(lambda e: e.scalar_tensor_tensor(out=xt[:], in0=xt[:], scalar=stat[:, 2:3], in1=gain[:],
                                               op0=ALU.mult, op1=ALU.mult), [xtk, "stat", gaink], [xtk])
        if shift is not None:
            P.dve(lambda e: e.tensor_add(xt[:], xt[:], shift[:]), [xtk, shiftk], [xtk])

    HT = {}

    def proj_fm(wt, wtk, c0, n=TG):
        hT = HT["hT"]
        bk, bkk = bank()
        for k in range(KC):
            P.pe(lambda e, k=k: e.matmul(bk[:, 0:n], lhsT=wt[:, k, c0:c0 + 128], rhs=hT[:, k, 0:n],
                                         start=(k == 0), stop=(k == KC - 1)), [wtk, "hT"], [bkk])
        return bk, bkk

    def proj_tm(wt, wtk, t, c0, n, bk=None, bkk=None, o0=0):
        hT = HT["hT"]
        if bk is None:
            bk, bkk = bank()
        for k in range(KC):
            P.pe(lambda e, k=k: e.matmul(bk[:, o0:o0 + n], lhsT=hT[:, k, t * 128:(t + 1) * 128], rhs=wt[:, k, c0:c0 + n],
                                         start=(k == 0), stop=(k == KC - 1)), [wtk, "hT"], [bkk])
        return bk, bkk

    def phase1(l, xsrc, xsrc_key):
        new_phase()
        hT = AR.alloc("hT", [KC, TG], BF16)
        HT["hT"] = hT
        wts = [AR.alloc("wt%d" % i, [KC, 512], BF16) for i in range(2)]
        xts = [AR.alloc("xt%d" % i, [D], F32) for i in range(2)]
        hb = AR.alloc("hb", [D], BF16)
        pin = AR.alloc("pin", [8, 15 + TG], F32)
        pacc = [AR.alloc("pacc%d" % i, [15 + TG], F32) for i in range(2)]
        pooled = AR.alloc("pooled", [8, TG], BF16)
        ypT = AR.alloc("ypT", [8, TG], BF16)
        pwb = AR.alloc("pwb", [8, 256], BF16)
        psc = AR.alloc("psc", [8], F32)
        cvw = AR.alloc("cvw", [24 * 4], F32)
        cin = AR.alloc("cin", [3, 3 + TG], F32)
        ctail = AR.alloc("ctail", [24, 3], F32)
        cacc = AR.alloc("cacc", [3, TG], F32)
        csq = AR.alloc("csq", [TG], F32)
        crn = AR.alloc("crn", [TG], F32)
        tm_bf = AR.alloc("tm_bf", [4, 1024], BF16)
        fqk = AR.alloc("fqk", [2, TG], BF16)
        junk = sm
        P.dma("sp", lambda e: e.dma_start(out=stg[:, 0:2048].rearrange("p (a c) -> p a c", a=8),
                                          in_=pool_w[l].rearrange("g (cc p) d -> p (g cc) d", p=128)), [], ["stg"])
        P.dve(lambda e: e.tensor_copy(pwb[:], stg[:, 0:2048].rearrange("p (a c) -> p a c", a=8)), ["stg"], ["pwb"])
        P.dma("sp", lambda e: e.dma_start(out=psc[:], in_=pool_scale_t[l]), [], ["psc"])
        P.dma("sp", lambda e: e.dma_start(out=cvw[:], in_=conv_t[l]), [], ["cvw"])
        P.pool(lambda e: e.memset(pin[:], 0.0), [], ["pin"])
        P.pool(lambda e: e.memset(ctail[:], 0.0), [], ["ctail"])
        mod_vector(l, 1, modt[0], "mod0", plus_one=True, mul_row=norm_mix_w[l:l + 1, :])
        mod_vector(l, 0, modt[1], "mod1")
        for g in range(NG):
            for t in range(4):
                xt, xtk = xts[t % 2], "xt%d" % (t % 2)
                r0 = g * TG + t * 128
                P.dma("sp", lambda e, xt=xt, r0=r0: e.dma_start(out=xt[:], in_=xsrc[r0:r0 + 128, :]), [xsrc_key + str(g * 4 + t)], [xtk])
                rmsnorm_tile(xt, xtk, modt[0], "mod0", modt[1], "mod1", hb)
                P.pool(lambda e, xt=xt: e.tensor_copy(hb[:], xt[:]), [xtk], ["hb"])
                for half in range(2):
                    bk, bkk = bank()
                    bkb = bk[:, :].bitcast(BF16)
                    for kk in range(8):
                        k = half * 8 + kk
                        P.pe(lambda e, bkb=bkb, kk=kk, k=k: e.transpose(out=bkb[:, kk * 128:(kk + 1) * 128],
                                                                        in_=hb[:, k * 128:(k + 1) * 128], identity=ident_bf[:]),
                             ["hb", "ident_bf"], [bkk])
                    P.act(lambda e, bkb=bkb, half=half, t=t: e.copy(
                        out=hT[:, half * 8:half * 8 + 8, t * 128:(t + 1) * 128],
                        in_=bkb[:, 0:1024].rearrange("p (k c) -> p k c", k=8)), [bkk], ["hT"])
            P.dma("sp", lambda e, g=g: e.dma_start(out=Hs[:, :, g * TG:(g + 1) * TG].rearrange("k p t -> p k t"), in_=hT[:]),
                  ["hT"], ["Hs"])
            for wi in range(2):
                wt, wtk = load_wt(wts, winb, OFF_P + wi * 512, 512, "winb")
                for cc in range(4):
                    ch = wi * 4 + cc
                    bk, bkk = proj_fm(wt, wtk, cc * 128)
                    P.act(lambda e, bk=bk, ch=ch: e.copy(out=pin[:, ch, 15:15 + TG], in_=bk[:, :]), [bkk], ["pin"])
            for ch in range(8):
                gi = ch // 2
                w = (2, 4, 8, 16)[gi]
                src = pin[:, ch, :]
                srck = "pin"
                sh = 1
                for lvl in range(gi + 1):
                    dst, dstk = pacc[lvl % 2], "pacc%d" % (lvl % 2)
                    n = 15 + TG - sh
                    P.pool(lambda e, dst=dst, src=src, sh=sh, n=n: e.tensor_tensor(
                        out=dst[:, sh:sh + n], in0=src[:, sh:sh + n], in1=src[:, 0:n], op=ALU.add), [srck], [dstk])
                    src, srck = dst[:, :], dstk
                    sh *= 2
                P.dve(lambda e, src=src, ch=ch, w=w: e.scalar_tensor_tensor(
                    out=pooled[:, ch, :], in0=src[:, 15:15 + TG], scalar=1.0 / w, in1=pin[:, ch, 15:15 + TG],
                    op0=ALU.mult, op1=ALU.subtract), [srck, "pin"], ["pooled"])
                if g == 0:
                    P.dve(lambda e, src=src, gi=gi: e.tensor_tensor(
                        out=junk[:, 0:16], in0=src[:, 15:31], in1=cst[:, C_INVCNT + gi * 16:C_INVCNT + gi * 16 + 16],
                        op=ALU.mult), [srck, "cst"], ["junk"])
                    P.dve(lambda e, ch=ch: e.tensor_sub(pooled[:, ch, 0:16], junk[:, 0:16], pin[:, ch, 15:31]),
                          ["junk", "pin"], ["pooled"])
            P.dve(lambda e: e.tensor_copy(junk[:, 0:120].rearrange("p (a c) -> p a c", a=8), pin[:, :, TG:TG + 15]), ["pin"], ["junk"])
            P.dve(lambda e: e.tensor_copy(pin[:, :, 0:15], junk[:, 0:120].rearrange("p (a c) -> p a c", a=8)), ["junk"], ["pin"])
            for gi in range(4):
                for dc in range(2):
                    bk, bkk = bank()
                    for cc in range(2):
                        P.pe(lambda e, bk=bk, gi=gi, dc=dc, cc=cc: e.matmul(
                            bk[:, :], lhsT=pwb[:, gi * 2 + cc, dc * 128:(dc + 1) * 128], rhs=pooled[:, gi * 2 + cc, :],
                            start=(cc == 0), stop=(cc == 1)), ["pwb", "pooled"], [bkk])
                    P.act(lambda e, bk=bk, gi=gi, dc=dc: e.activation(
                        out=ypT[:, gi * 2 + dc, :], in_=bk[:, :], func=AF.Copy, scale=psc[:, gi * 2 + dc:gi * 2 + dc + 1]),
                        [bkk, "psc"], ["ypT"])
            P.dma("sp", lambda e, g=g: e.dma_start(out=Yp[:, :, g * TG:(g + 1) * TG].rearrange("k p t -> p k t"), in_=ypT[:]),
                  ["ypT"], ["Yp"])
            for h in range(8):
                wt, wtk = load_wt(wts, winb, OFF_D + h * 384, 384, "winb")
                P.dve(lambda e, h=h: e.tensor_copy(cin[:, :, 0:3], ctail[:, h * 3:h * 3 + 3, :]), ["ctail"], ["cin"])
                for i3 in range(3):
                    bk, bkk = proj_fm(wt, wtk, i3 * 128)
                    P.act(lambda e, bk=bk, i3=i3: e.copy(out=cin[:, i3, 3:3 + TG], in_=bk[:, :]), [bkk], ["cin"])
                P.dve(lambda e, h=h: e.tensor_copy(ctail[:, h * 3:h * 3 + 3, :], cin[:, :, TG:TG + 3]), ["cin"], ["ctail"])
                for i3 in range(3):
                    ch = i3 * 8 + h
                    for j in range(4):
                        wcol = cvw[:, ch * 4 + j:ch * 4 + j + 1]
                        if j == 0:
                            P.dve(lambda e, i3=i3, wcol=wcol: e.tensor_scalar(cacc[:, i3, :], cin[:, i3, 0:TG], wcol, None, ALU.mult),
                                  ["cin", "cvw"], ["cacc"])
                        else:
                            P.dve(lambda e, i3=i3, j=j, wcol=wcol: e.scalar_tensor_tensor(
                                out=cacc[:, i3, :], in0=cin[:, i3, j:j + TG], scalar=wcol, in1=cacc[:, i3, :],
                                op0=ALU.mult, op1=ALU.add), ["cin", "cvw", "cacc"], ["cacc"])
                    P.act(lambda e, i3=i3: e.activation(out=cacc[:, i3, :], in_=cacc[:, i3, :], func=AF.Silu), ["cacc"], ["cacc"])
                    if i3 < 2:
                        P.act(lambda e, i3=i3: e.activation(out=csq[:], in_=cacc[:, i3, :], func=AF.Square), ["cacc"], ["csq"])
                        bk, bkk = bank()
                        P.pe(lambda e, bk=bk: e.matmul(bk[:, :], lhsT=cm("ones"), rhs=csq[:], start=True, stop=True), ["cst", "csq"], [bkk])
                        P.act(lambda e, bk=bk: e.activation(out=crn[:], in_=bk[:, :], func=AF.Sqrt, bias=EPS), [bkk], ["crn"])
                        P.dve(lambda e: e.reciprocal(out=crn[:], in_=crn[:]), ["crn"], ["crn"])
                        sc_ = (128.0 ** -0.5) if i3 == 0 else 1.0
                        P.dve(lambda e, i3=i3, sc_=sc_: e.scalar_tensor_tensor(
                            out=cacc[:, i3, :], in0=cacc[:, i3, :], scalar=sc_, in1=crn[:], op0=ALU.mult, op1=ALU.mult),
                            ["cacc", "crn"], ["cacc"])
                    dstT = (DNq, DNk, DNv)[i3]
                    P.dma("sp", lambda e, dstT=dstT, h=h, g=g, i3=i3: e.dma_start(
                        out=dstT[h, :, g * TG:(g + 1) * TG], in_=cacc[:, i3, :]), ["cacc"], ["DN%d" % i3])
            for blk, off in ((0, OFF_GD), (1, OFF_V)):
                for wi in range(2):
                    wt, wtk = load_wt(wts, winb, off + wi * 512, 512, "winb")
                    for t in range(4):
                        bk, bkk = proj_tm(wt, wtk, t, 0, 512)
                        if blk == 0:
                            P.act(lambda e, bk=bk, t=t, wi=wi: e.activation(out=tm_bf[:, t, wi * 512:(wi + 1) * 512], in_=bk[:, :], func=AF.Silu),
                                  [bkk], ["tm_bf"])
                        else:
                            P.act(lambda e, bk=bk, t=t, wi=wi: e.copy(out=tm_bf[:, t, wi * 512:(wi + 1) * 512], in_=bk[:, :]), [bkk], ["tm_bf"])
                if blk == 0:
                    P.dma("sp", lambda e, g=g: e.dma_start(out=Gdn[g * TG:(g + 1) * TG, :].rearrange("(t p) c -> p t c", p=128), in_=tm_bf[:]),
                          ["tm_bf"], ["Gdn"])
                else:
                    P.dma("sp", lambda e, g=g: e.dma_start(out=Vc[:, g * 4:(g + 1) * 4, :], in_=tm_bf[:]), ["tm_bf"], ["Vc"])
            wtA, wtAk = load_wt(wts, winb, OFF_SBA, 16, "winb")
            wtB, wtBk = load_wt(wts, winb, OFF_SF, 8, "winb")
            for t in range(4):
                bk, bkk = proj_tm(wtA, wtAk, t, 0, 16)
                proj_tm(wtB, wtBk, t, 0, 8, bk=bk, bkk=bkk, o0=16)
                P.act(lambda e, bk=bk, g=g, t=t: e.copy(out=sg[:, g * 4 + t, :], in_=bk[:, 0:24]), [bkk], ["sg"])
            for h in range(8):
                if h % 2 == 0:
                    wt, wtk = load_wt(wts, winb, OFF_F + h * 256, 512, "winb")
                for i2 in range(2):
                    bk, bkk = proj_fm(wt, wtk, (h % 2) * 256 + i2 * 128)
                    P.act(lambda e, bk=bk, i2=i2: e.copy(out=fqk[:, i2, :], in_=bk[:, :]), [bkk], ["fqk"])
                    dstT = (Qc, Kc)[i2]
                    P.dma("sp", lambda e, dstT=dstT, h=h, g=g, i2=i2: e.dma_start(
                        out=dstT[h, :, g * TG:(g + 1) * TG], in_=fqk[:, i2, :]), ["fqk"], ["QK%d" % i2])


    def bc_last(ap2, n):
        return ap2.unsqueeze(2).to_broadcast([128, ap2.shape[1], n])

    def bc_mid(ap2, a):
        return ap2.unsqueeze(1).to_broadcast([128, a, ap2.shape[1]])

    def v4(bk):
        return bk[:, :].rearrange("p (a c) -> p a c", a=4)

    def phase_dn(l):
        new_phase()
        NT = S // 128
        A = lambda n, sh, dt=F32: AR.alloc(n, sh, dt)
        beta, gg, gc, glt, eg, sdA, sdB, sbg, nbeta = [A(n, [NT, 8]) for n in
                                                       ("beta", "gg", "gc", "glt", "eg", "sdA", "sdB", "sbg", "nbeta")]
        glrep = A("glrep", [NT, 2, 8])
        dtb, alg = A("dtb", [8]), A("alg", [8])
        onb = A("onb", [128])
        qT, kT, vT = A("qT", [S]), A("kT", [S]), A("vT", [S])
        gtm = A("gtm", [NT, 128], BF16)
        yT = A("yT", [S], BF16)
        Sst = A("Sst", [128])
        vn = A("vn", [128])
        o1 = A("o1", [128])
        M = {n: A(n, [4, 128]) for n in ("rhsG", "Dm", "DTm", "tmpN", "Na", "Nb", "Pa", "Pb", "R", "aT", "kbg", "kdA",
                                         "kdB", "vb", "u", "wkT", "o4", "sq")}
        yb = A("yb", [4, 128], BF16)
        ssum = A("ssum", [8])
        G = ["dn_gates"]
        P.dma("sp", lambda e: e.dma_start(out=dtb[:], in_=dt_bias_bc[l]), [], ["dtb"])
        P.dma("sp", lambda e: e.dma_start(out=alg[:], in_=a_log_bc[l]), [], ["alg"])
        P.dma("sp", lambda e: e.dma_start(out=onb[:], in_=onorm_bc[l]), [], ["onb"])
        P.act(lambda e: e.activation(out=beta[:], in_=sg[:, :, 0:8], func=AF.Sigmoid), ["sg"], G)
        P.dve(lambda e: e.tensor_tensor(out=gg[:], in0=sg[:, :, 8:16], in1=bc_mid(dtb[:], NT), op=ALU.add), ["sg", "dtb"], G)
        P.act(lambda e: e.activation(out=gg[:], in_=gg[:], func=AF.Exp), G, G)
        P.act(lambda e: e.activation(out=gg[:], in_=gg[:], func=AF.Ln, bias=1.0), G, G)
        P.act(lambda e: e.activation(out=alg[:], in_=alg[:], func=AF.Exp), ["alg"], ["alg"])
        P.dve(lambda e: e.tensor_scalar(alg[:], alg[:], -1.0, None, ALU.mult), ["alg"], ["alg"])
        P.dve(lambda e: e.tensor_tensor(out=gg[:], in0=gg[:], in1=bc_mid(alg[:], NT), op=ALU.mult), G + ["alg"], G)
        gg2 = gg[:].rearrange("p a b -> p (a b)")
        for dst, cname in ((gc, "triblk"), (glt, "blkones")):
            bk, bkk = bank()
            P.pe(lambda e, bk=bk, cname=cname: e.matmul(bk[:, 0:256], lhsT=cm(cname), rhs=gg2, start=True, stop=True), ["cst"] + G, [bkk])
            P.act(lambda e, bk=bk, dst=dst: e.copy(out=dst[:].rearrange("p a b -> p (a b)"), in_=bk[:, 0:256]), [bkk], G)
        for bi, cname in ((0, "selA"), (1, "selB")):
            bk, bkk = bank()
            P.pe(lambda e, bk=bk, cname=cname: e.matmul(bk[:, 0:256], lhsT=cm(cname), rhs=gg2, start=True, stop=True), ["cst"] + G, [bkk])
            P.act(lambda e, bk=bk, bi=bi: e.activation(out=glrep[:, :, bi, :], in_=bk[:, 0:256].rearrange("p (a b) -> p a b", a=NT),
                                                       func=AF.Exp), [bkk], G)
        P.act(lambda e: e.activation(out=eg[:], in_=gc[:], func=AF.Exp), G, G)
        P.dve(lambda e: e.tensor_sub(glt[:], glt[:], gc[:]), G, G)
        P.act(lambda e: e.activation(out=glt[:], in_=glt[:], func=AF.Exp), G, G)
        P.dve(lambda e: e.tensor_scalar(sdA[:], glt[:], cst[:, C_MASKA:C_MASKA + 1], None, ALU.mult), G + ["cst"], G)
        P.dve(lambda e: e.tensor_scalar(sdB[:], glt[:], cst[:, C_MASKB:C_MASKB + 1], None, ALU.mult), G + ["cst"], G)
        P.dve(lambda e: e.tensor_mul(sbg[:], beta[:], eg[:]), G, G)
        P.dve(lambda e: e.tensor_scalar(nbeta[:], beta[:], -1.0, None, ALU.mult), G, G)
        P.pool(lambda e: e.memset(vn[:], 0.0), [], ["vn"])
        identf = cm("ident")

        for h in range(cfg.get("dn_heads", 8)):
            P.dma("sp", lambda e, h=h: e.dma_start(out=qT[:], in_=DNq[h]), ["DN0"], ["qT"])
            P.dma("sp", lambda e, h=h: e.dma_start(out=kT[:], in_=DNk[h]), ["DN1"], ["kT"])
            P.dma("sp", lambda e, h=h: e.dma_start(out=vT[:], in_=DNv[h]), ["DN2"], ["vT"])
            P.dma("sp", lambda e, h=h: e.dma_start(out=gtm[:], in_=Gdn[:, h * 128:(h + 1) * 128].rearrange("(t p) c -> p t c", p=128)),
                  ["Gdn"], ["gtm"])
            P.pool(lambda e: e.memset(Sst[:], 0.0), [], ["Sst"])
            for bt in range(cfg.get("dn_batches", S // 512)):
                t0 = bt * 4
                dn_stage = cfg.get("dn_stage", 9)
                tk = lambda i: slice((t0 + i) * 128, (t0 + i + 1) * 128)

                def mm4(lhs_fn, rhs_fn, reads, transpose=False):
                    bk, bkk = bank()
                    for i in range(4):
                        la = lhs_fn(i)
                        if transpose:
                            P.pe(lambda e, bk=bk, i=i, la=la: e.matmul(bk[:, i * 128:(i + 1) * 128], lhsT=la, rhs=identf,
                                                                       start=True, stop=True), reads + ["cst"], [bkk])
                        else:
                            ra = rhs_fn(i)
                            P.pe(lambda e, bk=bk, i=i, la=la, ra=ra: e.matmul(bk[:, i * 128:(i + 1) * 128], lhsT=la, rhs=ra,
                                                                              start=True, stop=True), reads, [bkk])
                    return bk, bkk
                if dn_stage < 1:
                    continue
                sub = cfg.get("dn_sub", 99)
                if sub < 99:
                    bKK, kKK = mm4(lambda i: kT[:, tk(i)], lambda i: kT[:, tk(i)], ["kT"])
                    if sub >= 1:
                        P.dve(lambda e, h=h, t0=t0: e.tensor_tensor(out=M["rhsG"][:], in0=bc_mid(cm("strictU"), 4),
                                                                    in1=bc_last(gg[:, t0:t0 + 4, h], 128), op=ALU.mult), ["cst"] + G, ["rhsG"])
                    if sub >= 2:
                        bDd, kDd = mm4(lambda i: cm("triblk"), lambda i: M["rhsG"][:, i, :], ["cst", "rhsG"])
                    if sub >= 3:
                        P.act(lambda e, bk=bDd: e.activation(out=M["Dm"][:], in_=v4(bk), func=AF.Exp), [kDd], ["Dm"])
                    if sub >= 4:
                        P.dve(lambda e, bk=bKK: e.tensor_tensor(out=M["Na"][:], in0=v4(bk), in1=M["Dm"][:], op=ALU.mult), [kKK, "Dm"], ["Na"])
                    if sub >= 5:
                        bP, kP = mm4(lambda i: M["Na"][:, i, :], None, ["Na"], transpose=True)
                    if sub >= 6:
                        P.act(lambda e, bk=bP: e.copy(out=M["Pa"][:], in_=v4(bk)), [kP], ["Pa"])
                    continue
                bKK, kKK = mm4(lambda i: kT[:, tk(i)], lambda i: kT[:, tk(i)], ["kT"])
                bKQ, kKQ = mm4(lambda i: kT[:, tk(i)], lambda i: qT[:, tk(i)], ["kT", "qT"])
                P.dve(lambda e, h=h, t0=t0: e.tensor_tensor(out=M["rhsG"][:], in0=bc_mid(cm("strictU"), 4),
                                                            in1=bc_last(gg[:, t0:t0 + 4, h], 128), op=ALU.mult), ["cst"] + G, ["rhsG"])
                bDd, kDd = mm4(lambda i: cm("triblk"), lambda i: M["rhsG"][:, i, :], ["cst", "rhsG"])
                P.act(lambda e, bk=bDd: e.activation(out=M["Dm"][:], in_=v4(bk), func=AF.Exp), [kDd], ["Dm"])
                bDT, kDT = mm4(lambda i: M["Dm"][:, i, :], None, ["Dm"], transpose=True)
                P.dve(lambda e, bk=bDT: e.tensor_tensor(out=M["DTm"][:], in0=v4(bk), in1=bc_mid(cm("triblk"), 4), op=ALU.mult), [kDT, "cst"], ["DTm"])
                P.dve(lambda e: e.tensor_tensor(out=M["Dm"][:], in0=M["Dm"][:], in1=bc_mid(cm("strictU"), 4), op=ALU.mult), ["Dm", "cst"], ["Dm"])
                P.dve(lambda e, h=h, t0=t0: e.tensor_tensor(out=M["tmpN"][:], in0=M["Dm"][:], in1=bc_last(nbeta[:, t0:t0 + 4, h], 128),
                                                            op=ALU.mult), ["Dm"] + G, ["tmpN"])
                P.dve(lambda e, bk=bKK: e.tensor_tensor(out=M["Na"][:], in0=v4(bk), in1=M["tmpN"][:], op=ALU.mult), [kKK, "tmpN"], ["Na"])
                P.dve(lambda e, bk=bKQ: e.tensor_tensor(out=M["aT"][:], in0=v4(bk), in1=M["DTm"][:], op=ALU.mult), [kKQ, "DTm"], ["aT"])
                bP, kP = mm4(lambda i: M["Na"][:, i, :], None, ["Na"], transpose=True)
                P.act(lambda e, bk=bP: e.copy(out=M["Pa"][:], in_=v4(bk)), [kP], ["Pa"])
                P.dve(lambda e: e.tensor_tensor(out=M["R"][:], in0=M["Pa"][:], in1=bc_mid(identf, 4), op=ALU.add), ["Pa", "cst"], ["R"])
                N1, P1, N2, P2 = "Na", "Pa", "Nb", "Pb"
                for lev in range(5):
                    bN, kN = mm4(lambda i, P1=P1: M[P1][:, i, :], lambda i, N1=N1: M[N1][:, i, :], [N1, P1])
                    P.act(lambda e, bk=bN, N2=N2: e.copy(out=M[N2][:], in_=v4(bk)), [kN], [N2])
                    if lev < 4:
                        bQ, kQ = mm4(lambda i, N1=N1: M[N1][:, i, :], lambda i, P1=P1: M[P1][:, i, :], [N1, P1])
                        P.act(lambda e, bk=bQ, P2=P2: e.copy(out=M[P2][:], in_=v4(bk)), [kQ], [P2])
                    bR, kR = mm4(lambda i, N2=N2: M[N2][:, i, :], lambda i: M["R"][:, i, :], [N2, "R"])
                    P.dve(lambda e, bk=bR: e.tensor_tensor(out=M["R"][:], in0=v4(bk), in1=M["R"][:], op=ALU.add), [kR, "R"], ["R"])
                    N1, P1, N2, P2 = N2, P2, N1, P1
                bKt, kKt = mm4(lambda i: kT[:, tk(i)], None, ["kT"], transpose=True)
                bVt, kVt = mm4(lambda i: vT[:, tk(i)], None, ["vT"], transpose=True)
                for dstn, scal in (("kbg", sbg), ("kdA", sdA), ("kdB", sdB)):
                    P.dve(lambda e, bk=bKt, dstn=dstn, scal=scal, h=h, t0=t0: e.tensor_tensor(
                        out=M[dstn][:], in0=v4(bk), in1=bc_last(scal[:, t0:t0 + 4, h], 128), op=ALU.mult), [kKt] + G, [dstn])
                P.dve(lambda e, bk=bVt, h=h, t0=t0: e.tensor_tensor(out=M["vb"][:], in0=v4(bk), in1=bc_last(beta[:, t0:t0 + 4, h], 128),
                                                                    op=ALU.mult), [kVt] + G, ["vb"])
                bU, kU = mm4(lambda i: M["R"][:, i, :], lambda i: M["vb"][:, i, :], ["R", "vb"])
                P.act(lambda e, bk=bU: e.copy(out=M["u"][:], in_=v4(bk)), [kU], ["u"])
                bW, kW = mm4(lambda i: M["kbg"][:, i, :], lambda i: M["R"][:, i, :], ["R", "kbg"])
                P.act(lambda e, bk=bW: e.copy(out=M["wkT"][:], in_=v4(bk)), [kW], ["wkT"])
                if cfg.get("dn_dump") and bt == 0 and h == 0:
                    lim = P.limit
                    P.limit = None
                    for nm in ("Na", "Pa", "Nb", "Pb", "R", "Dm", "DTm", "aT", "rhsG", "kbg", "kdA", "kdB", "vb", "u", "wkT"):
                        o = dout("dump_" + nm, [128, 4, 128])
                        P.dma("sp", lambda e, o=o, nm=nm: e.dma_start(out=o[:, :, :], in_=M[nm][:]), [nm], [])
                    for nm, t_ in (("gg", gg), ("beta", beta), ("gc", gc), ("eg", eg), ("sdA", sdA), ("sbg", sbg)):
                        o = dout("dump_" + nm, [128, NT, 8])
                        P.dma("sp", lambda e, o=o, t_=t_: e.dma_start(out=o[:, :, :], in_=t_[:]), G, [])
                    o = dout("dump_glrep", [128, NT, 2, 8])
                    P.dma("sp", lambda e, o=o: e.dma_start(out=o[:, :, :, :], in_=glrep[:]), G, [])
                    P.limit = lim
                for i in range(4 if dn_stage >= 2 else 0):
                    t = t0 + i
                    for half, (lo, hi), kd in ((0, (0, 64), "kdA"), (1, (64, 128), "kdB")):
                        bk, bkk = bank()
                        P.pe(lambda e, bk=bk, i=i: e.matmul(bk[:, 0:128], lhsT=M["wkT"][:, i, :], rhs=Sst[:], start=True, stop=True),
                             ["wkT", "Sst"], [bkk])
                        qsl = qT[:, tk(i)]
                        P.pe(lambda e, bk=bk, qsl=qsl: e.matmul(bk[:, 128:256], lhsT=qsl, rhs=Sst[:], start=True, stop=True),
                             ["qT", "Sst"], [bkk])
                        P.dve(lambda e, bk=bk, i=i, lo=lo, hi=hi: e.tensor_sub(vn[lo:hi, :], M["u"][lo:hi, i, :], bk[lo:hi, 0:128]),
                              ["u", bkk], ["vn"])
                        P.dve(lambda e, bk=bk, lo=lo, hi=hi, t=t, h=h: e.tensor_scalar(o1[lo:hi, :], bk[lo:hi, 128:256], eg[lo:hi, t, h:h + 1],
                                                                                     None, ALU.mult), [bkk] + G, ["o1"])
                        bk2, bkk2 = bank()
                        P.pe(lambda e, bk2=bk2, i=i, kd=kd: e.matmul(bk2[:, 0:128], lhsT=M[kd][:, i, :], rhs=vn[:], start=True, stop=True),
                             [kd, "vn"], [bkk2])
                        P.dve(lambda e, bk2=bk2, t=t, half=half, h=h: e.scalar_tensor_tensor(
                            out=Sst[:], in0=Sst[:], scalar=glrep[:, t, half, h:h + 1], in1=bk2[:, 0:128], op0=ALU.mult, op1=ALU.add),
                            [bkk2, "Sst"] + G, ["Sst"])
                    bk3, bkk3 = bank()
                    P.pe(lambda e, bk3=bk3, i=i: e.matmul(bk3[:, 0:128], lhsT=M["aT"][:, i, :], rhs=vn[:], start=True, stop=True),
                         ["aT", "vn"], [bkk3])
                    P.dve(lambda e, bk3=bk3, i=i: e.tensor_add(M["o4"][:, i, :], o1[:], bk3[:, 0:128]), [bkk3, "o1"], ["o4"])
                if cfg.get("dn_dump") and dn_stage >= 2 and bt == 0 and h == 0:
                    o = dout("dump_o4", [128, 4, 128])
                    P.dma("sp", lambda e, o=o: e.dma_start(out=o[:, :, :], in_=M["o4"][:]), ["o4"], [])
                    o = dout("dump_S", [128, 128])
                    P.dma("sp", lambda e, o=o: e.dma_start(out=o[:, :], in_=Sst[:]), ["Sst"], [])
                if dn_stage < 3:
                    continue
                P.pool(lambda e: e.tensor_tensor(out=M["sq"][:], in0=M["o4"][:], in1=M["o4"][:], op=ALU.mult), ["o4"], ["sq"])
                P.dve(lambda e: e.tensor_reduce(out=ssum[:, 0:4], in_=M["sq"][:], axis=AX.X, op=ALU.add), ["sq"], ["ssum"])
                P.act(lambda e: e.activation(out=ssum[:, 4:8], in_=ssum[:, 0:4], func=AF.Sqrt, scale=1.0 / 128, bias=EPS), ["ssum"], ["ssum"])
                P.dve(lambda e: e.reciprocal(out=ssum[:, 4:8], in_=ssum[:, 4:8]), ["ssum"], ["ssum"])
                P.dve(lambda e: e.tensor_tensor(out=M["o4"][:], in0=M["o4"][:], in1=bc_last(ssum[:, 4:8], 128), op=ALU.mult), ["o4", "ssum"], ["o4"])
                P.dve(lambda e: e.tensor_tensor(out=M["o4"][:], in0=M["o4"][:], in1=bc_mid(onb[:], 4), op=ALU.mult), ["o4", "onb"], ["o4"])
                P.dve(lambda e, t0=t0: e.tensor_tensor(out=yb[:], in0=M["o4"][:], in1=gtm[:, t0:t0 + 4, :], op=ALU.mult), ["o4", "gtm"], ["yb"])
                if cfg.get("dn_dump") and bt == 0 and h == 0:
                    o = dout("dump_yb", [128, 4, 128], BF16)
                    P.dma("sp", lambda e, o=o: e.dma_start(out=o[:, :, :], in_=yb[:]), ["yb"], [])
                    o = dout("dump_ssum", [128, 8])
                    P.dma("sp", lambda e, o=o: e.dma_start(out=o[:, :], in_=ssum[:]), ["ssum"], [])
                bk, bkk = bank()
                bkb = bk[:, :].bitcast(BF16)
                for i in range(4):
                    P.pe(lambda e, bkb=bkb, i=i: e.transpose(out=bkb[:, i * 128:(i + 1) * 128], in_=yb[:, i, :], identity=ident_bf[:]),
                         ["yb", "ident_bf"], [bkk])
                P.act(lambda e, bkb=bkb, bt=bt: e.copy(out=yT[:, bt * 512:(bt + 1) * 512], in_=bkb[:, 0:512]), [bkk], ["yT"])
            if cfg.get("dn_stage", 9) >= 3:
                P.dma("sp", lambda e, h=h: e.dma_start(out=Yd[h], in_=yT[:]), ["yT"], ["Yd"])


    FILL = {}

    def fill_reg(e):
        if "r" not in FILL:
            FILL["r"] = e.to_reg(-30000.0)
        return FILL["r"]

    def phase_fox(l):
        new_phase()
        NT = S // 128
        A = lambda n, sh, dt=F32: AR.alloc(n, sh, dt)
        lf, cf, tot, incl = A("lf", [NT, 8]), A("cf", [NT, 8]), A("tot", [NT, 8]), A("incl", [NT, 8])
        fbb = A("fbb", [8])
        rrep = A("rrep", [8, 8])
        sel8 = A("sel8", [8, 128])
        cfT = A("cfT", [S])
        rowv = A("rowv", [512])
        cfq = A("cfq", [512])
        bkg = A("bkg", [NT])
        qT, kT = A("fqT", [S], BF16), A("fkT", [S], BF16)
        Vh = A("Vh", [NT, 128], BF16)
        yT = A("fyT", [S], BF16)
        tmps = [A("ftmp%d" % i, [512]) for i in range(2)]
        pTs = [A("fpT%d" % i, [512], BF16) for i in range(2)]
        rinv = A("rinv", [512])
        F = ["fox_gates"]
        P.pool(lambda e: e.memset(cfT[:], 0.0), [], ["cfT"])
        P.dma("sp", lambda e: e.dma_start(out=fbb[:], in_=f_bias_bc[l]), [], ["fbb"])
        P.dve(lambda e: e.tensor_tensor(out=lf[:], in0=sg[:, :, 16:24], in1=bc_mid(fbb[:], NT), op=ALU.add), ["sg", "fbb"], F)
        P.act(lambda e: e.activation(out=lf[:], in_=lf[:], func=AF.Exp, scale=-1.0), F, F)
        P.act(lambda e: e.activation(out=lf[:], in_=lf[:], func=AF.Ln, bias=1.0), F, F)
        P.dve(lambda e: e.tensor_scalar(lf[:], lf[:], -1.0, None, ALU.mult), F, F)
        lf2 = lf[:].rearrange("p a b -> p (a b)")
        for dst, cname in ((cf, "tri128"), (tot, "ones")):
            bk, bkk = bank()
            P.pe(lambda e, bk=bk, cname=cname: e.matmul(bk[:, 0:256], lhsT=cm(cname), rhs=lf2, start=True, stop=True), ["cst"] + F, [bkk])
            P.act(lambda e, bk=bk, dst=dst: e.copy(out=dst[:].rearrange("p a b -> p (a b)"), in_=bk[:, 0:256]), [bkk], F)
        for h in range(8):
            P.dve(lambda e, h=h: e.tensor_tensor_scan(out=incl[:, :, h], data0=cm("ones")[:, 0:NT], data1=tot[:, :, h], initial=0.0,
                                                     op0=ALU.mult, op1=ALU.add), ["cst"] + F, F)
        P.dve(lambda e: e.tensor_sub(incl[:], incl[:], tot[:]), F, F)
        P.dve(lambda e: e.tensor_add(cf[:], cf[:], incl[:]), F, F)
        bk, bkk = bank()
        cf4 = cf[:].rearrange("p (g f) h -> p g f h", f=4)[:, :, 0, :]
        P.pe(lambda e, bk=bk: e.matmul(bk[:, 0:64].rearrange("p (g h) -> p g h", g=8), lhsT=cm("selrow0"), rhs=cf4, start=True, stop=True),
             ["cst"] + F, [bkk])
        P.act(lambda e, bk=bk: e.copy(out=rrep[:].rearrange("p g h -> p (g h)"), in_=bk[:, 0:64]), [bkk], F)
        for q4 in range(8):
            bk, bkk = bank()
            for i in range(4):
                t = q4 * 4 + i
                P.pe(lambda e, bk=bk, i=i, t=t: e.matmul(bk[0:8, i * 128:(i + 1) * 128], lhsT=cf[:, t, :], rhs=cm("ident"), start=True, stop=True),
                     ["cst"] + F, [bkk])
            P.act(lambda e, bk=bk, q4=q4: e.copy(out=cfT[0:8, q4 * 512:(q4 + 1) * 512], in_=bk[0:8, :]), [bkk], ["cfT"])
        P.pool(lambda e: e.memset(sel8[:], 0.0), [], ["sel8"])
        P.pool(lambda e: e.memset(rowv[:], 0.0), [], ["rowv"])
        P.dve(lambda e: e.tensor_copy(sel8[0:8, :, :], cm("ident")[0:8, 0:8].unsqueeze(2).to_broadcast([8, 8, 128])), ["cst", "sel8"], ["sel8"])
        scale = 128.0 ** -0.5
        nheads = cfg.get("fox_heads", 8)
        for h in range(nheads):
            P.dma("sp", lambda e, h=h: e.dma_start(out=qT[:], in_=Qc[h]), ["QK0"], ["fqT"])
            P.dma("sp", lambda e, h=h: e.dma_start(out=kT[:], in_=Kc[h]), ["QK1"], ["fkT"])
            P.dma("sp", lambda e, h=h: e.dma_start(out=Vh[:], in_=Vc[:, :, h * 128:(h + 1) * 128]), ["Vc"], ["Vh"])
            for g in range(cfg.get("fox_groups", NG)):
                gs = slice(g * 512, (g + 1) * 512)
                nj = 4 * g + 4
                P.dve(lambda e, g=g, gs=gs: e.tensor_scalar(rowv[0:8, :], cfT[0:8, gs], cfT[0:8, g * 512:g * 512 + 1], None,
                                                            ALU.subtract), ["cfT"], ["rowv"])
                P.bank_lo, P.bank_n = 0, 4
                bk, bkk = bank()
                P.pe(lambda e, bk=bk, h=h: e.matmul(bk[:, :], lhsT=sel8[0:8, h, :], rhs=rowv[0:8, :], start=True, stop=True), ["sel8", "rowv"], [bkk])
                P.act(lambda e, bk=bk: e.copy(out=cfq[:], in_=bk[:, :]), [bkk], ["cfq"])
                P.dve(lambda e, g=g, h=h, nj=nj: e.tensor_scalar(bkg[:, 0:nj], cf[:, 0:nj, h], -1.0, rrep[:, g, h:h + 1], ALU.mult, ALU.add),
                      F, ["bkg"])
                if cfg.get("fox_dump") and g == 0 and h == 0:
                    for nm, t_, shp in (("cfq", cfq, [128, 512]), ("rowv", rowv, [128, 512]), ("cfT", cfT, [128, S]), ("cf", cf, [128, NT, 8]),
                                        ("rrep", rrep, [128, 8, 8]), ("sel8", sel8, [128, 8, 128]), ("bkg", bkg, [128, NT])):
                        o = dout("dump_" + nm, shp)
                        P.dma("sp", lambda e, o=o, t_=t_: e.dma_start(out=o, in_=t_[:]), ["cfq", "rowv", "cfT", "sel8", "bkg"] + F, [])
                    continue
                ai = 4 + 2 * (g % 2)
                accO, kO, accS, kS = banks[ai], "bank%d" % ai, banks[ai + 1], "bank%d" % (ai + 1)

                def qk(j):
                    c0 = 0
                    n = 512
                    bs, bsk = bank()
                    ka = kT[:, j * 128:(j + 1) * 128]
                    qa = qT[:, g * 512 + c0:(g + 1) * 512]
                    P.pe(lambda e: e.matmul(bs[:, 0:n], lhsT=ka, rhs=qa, start=True, stop=True), ["fkT", "fqT"], [bsk])
                    return bs, bsk, c0, n
                nxt = qk(0)
                for j in range(nj):
                    bs, bsk, c0, n = nxt
                    tmp, tmpk = tmps[j % 2], "ftmp%d" % (j % 2)
                    pT, pTk = pTs[j % 2], "fpT%d" % (j % 2)
                    P.dve(lambda e, bs=bs, tmp=tmp, c0=c0, n=n: e.scalar_tensor_tensor(
                        out=tmp[:, 0:n], in0=bs[:, 0:n], scalar=scale, in1=cfq[:, c0:512], op0=ALU.mult, op1=ALU.add), [bsk, "cfq"], [tmpk])
                    if j >= 4 * g:
                        jj = j - 4 * g
                        P.pool(lambda e, tmp=tmp, jj=jj: e.affine_select(out=tmp[:, :], in_=tmp[:, :], pattern=[[1, 512]], compare_op=ALU.is_ge,
                                                                         fill=fill_reg(e), base=-128 * jj, channel_multiplier=-1), [tmpk], [tmpk])
                    P.act(lambda e, tmp=tmp, pT=pT, n=n, j=j: e.activation(out=pT[:, 0:n], in_=tmp[:, 0:n], func=AF.Exp, bias=bkg[:, j:j + 1]),
                          [tmpk, "bkg"], [pTk])
                    if j + 1 < nj:
                        nxt = qk(j + 1)
                    va = Vh[:, j, :]
                    P.pe(lambda e, pT=pT, va=va, c0=c0, n=n, j=j, accO=accO, nj=nj: e.matmul(accO[:, c0:512], lhsT=va, rhs=pT[:, 0:n], start=(j == 0), stop=(j == nj - 1)),
                         ["Vh", pTk], [kO])
                    P.pe(lambda e, pT=pT, c0=c0, n=n, j=j, accS=accS, nj=nj: e.matmul(accS[:, c0:512], lhsT=ones_bf[:], rhs=pT[:, 0:n], start=(j == 0), stop=(j == nj - 1)),
                         ["ones_bf", pTk], [kS])
                P.dve(lambda e, accS=accS: e.reciprocal(out=rinv[:], in_=accS[:, :]), [kS], ["rinv"])
                P.dve(lambda e, gs=gs, accO=accO: e.tensor_tensor(out=yT[:, gs], in0=accO[:, :], in1=rinv[:], op=ALU.mult), [kO, "rinv"], ["fyT"])
            P.bank_lo, P.bank_n = 0, NBANK
            if cfg.get("fox_groups", NG) == NG:
                P.dma("sp", lambda e, h=h: e.dma_start(out=Yf[h], in_=yT[:]), ["fyT"], ["Yf"])


    def phase_merge(l, xsrc, xkey):
        new_phase()
        A = lambda n, sh, dt=F32: AR.alloc(n, sh, dt)
        hT = A("hT", [KC, TG], BF16)
        HT["hT"] = hT
        Ys = [A("Y%d" % b, [8, TG], BF16) for b in range(3)]
        wts = [A("wt%d" % i, [KC, 384], BF16) for i in range(2)]
        wbt = [A("wbt%d" % i, [3, 8, 128], BF16) for i in range(2)]
        gsb = A("gsb", [3, TG])
        macc, mtmp = A("macc", [TG]), A("mtmp", [TG])
        mergedT = A("mergedT", [KC, TG], BF16)
        xts = [A("xt0", [D])] * 2
        wos = [A("wo%d" % i, [KC, 512], BF16) for i in range(2)]
        rtmp = A("rtmp", [512])
        mod_vector(l, 2, modt[2], "mod2")
        Ysrc = (Yp, Yd, Yf)
        Ykey = ("Yp", "Yd", "Yf")
        for g in range(cfg.get("mg_groups", NG)):
            gs = slice(g * TG, (g + 1) * TG)
            P.dma("sp", lambda e, gs=gs: e.dma_start(out=hT[:], in_=Hs[:, :, gs].rearrange("k p t -> p k t")), ["Hs"], ["hT"])
            for b in range(3):
                P.dma("sp", lambda e, gs=gs, b=b: e.dma_start(out=Ys[b][:], in_=Ysrc[b][:, :, gs].rearrange("k p t -> p k t")), [Ykey[b]], ["Y%d" % b])
            for j in range(KC):
                wt, wtk = load_wt(wts, winb, OFF_G + j * 384, 384, "winb")
                wb, wbk = wbt[j % 2], "wbt%d" % (j % 2)
                for b in range(3):
                    P.dma("sp", lambda e, wb=wb, b=b, j=j: e.dma_start(
                        out=wb[:, b, :, :], in_=wbrb[b][:, j * 128:(j + 1) * 128].rearrange("(k p) c -> p k c", p=128)), ["wbrb"], [wbk])
                for b in range(3):
                    bk, bkk = proj_fm(wt, wtk, b * 128)
                    P.act(lambda e, bk=bk, b=b: e.activation(out=gsb[:, b, :], in_=bk[:, :], func=AF.Sigmoid), [bkk], ["gsb"])
                for b in range(3):
                    bz, bzk = bank()
                    for k in range(8):
                        P.pe(lambda e, bz=bz, wb=wb, b=b, k=k: e.matmul(bz[:, :], lhsT=wb[:, b, k, :], rhs=Ys[b][:, k, :],
                                                                        start=(k == 0), stop=(k == 7)), [wbk, "Y%d" % b], [bzk])
                    if b == 0:
                        P.dve(lambda e, bz=bz: e.tensor_tensor(out=macc[:], in0=bz[:, :], in1=gsb[:, 0, :], op=ALU.mult), [bzk, "gsb"], ["macc"])
                    else:
                        P.dve(lambda e, bz=bz, b=b: e.tensor_tensor(out=mtmp[:], in0=bz[:, :], in1=gsb[:, b, :], op=ALU.mult), [bzk, "gsb"], ["mtmp"])
                        if b == 1:
                            P.pool(lambda e: e.tensor_tensor(out=macc[:], in0=macc[:], in1=mtmp[:], op=ALU.add), ["macc", "mtmp"], ["macc"])
                        else:
                            P.pool(lambda e, j=j: e.tensor_tensor(out=mergedT[:, j, :], in0=macc[:], in1=mtmp[:], op=ALU.add),
                                   ["macc", "mtmp"], ["mergedT"])
            for t in range(4):
                ti = g * 4 + t
                xt, xtk = xts[0], "xt0"
                r0 = ti * 128
                P.dma("sp", lambda e, xt=xt, r0=r0: e.dma_start(out=xt[:], in_=xsrc[r0:r0 + 128, :]), [xkey + str(ti)], [xtk])
                for cb in range(4):
                    wo, wok = load_wt(wos, woutb, cb * 512, 512, "woutb", pref="wo")
                    bk, bkk = bank()
                    for k in range(KC):
                        P.pe(lambda e, bk=bk, wo=wo, k=k, t=t: e.matmul(bk[:, :], lhsT=mergedT[:, k, t * 128:(t + 1) * 128], rhs=wo[:, k, :],
                                                                        start=(k == 0), stop=(k == KC - 1)), [wok, "mergedT"], [bkk])
                    P.dve(lambda e, bk=bk, cb=cb: e.tensor_tensor(out=rtmp[:], in0=bk[:, :], in1=modt[2][:, cb * 512:(cb + 1) * 512], op=ALU.mult),
                          [bkk, "mod2"], ["rtmp"])
                    P.dve(lambda e, xt=xt, cb=cb: e.tensor_add(xt[:, cb * 512:(cb + 1) * 512], xt[:, cb * 512:(cb + 1) * 512], rtmp[:]),
                          ["rtmp", xtk], [xtk])
                P.dma("sp", lambda e, xt=xt, r0=r0: e.dma_start(out=xres[r0:r0 + 128, :], in_=xt[:]), [xtk], ["xres" + str(ti)])


    peer_u_flat = peer_u.rearrange("l e d -> (l e) d") if peer_u is not None else None
    peer_v_flat = peer_v.rearrange("l e d -> (l e) d") if peer_v is not None else None

    pub = dscr("pub", [NL * NEXP, D], BF16)
    pvb = dscr("pvb", [NL * NEXP, D], BF16)

    def convert_tables():
        for src, dst, key in ((peer_u_flat, pub, "pub"), (peer_v_flat, pvb, "pvb")):
            for r0 in range(0, NL * NEXP, 8192):
                P.dma("pool", lambda e, src=src, dst=dst, r0=r0: e.dma_start(out=dst[r0:r0 + 8192, :], in_=src[r0:r0 + 8192, :]), [], [key + str(r0)])
    TABKEYS = [k + str(r0) for k in ("pub", "pvb") for r0 in range(0, NL * NEXP, 8192)]

    def phase_peer(l, last):
        new_phase()
        A = lambda n, sh, dt=F32: AR.alloc(n, sh, dt)
        hf = [A("hf%d" % i, [D]) for i in range(2)]
        hfT = A("hfT", [KC, 256])
        wq = A("wq", [KC, 128])
        qTc = A("qTc", [256])
        skt = A("skt", [16, 128])
        scs = A("scs", [2, 16, 128])
        cw = A("cw", [2048])
        cand = A("cand", [8, 16, 16])
        work = A("work", [2048])
        tv, tif = A("tv", [16, 16]), A("tif", [16, 16])
        ti = A("ti", [16, 16], U32)
        bs, bpf, i1, i2, sel1, sel2 = [A(n, [8, 16]) for n in ("bs", "bpf", "i1", "i2", "sel1", "sel2")]
        bp = A("bp", [8, 16], U32)
        idf = A("idf", [128])
        apre2 = [A("apre%d" % i, [128]) for i in range(2)]
        coef2 = [A("coef%d" % i, [128]) for i in range(2)]
        idi2 = [A("idi%d" % i, [128], I32) for i in range(2)]
        wsm2 = [A("wsm%d" % i, [8, 16]) for i in range(2)]
        wsum = A("wsum", [8])
        thr = A("thr", [16])
        NGB = 6
        Gs = [A("G%d" % i, [D], BF16) for i in range(NGB)]
        dgs = [A("dg%d" % i, [128], BF16) for i in range(4)]
        fnw = A("fnw", [D]) if last else None
        junk = A("pjunk", [D], BF16)
        cw16 = cw[:, :].rearrange("p (c n) -> p c n", c=16)
        cw8 = cw[:, :].rearrange("p (h n) -> p h n", h=8)
        tv4 = tv[:].rearrange("p (h t) k -> p h t k", t=2)
        tif4 = tif[:].rearrange("p (h t) k -> p h t k", t=2)
        iota16 = cst[:, C_IOTA16:C_IOTA16 + 16]
        mod_vector(l, 4, modt[0], "mod0", plus_one=True, mul_row=norm_ffn_w[l:l + 1, :])
        mod_vector(l, 3, modt[1], "mod1")
        mod_vector(l, 5, modt[2], "mod2")
        P.dma("sp", lambda e: e.dma_start(out=skt[:], in_=skT[l].rearrange("c d n -> d c n")), [], ["skt"])
        if last:
            P.dma("sp", lambda e: e.dma_start(out=fnw[:], in_=final_norm_w[0:1, :].to_broadcast([128, D])), [], ["fnw"])
        P.dve(lambda e: e.tensor_scalar(thr[:, 0:15], iota16[:, 1:16], 16.0, None, ALU.mult), ["cst"], ["thr"])
        identf = cm("ident")
        P.bank_lo, P.bank_n = 0, 4
        ngrp = cfg.get("peer_groups", (S // 512) if (last and SPLIT) else (S // 256))
        XALL = ["xres%d" % i_ for i_ in range(S // 128)]

        def load_x(dst, dstk, ti_, r0):
            if last and SPLIT:
                P.dma("pool", lambda e: e.indirect_dma_start(out=dst[:], out_offset=None, in_=xres,
                                                             in_offset=bass.IndirectOffsetOnAxis(ap=ridx[:, ti_:ti_ + 1], axis=0)),
                      XALL + ["ridx"], [dstk])
            else:
                P.dma("sp", lambda e: e.dma_start(out=dst[:], in_=xres[r0:r0 + 128, :]), ["xres%d" % ti_], [dstk])
        for g in range(ngrp):
            for t in range(2):
                ti_ = g * 2 + t
                r0 = ti_ * 128
                hk = "hf%d" % t
                load_x(hf[t], hk, ti_, r0)
                rmsnorm_tile(hf[t], hk, modt[0], "mod0", modt[1], "mod1", junk)
                for q4 in range(4):
                    bk, bkk = bank()
                    for i in range(4):
                        k = q4 * 4 + i
                        P.pe(lambda e, bk=bk, i=i, k=k, t=t: e.matmul(bk[:, i * 128:(i + 1) * 128], lhsT=hf[t][:, k * 128:(k + 1) * 128], rhs=identf,
                                                                      start=True, stop=True), [hk, "cst"], [bkk])
                    P.act(lambda e, bk=bk, q4=q4, t=t: e.copy(out=hfT[:, q4 * 4:q4 * 4 + 4, t * 128:(t + 1) * 128], in_=v4(bk)), [bkk], ["hfT"])
            for c in range(16):
                P.dma("sp", lambda e, c=c: e.dma_start(out=wq[:], in_=peer_w_q[l, :, c * 128:(c + 1) * 128].rearrange("(k p) c -> p k c", p=128)),
                      [], ["wq"])
                bk, bkk = bank()
                for k in range(KC):
                    P.pe(lambda e, bk=bk, k=k: e.matmul(bk[:, 0:256], lhsT=wq[:, k, :], rhs=hfT[:, k, :], start=(k == 0), stop=(k == KC - 1)),
                         ["wq", "hfT"], [bkk])
                P.act(lambda e, bk=bk: e.copy(out=qTc[:], in_=bk[:, 0:256]), [bkk], ["qTc"])
                bk2, bkk2 = bank()
                for t in range(2):
                    P.pe(lambda e, bk2=bk2, t=t, c=c: e.matmul(bk2[:, t * 128:(t + 1) * 128], lhsT=qTc[:, t * 128:(t + 1) * 128], rhs=skt[:, c, :],
                                                               start=True, stop=True), ["qTc", "skt"], [bkk2])
                P.act(lambda e, bk2=bk2, c=c: e.copy(out=scs[:, :, c, :], in_=bk2[:, 0:256].rearrange("p (t n) -> p t n", t=2)), [bkk2], ["scs"])
            def stage_c(t):
                ti_ = g * 2 + t
                r0 = ti_ * 128
                hk = "hf%d" % t
                idi, wsm, apre, coef = idi2[t], wsm2[t], apre2[t], coef2[t]
                kI, kW, kA, kC = "idi%d" % t, "wsm%d" % t, "apre%d" % t, "coef%d" % t
                nslot = cfg.get("peer_slots", 128)
                for c in range(16):
                    sv = scs[:, t, c, :]
                    P.dve(lambda e, c=c, sv=sv: e.max(out=tv[:, c, 0:8], in_=sv), ["scs"], ["tv"])
                    P.dve(lambda e, c=c, sv=sv: e.max_index(out=ti[:, c, 0:8], in_max=tv[:, c, 0:8], in_values=sv), ["scs", "tv"], ["ti"])
                    P.dve(lambda e, c=c, sv=sv: e.match_replace(out=cw16[:, c, :], in_to_replace=tv[:, c, 0:8], in_values=sv, imm_value=-1e30),
                          ["scs", "tv"], ["cw"])
                    P.dve(lambda e, c=c: e.max(out=tv[:, c, 8:16], in_=cw16[:, c, :]), ["cw"], ["tv"])
                    P.dve(lambda e, c=c: e.max_index(out=ti[:, c, 8:16], in_max=tv[:, c, 8:16], in_values=cw16[:, c, :]), ["cw", "tv"], ["ti"])
                P.dve(lambda e: e.tensor_copy(tif[:], ti[:]), ["ti"], ["tif"])
                P.dve(lambda e: e.tensor_tensor(out=cand[:], in0=tv4[:, :, 0, :].unsqueeze(3).to_broadcast([128, 8, 16, 16]),
                                                in1=tv4[:, :, 1, :].unsqueeze(2).to_broadcast([128, 8, 16, 16]), op=ALU.add), ["tv"], ["cand"])
                for h in range(8):
                    ch = cand[:, h, :, :].rearrange("p a b -> p (a b)")
                    P.dve(lambda e, h=h, ch=ch: e.max(out=bs[:, h, 0:8], in_=ch), ["cand"], ["bs"])
                    P.dve(lambda e, h=h, ch=ch: e.max_index(out=bp[:, h, 0:8], in_max=bs[:, h, 0:8], in_values=ch), ["cand", "bs"], ["bp"])
                    P.dve(lambda e, h=h, ch=ch: e.match_replace(out=cw8[:, h, :], in_to_replace=bs[:, h, 0:8], in_values=ch, imm_value=-1e30),
                          ["cand", "bs"], ["cw"])
                    P.dve(lambda e, h=h: e.max(out=bs[:, h, 8:16], in_=cw8[:, h, :]), ["cw"], ["bs"])
                    P.dve(lambda e, h=h: e.max_index(out=bp[:, h, 8:16], in_max=bs[:, h, 8:16], in_values=cw8[:, h, :]), ["cw", "bs"], ["bp"])
                P.dve(lambda e: e.tensor_copy(bpf[:], bp[:]), ["bp"], ["bpf"])
                w15 = work[:, 0:1920].rearrange("p (h k m) -> p h k m", h=8, k=16)
                P.dve(lambda e: e.tensor_tensor(out=w15, in0=bpf[:].unsqueeze(3).to_broadcast([128, 8, 16, 15]),
                                                in1=thr[:, 0:15].unsqueeze(1).unsqueeze(1).to_broadcast([128, 8, 16, 15]), op=ALU.is_ge),
                      ["bpf", "thr"], ["work"])
                P.dve(lambda e: e.tensor_reduce(out=i1[:], in_=w15, axis=AX.X, op=ALU.add), ["work"], ["i1"])
                P.dve(lambda e: e.scalar_tensor_tensor(out=i2[:].rearrange("p a b -> p (a b)"), in0=i1[:].rearrange("p a b -> p (a b)"), scalar=-16.0,
                                                       in1=bpf[:].rearrange("p a b -> p (a b)"), op0=ALU.mult, op1=ALU.add), ["i1", "bpf"], ["i2"])
                w16 = work[:, 0:2048].rearrange("p (h k m) -> p h k m", h=8, k=16)
                for ii, src_half, dsel in ((i1, 0, sel1), (i2, 1, sel2)):
                    iik = "i1" if src_half == 0 else "i2"
                    dk_ = "sel1" if src_half == 0 else "sel2"
                    P.dve(lambda e, ii=ii: e.tensor_tensor(out=w16, in0=ii[:].unsqueeze(3).to_broadcast([128, 8, 16, 16]),
                                                           in1=iota16.unsqueeze(1).unsqueeze(1).to_broadcast([128, 8, 16, 16]), op=ALU.is_equal),
                          [iik, "cst"], ["work"])
                    P.dve(lambda e, src_half=src_half: e.tensor_tensor(out=w16, in0=w16, in1=tif4[:, :, src_half, :].unsqueeze(2).to_broadcast([128, 8, 16, 16]),
                                                                       op=ALU.mult), ["work", "tif"], ["work"])
                    P.dve(lambda e, dsel=dsel: e.tensor_reduce(out=dsel[:], in_=w16, axis=AX.X, op=ALU.add), ["work"], [dk_])
                P.dve(lambda e: e.scalar_tensor_tensor(out=idf[:], in0=sel1[:].rearrange("p a b -> p (a b)"), scalar=128.0,
                                                       in1=sel2[:].rearrange("p a b -> p (a b)"), op0=ALU.mult, op1=ALU.add), ["sel1", "sel2"], ["idf"])
                if l > 0:
                    P.dve(lambda e: e.tensor_scalar(idf[:], idf[:], float(l * NEXP), None, ALU.add), ["idf"], ["idf"])
                P.dve(lambda e: e.tensor_copy(idi[:], idf[:]), ["idf"], [kI])
                P.dve(lambda e: e.tensor_tensor(out=wsm[:], in0=bs[:], in1=bs[:, :, 0:1].to_broadcast([128, 8, 16]), op=ALU.subtract), ["bs"], [kW])
                P.act(lambda e: e.activation(out=wsm[:], in_=wsm[:], func=AF.Exp), [kW], [kW])
                P.dve(lambda e: e.tensor_reduce(out=wsum[:], in_=wsm[:], axis=AX.X, op=ALU.add), [kW], ["wsum"])
                P.dve(lambda e: e.reciprocal(out=wsum[:], in_=wsum[:]), ["wsum"], ["wsum"])
                P.dve(lambda e: e.tensor_tensor(out=wsm[:], in0=wsm[:], in1=wsum[:].unsqueeze(2).to_broadcast([128, 8, 16]), op=ALU.mult),
                      [kW, "wsum"], [kW])
                if cfg.get("peer_dump") and g == 0 and t == 0:
                    for nm, t_, shp, dt_ in ((kI, idi, [128, 128], I32), (kW, wsm, [128, 8, 16], F32), ("bs", bs, [128, 8, 16], F32),
                                             ("tv", tv, [128, 16, 16], F32), ("tif", tif, [128, 16, 16], F32)):
                        o = dout("dump_" + nm, shp, dt_)
                        P.dma("sp", lambda e, o=o, t_=t_: e.dma_start(out=o, in_=t_[:]), [kI, kW, "bs", "tv", "tif"], [])
            def stage_u(t):
                ti_ = g * 2 + t
                r0 = ti_ * 128
                hk = "hf%d" % t
                idi, wsm, apre, coef = idi2[t], wsm2[t], apre2[t], coef2[t]
                kI, kW, kA, kC = "idi%d" % t, "wsm%d" % t, "apre%d" % t, "coef%d" % t
                nslot = cfg.get("peer_slots", 128)
                nslot = cfg.get("peer_slots", 128)
                for s_ in range(nslot):
                    Gb, Gk = Gs[s_ % NGB], "G%d" % (s_ % NGB)
                    P.dma("pool", lambda e, Gb=Gb, s_=s_: e.indirect_dma_start(out=Gb[:], out_offset=None, in_=pub,
                                                                               in_offset=bass.IndirectOffsetOnAxis(ap=idi[:, s_:s_ + 1], axis=0)),
                          [kI] + TABKEYS, [Gk])
                    P.dve(lambda e, Gb=Gb, s_=s_, t=t: e.scalar_tensor_tensor(out=junk[:], in0=Gb[:], scalar=1.0, in1=hf[t][:], op0=ALU.mult, op1=ALU.mult,
                                                                              accum_out=apre[:, s_:s_ + 1]), [Gk, hk], ["pjunk", kA])
                P.act(lambda e: e.activation(out=coef[:, 0:nslot], in_=apre[:, 0:nslot], func=AF.Gelu), [kA], [kC])
                P.dve(lambda e: e.tensor_tensor(out=coef[:, 0:nslot], in0=coef[:, 0:nslot], in1=wsm[:].rearrange("p a b -> p (a b)")[:, 0:nslot], op=ALU.mult),
                      [kC, kW], [kC])
            def stage_v(t):
                ti_ = g * 2 + t
                r0 = ti_ * 128
                hk = "hf%d" % t
                idi, wsm, apre, coef = idi2[t], wsm2[t], apre2[t], coef2[t]
                kI, kW, kA, kC = "idi%d" % t, "wsm%d" % t, "apre%d" % t, "coef%d" % t
                nslot = cfg.get("peer_slots", 128)
                P.bank_lo, P.bank_n = 0, 4
                for s_ in range(nslot):
                    Gb, Gk = Gs[s_ % NGB], "G%d" % (s_ % NGB)
                    dg, dgk = dgs[s_ % 4], "dg%d" % (s_ % 4)
                    P.dma("pool", lambda e, Gb=Gb, s_=s_: e.indirect_dma_start(out=Gb[:], out_offset=None, in_=pvb,
                                                                               in_offset=bass.IndirectOffsetOnAxis(ap=idi[:, s_:s_ + 1], axis=0)),
                          [kI] + TABKEYS, [Gk])
                    P.act(lambda e, dg=dg, s_=s_: e.activation(out=dg[:], in_=identf, func=AF.Copy, scale=coef[:, s_:s_ + 1]), [kC, "cst"], [dgk])
                    for cb in range(4):
                        P.pe(lambda e, Gb=Gb, dg=dg, cb=cb, s_=s_: e.matmul(banks[4 + cb][:, :], lhsT=dg[:], rhs=Gb[:, cb * 512:(cb + 1) * 512],
                                                                             start=(s_ == 0), stop=(s_ == nslot - 1)), [Gk, dgk], ["bank%d" % (4 + cb)])
                xt = hf[t]
                load_x(xt, hk, ti_, r0)
                for cb in range(4):
                    P.dve(lambda e, cb=cb: e.tensor_tensor(out=work[:, 0:512], in0=banks[4 + cb][:, :], in1=modt[2][:, cb * 512:(cb + 1) * 512], op=ALU.mult),
                          ["bank%d" % (4 + cb), "mod2"], ["work"])
                    P.dve(lambda e, xt=xt, cb=cb: e.tensor_add(xt[:, cb * 512:(cb + 1) * 512], xt[:, cb * 512:(cb + 1) * 512], work[:, 0:512]), ["work", hk], [hk])
                if last:
                    rmsnorm_tile(xt, hk, fnw, "fnw", None, None, junk)
                    P.dma("sp", lambda e, xt=xt, r0=r0: e.dma_start(out=y_out[r0:r0 + 128, :], in_=xt[:]), [hk], ["y_out"])
                else:
                    P.dma("sp", lambda e, xt=xt, r0=r0: e.dma_start(out=xres[r0:r0 + 128, :], in_=xt[:]), [hk], ["xres%d" % ti_])
            for t in range(2):
                stage_c(t)
            for t in range(2):
                stage_u(t)
            for t in range(2):
                stage_v(t)

    dbg_outs = {}
    if phases in ("p1",):
        l = 0
        prepass(l)
        phase1(l, x_in, "x_in")
        for nm, src, shape, dt in (("d_Hs", Hs, [KC, 128, S], BF16), ("d_Yp", Yp, [8, 128, S], BF16),
                                   ("d_DNq", DNq, [8, 128, S], F32), ("d_DNk", DNk, [8, 128, S], F32), ("d_DNv", DNv, [8, 128, S], F32),
                                   ("d_Qc", Qc, [8, 128, S], BF16), ("d_Kc", Kc, [8, 128, S], BF16)):
            o = dout(nm, shape, dt)
            for a in range(shape[0]):
                P.dma("sp", lambda e, o=o, src=src, a=a: e.dma_start(out=o[a], in_=src[a]),
                      ["Hs", "Yp", "DN0", "DN1", "DN2", "QK0", "QK1"], [])
        o = dout("d_Gdn", [S, 1024], BF16)
        P.dma("sp", lambda e, o=o: e.dma_start(out=o[:, :], in_=Gdn[:, :]), ["Gdn"], [])
        o = dout("d_Vc", [128, 32, 1024], BF16)
        P.dma("sp", lambda e, o=o: e.dma_start(out=o[:, :, :], in_=Vc[:, :, :]), ["Vc"], [])
        o = dout("d_sg", [128, 32, 24], F32)
        P.dma("sp", lambda e, o=o: e.dma_start(out=o[:, :, :], in_=sg[:]), ["sg"], [])

    if phases == "dn":
        phase_dn(0)
        o = dout("d_Yd", [8, 128, S], BF16)
        for a in range(cfg.get("dn_heads", 8) if cfg.get("dn_stage", 9) >= 3 else 0):
            P.dma("sp", lambda e, o=o, a=a: e.dma_start(out=o[a], in_=Yd[a]), ["Yd"], [])

    if phases == "fox":
        phase_fox(0)
        o = dout("d_Yf", [8, 128, S], BF16)
        for a in range(cfg.get("fox_heads", 8)):
            P.dma("sp", lambda e, o=o, a=a: e.dma_start(out=o[a], in_=Yf[a]), ["Yf"], [])

    if phases == "merge":
        if "winb" not in feed:
            prepass(0)
        phase_merge(0, x_in, "x_in")
        o = dout("d_xres", [S, D])
        ng = cfg.get("mg_groups", NG)
        P.dma("sp", lambda e, o=o: e.dma_start(out=o[0:ng * TG, :], in_=xres[0:ng * TG, :]), ["xres%d" % i for i in range(ng * 4)], [])

    if phases == "peer":
        convert_tables()
        phase_peer(0, cfg.get("peer_last", False))
        if not cfg.get("peer_last", False):
            o = dout("d_xres", [S, D])
            ng = cfg.get("peer_groups", S // 256)
            P.dma("sp", lambda e, o=o: e.dma_start(out=o[0:ng * 256, :], in_=xres[0:ng * 256, :]), ["xres%d" % i for i in range(ng * 2)], [])

    if phases == "all":
        convert_tables()
        for l in layers:
            xsrc, xkey = (x_in, "x_in") if l == 0 else (xres, "xres")
            prepass(l)
            phase1(l, xsrc, xkey)
            phase_dn(l)
            phase_fox(l)
            phase_merge(l, xsrc, xkey)
            phase_peer(l, l == NL - 1)

    P.limit = None
    P.finish()
    print("ops", P.n_ops, "sbuf bytes/partition", P.sb_bytes)
    P.emit()
    return nc


def prep_shared(inputs):
    f = lambda a: np.ascontiguousarray(np.asarray(a, dtype=np.float32))
    m = {}
    for k in ("ada_w", "ada_b", "norm_mix_w", "norm_ffn_w", "w_in", "pool_w", "w_branch_pool", "w_branch_dn",
              "w_branch_fox", "w_out", "peer_w_q", "peer_u", "peer_v"):
        m[k] = f(inputs[k])
    m["final_norm_w"] = f(inputs["final_norm_w"]).reshape(1, D)
    m["pool_scale_t"] = f(np.asarray(inputs["pool_scale"]).reshape(NL, 8, 128).transpose(0, 2, 1))
    cw = np.asarray(inputs["dn_conv_w"]).reshape(NL, 4, 24, 128)
    m["conv_t"] = f(cw.transpose(0, 3, 2, 1).reshape(NL, 128, 96))
    bc = lambda a: f(np.broadcast_to(np.asarray(a)[:, None, :], (NL, 128, np.asarray(a).shape[-1])))
    m["a_log_bc"] = bc(inputs["dn_a_log"])
    m["dt_bias_bc"] = bc(inputs["dn_dt_bias"])
    m["f_bias_bc"] = bc(inputs["fox_f_bias"])
    m["onorm_bc"] = bc(inputs["dn_onorm_w"])
    sk = np.asarray(inputs["peer_sub_keys"])
    m["skT"] = f(sk.reshape(NL, 16, 128, 128).transpose(0, 1, 3, 2))
    m["consts"] = make_consts()
    return m


def prep_core(inputs, shared, b, core=0):
    m = dict(shared)
    half = 0 if core < 4 else 1
    tiles = (np.arange(S // 128) + half * (S // 256)) % (S // 128)
    m["rowidx"] = np.ascontiguousarray((tiles[None, :] * 128 + np.arange(128)[:, None]).astype(np.int32))
    m["x"] = np.ascontiguousarray(np.asarray(inputs["x"][b], dtype=np.float32))
    m["cT"] = np.ascontiguousarray(np.asarray(inputs["c"][b], dtype=np.float32).reshape(KC, 128).T)
    return m


_NC_CACHE = {}


def kernel(**inputs):
    if "nc" not in _NC_CACHE:
        _NC_CACHE["nc"] = build({"phases": "all"})
    nc = _NC_CACHE["nc"]
    shared = prep_shared(inputs)
    in_maps = [prep_core(inputs, shared, i % 4, core=i) for i in range(8)]
    res = run_bass_kernel_spmd(nc, in_maps, core_ids=list(range(8)))
    out = np.empty((4, S, D), np.float32)
    for b in range(4):
        out[b, :S // 2] = np.asarray(res.results[b]["y"], dtype=np.float32)
        out[b, S // 2:] = np.asarray(res.results[b + 4]["y"], dtype=np.float32)
    return out
```

```python
import numpy as np
from contextlib import ExitStack
import concourse.bass as bass
import concourse.mybir as mybir
from concourse.bass_utils import run_bass_kernel_spmd

F32 = mybir.dt.float32
BF16 = mybir.dt.bfloat16
I32 = mybir.dt.int32
U32 = mybir.dt.uint32
ALU = mybir.AluOpType
AF = mybir.ActivationFunctionType
AX = mybir.AxisListType

ENGS = ("pe", "act", "dve", "pool", "sp")


class Prog:
    def __init__(self, nc, dma_sems=None):
        self.nc = nc
        self.streams = {e: [] for e in ENGS}
        self.count = {}
        self.waited = {e: {} for e in ENGS}
        self.res_w = {}
        self.res_r = {}
        self.dma_sems = dma_sems or {"sp": 8, "act": 6, "pool": 12}
        self.dma_rr = {q: 0 for q in self.dma_sems}
        self.es = ExitStack()
        self.n_ops = 0
        self.sb_bytes = 0
        self.nbank = 0
        self.limit = None

    def sb(self, name, shape, dt):
        n = 1
        for s in shape[1:]:
            n *= s
        self.sb_bytes += n * (2 if dt == BF16 else 4)
        return self.es.enter_context(self.nc.sbuf_tensor(name, list(shape), dt))

    def ps(self, name, shape, dt):
        return self.es.enter_context(self.nc.psum_tensor(name, list(shape), dt))

    def _deps(self, reads, writes):
        deps = {}

        def add(k, v):
            if deps.get(k, 0) < v:
                deps[k] = v
        for r in reads:
            if r in self.res_w:
                add(*self.res_w[r])
        for w in writes:
            if w in self.res_w:
                add(*self.res_w[w])
            for k, v in self.res_r.get(w, {}).items():
                add(k, v)
        return deps

    def _emit_waits(self, eng, deps):
        for k, v in deps.items():
            if k == eng and eng == "pe":
                continue
            if self.waited[eng].get(k, 0) >= v:
                continue
            self.streams[eng].append(("wait", k, v))
            self.waited[eng][k] = v

    def _mark(self, key, val, reads, writes):
        for r in reads:
            d = self.res_r.setdefault(r, {})
            if d.get(key, 0) < val:
                d[key] = val
        for w in writes:
            self.res_w[w] = (key, val)
            self.res_r[w] = {}

    def op(self, eng, fn, reads=(), writes=()):
        if self.limit is not None and self.n_ops >= self.limit:
            return
        self._emit_waits(eng, self._deps(reads, writes))
        v = self.count.get(eng, 0) + 1
        self.count[eng] = v
        self.streams[eng].append(("op", fn, eng, 1))
        self._mark(eng, v, reads, writes)
        self.n_ops += 1

    def pe(self, fn, r=(), w=()):
        self.op("pe", fn, r, w)

    def act(self, fn, r=(), w=()):
        self.op("act", fn, r, w)

    def dve(self, fn, r=(), w=()):
        self.op("dve", fn, r, w)

    def pool(self, fn, r=(), w=()):
        self.op("pool", fn, r, w)

    def dma(self, q, fn, reads=(), writes=()):
        if self.limit is not None and self.n_ops >= self.limit:
            return
        deps = self._deps(reads, writes)
        k = self.dma_rr[q] % self.dma_sems[q]
        self.dma_rr[q] += 1
        key = ("dma", q, k)
        prev = self.count.get(key, 0)
        if prev:
            deps[key] = max(deps.get(key, 0), prev)
        self._emit_waits(q, deps)
        v = prev + 16
        self.count[key] = v
        self.streams[q].append(("op", fn, key, 16))
        self._mark(key, v, reads, writes)
        self.n_ops += 1

    def barrier(self):
        for e in ENGS:
            for k, v in self.count.items():
                if k == e and e == "pe":
                    continue
                if self.waited[e].get(k, 0) < v:
                    self.streams[e].append(("wait", k, v))
                    self.waited[e][k] = v

    def finish(self):
        for k, v in self.count.items():
            if self.waited["sp"].get(k, 0) < v:
                self.streams["sp"].append(("wait", k, v))
                self.waited["sp"][k] = v

    def emit(self):
        nc = self.nc
        sems = {}
        for k in self.count:
            nm = k if isinstance(k, str) else "d_%s_%d" % (k[1], k[2])
            sems[k] = self.es.enter_context(nc.semaphore("s_" + nm))
        block = self.es.enter_context(nc.Block())

        def run(ename):
            def f(e):
                for it in self.streams[ename]:
                    if it[0] == "wait":
                        e.wait_ge(sems[it[1]], it[2])
                    else:
                        it[1](e).then_inc(sems[it[2]], it[3])
            return f
        block.tensor(run("pe"))
        block.scalar(run("act"))
        block.vector(run("dve"))
        block.gpsimd(run("pool"))
        block.sync(run("sp"))
        self.es.close()


class Arena:
    def __init__(self, t, nwords):
        self.t, self.n, self.off, self.gen = t, nwords, 0, 0

    def reset(self):
        self.off = 0
        self.gen += 1

    def alloc(self, name, free_shape, dt):
        n = 1
        for d_ in free_shape:
            n *= d_
        words = n if dt in (F32, I32, U32) else (n + 1) // 2
        words = (words + 3) // 4 * 4
        assert self.off + words <= self.n, ("arena overflow", name, self.off, words, self.n)
        ap = self.t[:, self.off:self.off + words]
        self.off += words
        if dt != F32:
            ap = ap.bitcast(dt)
        ap = ap[:, 0:n]
        if len(free_shape) > 1:
            names = ["a%d" % i for i in range(len(free_shape))]
            pat = "p (%s) -> p %s" % (" ".join(names), " ".join(names))
            ap = ap.rearrange(pat, **{nm: d_ for nm, d_ in zip(names[1:], free_shape[1:])})
        return ap


S = 4096
D = 2048
TG = 512
NG = S // TG
KC = 16
NL = 2
EPS = 1e-6
NW = 14360
NEXP = 16384
OFF_P, OFF_D, OFF_GD, OFF_F, OFF_SBA, OFF_V, OFF_G, OFF_SF = 0, 1024, 4096, 5120, 7168, 7184, 8208, 14352
HALF_A = 7184

C_NAMES = ["ident", "ones", "tri128", "triblk", "strictU", "blkones", "selA", "selB", "selrow0"]
C_OFF = {n: i * 128 for i, n in enumerate(C_NAMES)}
C_MASKA = len(C_NAMES) * 128
C_MASKB = C_MASKA + 1
C_IOTA16 = C_MASKB + 1
C_INVCNT = C_IOTA16 + 16
C_TOTAL = C_INVCNT + 64


def make_consts():
    c = np.zeros((128, C_TOTAL), np.float32)
    m = np.arange(128)[:, None]
    i = np.arange(128)[None, :]
    same = (m // 64) == (i // 64)
    mats = {
        "ident": (m == i), "ones": np.ones((128, 128), bool), "tri128": (m <= i),
        "triblk": (m <= i) & same, "strictU": (m > i) & same, "blkones": same,
        "selA": (m < 64) & (i >= 0), "selB": (m >= 64) & (i >= 0), "selrow0": (m == 0) & (i >= 0),
    }
    for n in C_NAMES:
        c[:, C_OFF[n]:C_OFF[n] + 128] = mats[n].astype(np.float32)
    c[:, C_MASKA] = (np.arange(128) < 64)
    c[:, C_MASKB] = (np.arange(128) >= 64)
    c[:, C_IOTA16:C_IOTA16 + 16] = np.arange(16)[None, :]
    for gi, w in enumerate((2, 4, 8, 16)):
        c[:, C_INVCNT + gi * 16:C_INVCNT + gi * 16 + 16] = 1.0 / np.minimum(np.arange(16) + 1, w)[None, :]
    return c


def build(cfg):
    nc = bass.Bass("TRN2", target_bir_lowering=False)
    P = Prog(nc)
    P.limit = cfg.get("max_ops")
    dbg = cfg.get("debug", {})
    phases = cfg.get("phases", "all")
    layers = cfg.get("layers", [0, 1])

    shrink = cfg.get("shrink", ())

    def din(name, shape, dt=F32):
        if name in shrink:
            return None
        return nc.dram_tensor(name, list(shape), dt, kind="ExternalInput").ap()

    feed = cfg.get("feed", ())

    def dscr(name, shape, dt):
        kind = "ExternalInput" if name in feed else "Internal"
        return nc.dram_tensor(name, list(shape), dt, kind=kind).ap()

    def dout(name, shape, dt=F32):
        return nc.dram_tensor(name, list(shape), dt, kind="ExternalOutput").ap()

    x_in = din("x", [S, D])
    cT_in = din("cT", [128, KC])
    ada_w = din("ada_w", [NL, D, 6 * D])
    ada_b = din("ada_b", [NL, 6 * D])
    norm_mix_w = din("norm_mix_w", [NL, D])
    norm_ffn_w = din("norm_ffn_w", [NL, D])
    final_norm_w = din("final_norm_w", [1, D])
    w_in = din("w_in", [NL, D, NW])
    pool_w = din("pool_w", [NL, 4, 256, 256])
    pool_scale_t = din("pool_scale_t", [NL, 128, 8])
    conv_t = din("conv_t", [NL, 128, 24 * 4])
    a_log_bc = din("a_log_bc", [NL, 128, 8])
    dt_bias_bc = din("dt_bias_bc", [NL, 128, 8])
    f_bias_bc = din("f_bias_bc", [NL, 128, 8])
    onorm_bc = din("onorm_bc", [NL, 128, 128])
    w_br = [din("w_branch_pool", [NL, 1024, D]), din("w_branch_dn", [NL, 1024, D]), din("w_branch_fox", [NL, 1024, D])]
    if cT_in is None:
        cT_in = din("cT_dummy", [128, KC])
    w_out = din("w_out", [NL, D, D])
    peer_w_q = din("peer_w_q", [NL, D, D])
    skT = din("skT", [NL, 16, 128, 128])
    peer_u = din("peer_u", [NL, NEXP, D])
    peer_v = din("peer_v", [NL, NEXP, D])
    consts_in = din("consts", [128, C_TOTAL])
    rowidx_in = din("rowidx", [128, S // 128], I32)
    SPLIT = cfg.get("split_last", True)
    y_out = dout("y", [S // 2 if SPLIT else S, D])

    winb = dscr("winb", [D, NW], BF16)
    wbrb = dscr("wbrb", [3, 1024, D], BF16)
    woutb = dscr("woutb", [D, D], BF16)
    xres = dscr("xres", [S, D], F32)
    for i_ in range(S // 128):
        if "xres" in feed:
            P.res_w.pop("xres%d" % i_, None)
    Hs = dscr("Hs", [KC, 128, S], BF16)
    Yp = dscr("Yp", [8, 128, S], BF16)
    Yd = dscr("Yd", [8, 128, S], BF16)
    Yf = dscr("Yf", [8, 128, S], BF16)
    DNq = dscr("DNq", [8, 128, S], F32)
    DNk = dscr("DNk", [8, 128, S], F32)
    DNv = dscr("DNv", [8, 128, S], F32)
    Gdn = dscr("Gdn", [S, 1024], BF16)
    Qc = dscr("Qc", [8, 128, S], BF16)
    Kc = dscr("Kc", [8, 128, S], BF16)
    Vc = dscr("Vc", [128, S // 128, 1024], BF16)

    NBANK = cfg.get("nbank", 8)
    banks = [P.ps("bank%d" % i, [128, 512], F32) for i in range(8)]

    P.bank_lo, P.bank_n = 0, NBANK

    def bank():
        i = P.bank_lo + P.nbank % P.bank_n
        P.nbank += 1
        return banks[i], "bank%d" % i

    cst = P.sb("cst", [128, C_TOTAL], F32)
    P.dma("sp", lambda e: e.dma_start(out=cst[:], in_=consts_in[:, :]), [], ["cst"])

    def cm(name):
        return cst[:, C_OFF[name]:C_OFF[name] + 128]
    ridx = P.sb("ridx", [128, S // 128], I32)
    if rowidx_in is not None:
        P.dma("sp", lambda e: e.dma_start(out=ridx[:], in_=rowidx_in[:, :]), [], ["ridx"])
    ident_bf = P.sb("ident_bf", [128, 128], BF16)
    ones_bf = P.sb("ones_bf", [128, 128], BF16)
    P.dve(lambda e: e.tensor_copy(ident_bf[:], cm("ident")), ["cst"], ["ident_bf"])
    P.dve(lambda e: e.tensor_copy(ones_bf[:], cm("ones")), ["cst"], ["ones_bf"])

    ct = P.sb("ct", [128, KC], F32)
    csbc = P.sb("csbc", [128, KC, 128], F32)
    P.dma("sp", lambda e: e.dma_start(out=ct[:], in_=cT_in[:, :]), [], ["ct"])
    P.act(lambda e: e.activation(out=ct[:], in_=ct[:], func=AF.Silu), ["ct"], ["ct"])
    P.dve(lambda e: e.tensor_copy(csbc[:], ct[:].unsqueeze(2).to_broadcast([128, KC, 128])), ["ct"], ["csbc"])

    stg = P.sb("stg", [128, 4096], F32)
    modt = [P.sb("mod%d" % i, [128, D], F32) for i in range(3)]
    stat = P.sb("stat", [128, 8], F32)
    sm = P.sb("sm", [128, 128], F32)
    sg = P.sb("sg", [128, S // 128, 24], F32)
    if "sg_in" in feed:
        sg_in = din("sg_in", [128, S // 128, 24])
        P.dma("sp", lambda e: e.dma_start(out=sg[:], in_=sg_in[:, :, :]), [], ["sg"])
    ARW = cfg.get("arw", 37888)
    arena_t = P.sb("arena", [128, ARW], F32)
    AR = Arena(arena_t, ARW)

    def new_phase():
        P.barrier()
        AR.reset()
        P.bank_lo, P.bank_n = 0, NBANK

    def mod_vector(l, j, dst, dst_key, plus_one=False, mul_row=None):
        stv = stg[:, 0:KC * 256].rearrange("p (k c) -> p k c", k=KC)
        for cb in range(8):
            c0 = j * D + cb * 256
            P.dma("sp", lambda e, c0=c0: e.dma_start(
                out=stv, in_=ada_w[l, :, c0:c0 + 256].rearrange("(k p) c -> p k c", p=128)), [], ["stg"])
            bk, bkk = bank()
            for k in range(KC):
                P.pe(lambda e, bk=bk, k=k: e.matmul(bk[:, 0:256], lhsT=csbc[:, k, :], rhs=stv[:, k, :],
                                                    start=(k == 0), stop=(k == KC - 1)), ["csbc", "stg"], [bkk])
            P.act(lambda e, bk=bk, cb=cb: e.copy(out=dst[:, cb * 256:(cb + 1) * 256], in_=bk[:, 0:256]), [bkk], [dst_key])
        P.dma("sp", lambda e: e.dma_start(out=stg[:, 0:D], in_=ada_b[l:l + 1, j * D:(j + 1) * D].to_broadcast([128, D])),
              [], ["stg"])
        P.dve(lambda e: e.tensor_add(dst[:], dst[:], stg[:, 0:D]), ["stg", dst_key], [dst_key])
        if plus_one:
            P.dma("sp", lambda e: e.dma_start(out=stg[:, D:2 * D], in_=mul_row.to_broadcast([128, D])), [], ["stg"])
            P.dve(lambda e: e.scalar_tensor_tensor(out=dst[:], in0=dst[:], scalar=1.0, in1=stg[:, D:2 * D],
                                                   op0=ALU.add, op1=ALU.mult), ["stg", dst_key], [dst_key])

    def cast_rows(src2d, dst2d, ncols, segs, key_dst, pst, pstb):
        rows = src2d.shape[0]
        for r0 in range(0, rows, 128):
            P.dma("sp", lambda e, r0=r0: e.dma_start(out=pst[:, 0:ncols], in_=src2d[r0:r0 + 128, :]), [], ["pst"])
            for si, (ov, iv) in enumerate(segs):
                eng = ("act", "dve", "pool")[si % 3]
                if eng == "act":
                    P.act(lambda e, ov=ov, iv=iv: e.copy(out=ov(pstb), in_=iv(pst)), ["pst"], ["pstb"])
                elif eng == "dve":
                    P.dve(lambda e, ov=ov, iv=iv: e.tensor_copy(ov(pstb), iv(pst)), ["pst"], ["pstb"])
                else:
                    P.pool(lambda e, ov=ov, iv=iv: e.tensor_copy(ov(pstb), iv(pst)), ["pst"], ["pstb"])
            P.dma("sp", lambda e, r0=r0: e.dma_start(out=dst2d[r0:r0 + 128, :], in_=pstb[:, 0:ncols]), ["pstb"], [key_dst])

    def prepass(l):
        new_phase()
        pst = AR.alloc("pst", [HALF_A], F32)
        pstb = AR.alloc("pstb", [HALF_A], BF16)
        segsA = [
            (lambda o: o[:, 0:1024], lambda i: i[:, 0:1024]),
            (lambda o: o[:, OFF_D:OFF_D + 3072].rearrange("p (h t c) -> p h t c", h=8, t=3),
             lambda i: i[:, 1024:4096].rearrange("p (t h c) -> p h t c", t=3, h=8)),
            (lambda o: o[:, OFF_GD:OFF_GD + 1024], lambda i: i[:, 4112:5136]),
            (lambda o: o[:, OFF_F:OFF_F + 2048].rearrange("p (h t c) -> p h t c", h=8, t=2),
             lambda i: i[:, 5136:7184].rearrange("p (t h c) -> p h t c", t=2, h=8)),
            (lambda o: o[:, OFF_SBA:OFF_SBA + 16], lambda i: i[:, 4096:4112]),
        ]
        cast_rows(w_in[l, :, 0:HALF_A], winb[:, 0:HALF_A], HALF_A, segsA, "winb", pst, pstb)
        nB = NW - HALF_A
        segsB = [
            (lambda o: o[:, 0:1024], lambda i: i[:, 0:1024]),
            (lambda o: o[:, 1024:1024 + 6144].rearrange("p (j b c) -> p j b c", j=16, b=3),
             lambda i: i[:, 1032:1032 + 6144].rearrange("p (b j c) -> p j b c", b=3, j=16)),
            (lambda o: o[:, 7168:7176], lambda i: i[:, 1024:1032]),
        ]
        cast_rows(w_in[l, :, HALF_A:NW], winb[:, HALF_A:NW], nB, segsB, "winb", pst, pstb)
        plain = [(lambda o: o[:, 0:1024], lambda i: i[:, 0:1024]), (lambda o: o[:, 1024:2048], lambda i: i[:, 1024:2048])]
        for b in range(3):
            cast_rows(w_br[b][l], wbrb[b], 2048, plain, "wbrb", pst, pstb)
        cast_rows(w_out[l], woutb, 2048, plain, "woutb", pst, pstb)

    def load_wt(wts, src2d, c0, n, srckey, pref="wt"):
        i = P.wt_rr % len(wts)
        P.wt_rr += 1
        t, k = wts[i], "%s%d" % (pref, i)
        P.dma("sp", lambda e: e.dma_start(out=t[:, :, 0:n], in_=src2d[:, c0:c0 + n].rearrange("(k p) c -> p k c", p=128)),
              [srckey], [k])
        return t, k
    P.wt_rr = 0

    def rmsnorm_tile(xt, xtk, gain, gaink, shift, shiftk, junk):
        P.act(lambda e: e.activation(out=junk[:], in_=xt[:], func=AF.Square, accum_out=stat[:, 0:1]), [xtk], ["hb", "pjunk", "stat"])
        P.act(lambda e: e.activation(out=stat[:, 1:2], in_=stat[:, 0:1], func=AF.Sqrt, scale=1.0 / D, bias=EPS), ["stat"], ["stat"])
        P.dve(lambda e: e.reciprocal(out=stat[:, 2:3], in_=stat[:, 1:2]), ["stat"], ["stat"])
        P.dve(lambda e: e.scalar_tensor_tensor(out=xt[:], in0=xt[:], scalar=stat[:, 2:3], in1=gain[:],
                                               op0=ALU.mult, op1=ALU.mult), [xtk, "stat", gaink], [xtk])
        if shift is not None:
            P.dve(lambda e: e.tensor_add(xt[:], xt[:], shift[:]), [xtk, shiftk], [xtk])

    HT = {}

    def proj_fm(wt, wtk, c0, n=TG):
        hT = HT["hT"]
        bk, bkk = bank()
        for k in range(KC):
            P.pe(lambda e, k=k: e.matmul(bk[:, 0:n], lhsT=wt[:, k, c0:c0 + 128], rhs=hT[:, k, 0:n],
                                         start=(k == 0), stop=(k == KC - 1)), [wtk, "hT"], [bkk])
        return bk, bkk

    def proj_tm(wt, wtk, t, c0, n, bk=None, bkk=None, o0=0):
        hT = HT["hT"]
        if bk is None:
            bk, bkk = bank()
        for k in range(KC):
            P.pe(lambda e, k=k: e.matmul(bk[:, o0:o0 + n], lhsT=hT[:, k, t * 128:(t + 1) * 128], rhs=wt[:, k, c0:c0 + n],
                                         start=(k == 0), stop=(k == KC - 1)), [wtk, "hT"], [bkk])
        return bk, bkk

    def phase1(l, xsrc, xsrc_key):
        new_phase()
        hT = AR.alloc("hT", [KC, TG], BF16)
        HT["hT"] = hT
        wts = [AR.alloc("wt%d" % i, [KC, 512], BF16) for i in range(2)]
        xts = [AR.alloc("xt%d" % i, [D], F32) for i in range(2)]
        hb = AR.alloc("hb", [D], BF16)
        pin = AR.alloc("pin", [8, 15 + TG], F32)
        pacc = [AR.alloc("pacc%d" % i, [15 + TG], F32) for i in range(2)]
        pooled = AR.alloc("pooled", [8, TG], BF16)
        ypT = AR.alloc("ypT", [8, TG], BF16)
        pwb = AR.alloc("pwb", [8, 256], BF16)
        psc = AR.alloc("psc", [8], F32)
        cvw = AR.alloc("cvw", [24 * 4], F32)
        cin = AR.alloc("cin", [3, 3 + TG], F32)
        ctail = AR.alloc("ctail", [24, 3], F32)
        cacc = AR.alloc("cacc", [3, TG], F32)
        csq = AR.alloc("csq", [TG], F32)
        crn = AR.alloc("crn", [TG], F32)
        tm_bf = AR.alloc("tm_bf", [4, 1024], BF16)
        fqk = AR.alloc("fqk", [2, TG], BF16)
        junk = sm
        P.dma("sp", lambda e: e.dma_start(out=stg[:, 0:2048].rearrange("p (a c) -> p a c", a=8),
                                          in_=pool_w[l].rearrange("g (cc p) d -> p (g cc) d", p=128)), [], ["stg"])
        P.dve(lambda e: e.tensor_copy(pwb[:], stg[:, 0:2048].rearrange("p (a c) -> p a c", a=8)), ["stg"], ["pwb"])
        P.dma("sp", lambda e: e.dma_start(out=psc[:], in_=pool_scale_t[l]), [], ["psc"])
        P.dma("sp", lambda e: e.dma_start(out=cvw[:], in_=conv_t[l]), [], ["cvw"])
        P.pool(lambda e: e.memset(pin[:], 0.0), [], ["pin"])
        P.pool(lambda e: e.memset(ctail[:], 0.0), [], ["ctail"])
        mod_vector(l, 1, modt[0], "mod0", plus_one=True, mul_row=norm_mix_w[l:l + 1, :])
        mod_vector(l, 0, modt[1], "mod1")
        for g in range(NG):
            for t in range(4):
                xt, xtk = xts[t % 2], "xt%d" % (t % 2)
                r0 = g * TG + t * 128
                P.dma("sp", lambda e, xt=xt, r0=r0: e.dma_start(out=xt[:], in_=xsrc[r0:r0 + 128, :]), [xsrc_key + str(g * 4 + t)], [xtk])
                rmsnorm_tile(xt, xtk, modt[0], "mod0", modt[1], "mod1", hb)
                P.pool(lambda e, xt=xt: e.tensor_copy(hb[:], xt[:]), [xtk], ["hb"])
                for half in range(2):
                    bk, bkk = bank()
                    bkb = bk[:, :].bitcast(BF16)
                    for kk in range(8):
                        k = half * 8 + kk
                        P.pe(lambda e, bkb=bkb, kk=kk, k=k: e.transpose(out=bkb[:, kk * 128:(kk + 1) * 128],
                                                                        in_=hb[:, k * 128:(k + 1) * 128], identity=ident_bf[:]),
                             ["hb", "ident_bf"], [bkk])
                    P.act(lambda e, bkb=bkb, half=half, t=t: e.copy(
                        out=hT[:, half * 8:half * 8 + 8, t * 128:(t + 1) * 128],
                        in_=bkb[:, 0:1024].rearrange("p (k c) -> p k c", k=8)), [bkk], ["hT"])
            P.dma("sp", lambda e, g=g: e.dma_start(out=Hs[:, :, g * TG:(g + 1) * TG].rearrange("k p t -> p k t"), in_=hT[:]),
                  ["hT"], ["Hs"])
            for wi in range(2):
                wt, wtk = load_wt(wts, winb, OFF_P + wi * 512, 512, "winb")
                for cc in range(4):
                    ch = wi * 4 + cc
                    bk, bkk = proj_fm(wt, wtk, cc * 128)
                    P.act(lambda e, bk=bk, ch=ch: e.copy(out=pin[:, ch, 15:15 + TG], in_=bk[:, :]), [bkk], ["pin"])
            for ch in range(8):
                gi = ch // 2
                w = (2, 4, 8, 16)[gi]
                src = pin[:, ch, :]
                srck = "pin"
                sh = 1
                for lvl in range(gi + 1):
                    dst, dstk = pacc[lvl % 2], "pacc%d" % (lvl % 2)
                    n = 15 + TG - sh
                    P.pool(lambda e, dst=dst, src=src, sh=sh, n=n: e.tensor_tensor(
                        out=dst[:, sh:sh + n], in0=src[:, sh:sh + n], in1=src[:, 0:n], op=ALU.add), [srck], [dstk])
                    src, srck = dst[:, :], dstk
                    sh *= 2
                P.dve(lambda e, src=src, ch=ch, w=w: e.scalar_tensor_tensor(
                    out=pooled[:, ch, :], in0=src[:, 15:15 + TG], scalar=1.0 / w, in1=pin[:, ch, 15:15 + TG],
                    op0=ALU.mult, op1=ALU.subtract), [srck, "pin"], ["pooled"])
                if g == 0:
                    P.dve(lambda e, src=src, gi=gi: e.tensor_tensor(
                        out=junk[:, 0:16], in0=src[:, 15:31], in1=cst[:, C_INVCNT + gi * 16:C_INVCNT + gi * 16 + 16],
                        op=ALU.mult), [srck, "cst"], ["junk"])
                    P.dve(lambda e, ch=ch: e.tensor_sub(pooled[:, ch, 0:16], junk[:, 0:16], pin[:, ch, 15:31]),
                          ["junk", "pin"], ["pooled"])
            P.dve(lambda e: e.tensor_copy(junk[:, 0:120].rearrange("p (a c) -> p a c", a=8), pin[:, :, TG:TG + 15]), ["pin"], ["junk"])
            P.dve(lambda e: e.tensor_copy(pin[:, :, 0:15], junk[:, 0:120].rearrange("p (a c) -> p a c", a=8)), ["junk"], ["pin"])
            for gi in range(4):
                for dc in range(2):
                    bk, bkk = bank()
                    for cc in range(2):
                        P.pe(lambda e, bk=bk, gi=gi, dc=dc, cc=cc: e.matmul(
                            bk[:, :], lhsT=pwb[:, gi * 2 + cc, dc * 128:(dc + 1) * 128], rhs=pooled[:, gi * 2 + cc, :],
                            start=(cc == 0), stop=(cc == 1)), ["pwb", "pooled"], [bkk])
                    P.act(lambda e, bk=bk, gi=gi, dc=dc: e.activation(
                        out=ypT[:, gi * 2 + dc, :], in_=bk[:, :], func=AF.Copy, scale=psc[:, gi * 2 + dc:gi * 2 + dc + 1]),
                        [bkk, "psc"], ["ypT"])
            P.dma("sp", lambda e, g=g: e.dma_start(out=Yp[:, :, g * TG:(g + 1) * TG].rearrange("k p t -> p k t"), in_=ypT[:]),
                  ["ypT"], ["Yp"])
            for h in range(8):
                wt, wtk = load_wt(wts, winb, OFF_D + h * 384, 384, "winb")
                P.dve(lambda e, h=h: e.tensor_copy(cin[:, :, 0:3], ctail[:, h * 3:h * 3 + 3, :]), ["ctail"], ["cin"])
                for i3 in range(3):
                    bk, bkk = proj_fm(wt, wtk, i3 * 128)
                    P.act(lambda e, bk=bk, i3=i3: e.copy(out=cin[:, i3, 3:3 + TG], in_=bk[:, :]), [bkk], ["cin"])
                P.dve(lambda e, h=h: e.tensor_copy(ctail[:, h * 3:h * 3 + 3, :], cin[:, :, TG:TG + 3]), ["cin"], ["ctail"])
                for i3 in range(3):
                    ch = i3 * 8 + h
                    for j in range(4):
                        wcol = cvw[:, ch * 4 + j:ch * 4 + j + 1]
                        if j == 0:
                            P.dve(lambda e, i3=i3, wcol=wcol: e.tensor_scalar(cacc[:, i3, :], cin[:, i3, 0:TG], wcol, None, ALU.mult),
                                  ["cin", "cvw"], ["cacc"])
                        else:
                            P.dve(lambda e, i3=i3, j=j, wcol=wcol: e.scalar_tensor_tensor(
                                out=cacc[:, i3, :], in0=cin[:, i3, j:j + TG], scalar=wcol, in1=cacc[:, i3, :],
                                op0=ALU.mult, op1=ALU.add), ["cin", "cvw", "cacc"], ["cacc"])
                    P.act(lambda e, i3=i3: e.activation(out=cacc[:, i3, :], in_=cacc[:, i3, :], func=AF.Silu), ["cacc"], ["cacc"])
                    if i3 < 2:
                        P.act(lambda e, i3=i3: e.activation(out=csq[:], in_=cacc[:, i3, :], func=AF.Square), ["cacc"], ["csq"])
                        bk, bkk = bank()
                        P.pe(lambda e, bk=bk: e.matmul(bk[:, :], lhsT=cm("ones"), rhs=csq[:], start=True, stop=True), ["cst", "csq"], [bkk])
                        P.act(lambda e, bk=bk: e.activation(out=crn[:], in_=bk[:, :], func=AF.Sqrt, bias=EPS), [bkk], ["crn"])
                        P.dve(lambda e: e.reciprocal(out=crn[:], in_=crn[:]), ["crn"], ["crn"])
                        sc_ = (128.0 ** -0.5) if i3 == 0 else 1.0
                        P.dve(lambda e, i3=i3, sc_=sc_: e.scalar_tensor_tensor(
                            out=cacc[:, i3, :], in0=cacc[:, i3, :], scalar=sc_, in1=crn[:], op0=ALU.mult, op1=ALU.mult),
                            ["cacc", "crn"], ["cacc"])
                    dstT = (DNq, DNk, DNv)[i3]
                    P.dma("sp", lambda e, dstT=dstT, h=h, g=g, i3=i3: e.dma_start(
                        out=dstT[h, :, g * TG:(g + 1) * TG], in_=cacc[:, i3, :]), ["cacc"], ["DN%d" % i3])
            for blk, off in ((0, OFF_GD), (1, OFF_V)):
                for wi in range(2):
                    wt, wtk = load_wt(wts, winb, off + wi * 512, 512, "winb")
                    for t in range(4):
                        bk, bkk = proj_tm(wt, wtk, t, 0, 512)
                        if blk == 0:
                            P.act(lambda e, bk=bk, t=t, wi=wi: e.activation(out=tm_bf[:, t, wi * 512:(wi + 1) * 512], in_=bk[:, :], func=AF.Silu),
                                  [bkk], ["tm_bf"])
                        else:
                            P.act(lambda e, bk=bk, t=t, wi=wi: e.copy(out=tm_bf[:, t, wi * 512:(wi + 1) * 512], in_=bk[:, :]), [bkk], ["tm_bf"])
                if blk == 0:
                    P.dma("sp", lambda e, g=g: e.dma_start(out=Gdn[g * TG:(g + 1) * TG, :].rearrange("(t p) c -> p t c", p=128), in_=tm_bf[:]),
                          ["tm_bf"], ["Gdn"])
                else:
                    P.dma("sp", lambda e, g=g: e.dma_start(out=Vc[:, g * 4:(g + 1) * 4, :], in_=tm_bf[:]), ["tm_bf"], ["Vc"])
            wtA, wtAk = load_wt(wts, winb, OFF_SBA, 16, "winb")
            wtB, wtBk = load_wt(wts, winb, OFF_SF, 8, "winb")
            for t in range(4):
                bk, bkk = proj_tm(wtA, wtAk, t, 0, 16)
                proj_tm(wtB, wtBk, t, 0, 8, bk=bk, bkk=bkk, o0=16)
                P.act(lambda e, bk=bk, g=g, t=t: e.copy(out=sg[:, g * 4 + t, :], in_=bk[:, 0:24]), [bkk], ["sg"])
            for h in range(8):
                if h % 2 == 0:
                    wt, wtk = load_wt(wts, winb, OFF_F + h * 256, 512, "winb")
                for i2 in range(2):
                    bk, bkk = proj_fm(wt, wtk, (h % 2) * 256 + i2 * 128)
                    P.act(lambda e, bk=bk, i2=i2: e.copy(out=fqk[:, i2, :], in_=bk[:, :]), [bkk], ["fqk"])
                    dstT = (Qc, Kc)[i2]
                    P.dma("sp", lambda e, dstT=dstT, h=h, g=g, i2=i2: e.dma_start(
                        out=dstT[h, :, g * TG:(g + 1) * TG], in_=fqk[:, i2, :]), ["fqk"], ["QK%d" % i2])


    def bc_last(ap2, n):
        return ap2.unsqueeze(2).to_broadcast([128, ap2.shape[1], n])

    def bc_mid(ap2, a):
        return ap2.unsqueeze(1).to_broadcast([128, a, ap2.shape[1]])

    def v4(bk):
        return bk[:, :].rearrange("p (a c) -> p a c", a=4)

    def phase_dn(l):
        new_phase()
        NT = S // 128
        A = lambda n, sh, dt=F32: AR.alloc(n, sh, dt)
        beta, gg, gc, glt, eg, sdA, sdB, sbg, nbeta = [A(n, [NT, 8]) for n in
                                                       ("beta", "gg", "gc", "glt", "eg", "sdA", "sdB", "sbg", "nbeta")]
        glrep = A("glrep", [NT, 2, 8])
        dtb, alg = A("dtb", [8]), A("alg", [8])
        onb = A("onb", [128])
        qT, kT, vT = A("qT", [S]), A("kT", [S]), A("vT", [S])
        gtm = A("gtm", [NT, 128], BF16)
        yT = A("yT", [S], BF16)
        Sst = A("Sst", [128])
        vn = A("vn", [128])
        o1 = A("o1", [128])
        M = {n: A(n, [4, 128]) for n in ("rhsG", "Dm", "DTm", "tmpN", "Na", "Nb", "Pa", "Pb", "R", "aT", "kbg", "kdA",
                                         "kdB", "vb", "u", "wkT", "o4", "sq")}
        yb = A("yb", [4, 128], BF16)
        ssum = A("ssum", [8])
        G = ["dn_gates"]
        P.dma("sp", lambda e: e.dma_start(out=dtb[:], in_=dt_bias_bc[l]), [], ["dtb"])
        P.dma("sp", lambda e: e.dma_start(out=alg[:], in_=a_log_bc[l]), [], ["alg"])
        P.dma("sp", lambda e: e.dma_start(out=onb[:], in_=onorm_bc[l]), [], ["onb"])
        P.act(lambda e: e.activation(out=beta[:], in_=sg[:, :, 0:8], func=AF.Sigmoid), ["sg"], G)
        P.dve(lambda e: e.tensor_tensor(out=gg[:], in0=sg[:, :, 8:16], in1=bc_mid(dtb[:], NT), op=ALU.add), ["sg", "dtb"], G)
        P.act(lambda e: e.activation(out=gg[:], in_=gg[:], func=AF.Exp), G, G)
        P.act(lambda e: e.activation(out=gg[:], in_=gg[:], func=AF.Ln, bias=1.0), G, G)
        P.act(lambda e: e.activation(out=alg[:], in_=alg[:], func=AF.Exp), ["alg"], ["alg"])
        P.dve(lambda e: e.tensor_scalar(alg[:], alg[:], -1.0, None, ALU.mult), ["alg"], ["alg"])
        P.dve(lambda e: e.tensor_tensor(out=gg[:], in0=gg[:], in1=bc_mid(alg[:], NT), op=ALU.mult), G + ["alg"], G)
        gg2 = gg[:].rearrange("p a b -> p (a b)")
        for dst, cname in ((gc, "triblk"), (glt, "blkones")):
            bk, bkk = bank()
            P.pe(lambda e, bk=bk, cname=cname: e.matmul(bk[:, 0:256], lhsT=cm(cname), rhs=gg2, start=True, stop=True), ["cst"] + G, [bkk])
            P.act(lambda e, bk=bk, dst=dst: e.copy(out=dst[:].rearrange("p a b -> p (a b)"), in_=bk[:, 0:256]), [bkk], G)
        for bi, cname in ((0, "selA"), (1, "selB")):
            bk, bkk = bank()
            P.pe(lambda e, bk=bk, cname=cname: e.matmul(bk[:, 0:256], lhsT=cm(cname), rhs=gg2, start=True, stop=True), ["cst"] + G, [bkk])
            P.act(lambda e, bk=bk, bi=bi: e.activation(out=glrep[:, :, bi, :], in_=bk[:, 0:256].rearrange("p (a b) -> p a b", a=NT),
                                                       func=AF.Exp), [bkk], G)
        P.act(lambda e: e.activation(out=eg[:], in_=gc[:], func=AF.Exp), G, G)
        P.dve(lambda e: e.tensor_sub(glt[:], glt[:], gc[:]), G, G)
        P.act(lambda e: e.activation(out=glt[:], in_=glt[:], func=AF.Exp), G, G)
        P.dve(lambda e: e.tensor_scalar(sdA[:], glt[:], cst[:, C_MASKA:C_MASKA + 1], None, ALU.mult), G + ["cst"], G)
        P.dve(lambda e: e.tensor_scalar(sdB[:], glt[:], cst[:, C_MASKB:C_MASKB + 1], None, ALU.mult), G + ["cst"], G)
        P.dve(lambda e: e.tensor_mul(sbg[:], beta[:], eg[:]), G, G)
        P.dve(lambda e: e.tensor_scalar(nbeta[:], beta[:], -1.0, None, ALU.mult), G, G)
        P.pool(lambda e: e.memset(vn[:], 0.0), [], ["vn"])
        identf = cm("ident")

        for h in range(cfg.get("dn_heads", 8)):
            P.dma("sp", lambda e, h=h: e.dma_start(out=qT[:], in_=DNq[h]), ["DN0"], ["qT"])
            P.dma("sp", lambda e, h=h: e.dma_start(out=kT[:], in_=DNk[h]), ["DN1"], ["kT"])
            P.dma("sp", lambda e, h=h: e.dma_start(out=vT[:], in_=DNv[h]), ["DN2"], ["vT"])
            P.dma("sp", lambda e, h=h: e.dma_start(out=gtm[:], in_=Gdn[:, h * 128:(h + 1) * 128].rearrange("(t p) c -> p t c", p=128)),
                  ["Gdn"], ["gtm"])
            P.pool(lambda e: e.memset(Sst[:], 0.0), [], ["Sst"])
            for bt in range(cfg.get("dn_batches", S // 512)):
                t0 = bt * 4
                dn_stage = cfg.get("dn_stage", 9)
                tk = lambda i: slice((t0 + i) * 128, (t0 + i + 1) * 128)

                def mm4(lhs_fn, rhs_fn, reads, transpose=False):
                    bk, bkk = bank()
                    for i in range(4):
                        la = lhs_fn(i)
                        if transpose:
                            P.pe(lambda e, bk=bk, i=i, la=la: e.matmul(bk[:, i * 128:(i + 1) * 128], lhsT=la, rhs=identf,
                                                                       start=True, stop=True), reads + ["cst"], [bkk])
                        else:
                            ra = rhs_fn(i)
                            P.pe(lambda e, bk=bk, i=i, la=la, ra=ra: e.matmul(bk[:, i * 128:(i + 1) * 128], lhsT=la, rhs=ra,
                                                                              start=True, stop=True), reads, [bkk])
                    return bk, bkk
                if dn_stage < 1:
                    continue
                sub = cfg.get("dn_sub", 99)
                if sub < 99:
                    bKK, kKK = mm4(lambda i: kT[:, tk(i)], lambda i: kT[:, tk(i)], ["kT"])
                    if sub >= 1:
                        P.dve(lambda e, h=h, t0=t0: e.tensor_tensor(out=M["rhsG"][:], in0=bc_mid(cm("strictU"), 4),
                                                                    in1=bc_last(gg[:, t0:t0 + 4, h], 128), op=ALU.mult), ["cst"] + G, ["rhsG"])
                    if sub >= 2:
                        bDd, kDd = mm4(lambda i: cm("triblk"), lambda i: M["rhsG"][:, i, :], ["cst", "rhsG"])
                    if sub >= 3:
                        P.act(lambda e, bk=bDd: e.activation(out=M["Dm"][:], in_=v4(bk), func=AF.Exp), [kDd], ["Dm"])
                    if sub >= 4:
                        P.dve(lambda e, bk=bKK: e.tensor_tensor(out=M["Na"][:], in0=v4(bk), in1=M["Dm"][:], op=ALU.mult), [kKK, "Dm"], ["Na"])
                    if sub >= 5:
                        bP, kP = mm4(lambda i: M["Na"][:, i, :], None, ["Na"], transpose=True)
                    if sub >= 6:
                        P.act(lambda e, bk=bP: e.copy(out=M["Pa"][:], in_=v4(bk)), [kP], ["Pa"])
                    continue
                bKK, kKK = mm4(lambda i: kT[:, tk(i)], lambda i: kT[:, tk(i)], ["kT"])
                bKQ, kKQ = mm4(lambda i: kT[:, tk(i)], lambda i: qT[:, tk(i)], ["kT", "qT"])
                P.dve(lambda e, h=h, t0=t0: e.tensor_tensor(out=M["rhsG"][:], in0=bc_mid(cm("strictU"), 4),
                                                            in1=bc_last(gg[:, t0:t0 + 4, h], 128), op=ALU.mult), ["cst"] + G, ["rhsG"])
                bDd, kDd = mm4(lambda i: cm("triblk"), lambda i: M["rhsG"][:, i, :], ["cst", "rhsG"])
                P.act(lambda e, bk=bDd: e.activation(out=M["Dm"][:], in_=v4(bk), func=AF.Exp), [kDd], ["Dm"])
                bDT, kDT = mm4(lambda i: M["Dm"][:, i, :], None, ["Dm"], transpose=True)
                P.dve(lambda e, bk=bDT: e.tensor_tensor(out=M["DTm"][:], in0=v4(bk), in1=bc_mid(cm("triblk"), 4), op=ALU.mult), [kDT, "cst"], ["DTm"])
                P.dve(lambda e: e.tensor_tensor(out=M["Dm"][:], in0=M["Dm"][:], in1=bc_mid(cm("strictU"), 4), op=ALU.mult), ["Dm", "cst"], ["Dm"])
                P.dve(lambda e, h=h, t0=t0: e.tensor_tensor(out=M["tmpN"][:], in0=M["Dm"][:], in1=bc_last(nbeta[:, t0:t0 + 4, h], 128),
                                                            op=ALU.mult), ["Dm"] + G, ["tmpN"])
                P.dve(lambda e, bk=bKK: e.tensor_tensor(out=M["Na"][:], in0=v4(bk), in1=M["tmpN"][:], op=ALU.mult), [kKK, "tmpN"], ["Na"])
                P.dve(lambda e, bk=bKQ: e.tensor_tensor(out=M["aT"][:], in0=v4(bk), in1=M["DTm"][:], op=ALU.mult), [kKQ, "DTm"], ["aT"])
                bP, kP = mm4(lambda i: M["Na"][:, i, :], None, ["Na"], transpose=True)
                P.act(lambda e, bk=bP: e.copy(out=M["Pa"][:], in_=v4(bk)), [kP], ["Pa"])
                P.dve(lambda e: e.tensor_tensor(out=M["R"][:], in0=M["Pa"][:], in1=bc_mid(identf, 4), op=ALU.add), ["Pa", "cst"], ["R"])
                N1, P1, N2, P2 = "Na", "Pa", "Nb", "Pb"
                for lev in range(5):
                    bN, kN = mm4(lambda i, P1=P1: M[P1][:, i, :], lambda i, N1=N1: M[N1][:, i, :], [N1, P1])
                    P.act(lambda e, bk=bN, N2=N2: e.copy(out=M[N2][:], in_=v4(bk)), [kN], [N2])
                    if lev < 4:
                        bQ, kQ = mm4(lambda i, N1=N1: M[N1][:, i, :], lambda i, P1=P1: M[P1][:, i, :], [N1, P1])
                        P.act(lambda e, bk=bQ, P2=P2: e.copy(out=M[P2][:], in_=v4(bk)), [kQ], [P2])
                    bR, kR = mm4(lambda i, N2=N2: M[N2][:, i, :], lambda i: M["R"][:, i, :], [N2, "R"])
                    P.dve(lambda e, bk=bR: e.tensor_tensor(out=M["R"][:], in0=v4(bk), in1=M["R"][:], op=ALU.add), [kR, "R"], ["R"])
                    N1, P1, N2, P2 = N2, P2, N1, P1
                bKt, kKt = mm4(lambda i: kT[:, tk(i)], None, ["kT"], transpose=True)
                bVt, kVt = mm4(lambda i: vT[:, tk(i)], None, ["vT"], transpose=True)
                for dstn, scal in (("kbg", sbg), ("kdA", sdA), ("kdB", sdB)):
                    P.dve(lambda e, bk=bKt, dstn=dstn, scal=scal, h=h, t0=t0: e.tensor_tensor(
                        out=M[dstn][:], in0=v4(bk), in1=bc_last(scal[:, t0:t0 + 4, h], 128), op=ALU.mult), [kKt] + G, [dstn])
                P.dve(lambda e, bk=bVt, h=h, t0=t0: e.tensor_tensor(out=M["vb"][:], in0=v4(bk), in1=bc_last(beta[:, t0:t0 + 4, h], 128),
                                                                    op=ALU.mult), [kVt] + G, ["vb"])
                bU, kU = mm4(lambda i: M["R"][:, i, :], lambda i: M["vb"][:, i, :], ["R", "vb"])
                P.act(lambda e, bk=bU: e.copy(out=M["u"][:], in_=v4(bk)), [kU], ["u"])
                bW, kW = mm4(lambda i: M["kbg"][:, i, :], lambda i: M["R"][:, i, :], ["R", "kbg"])
                P.act(lambda e, bk=bW: e.copy(out=M["wkT"][:], in_=v4(bk)), [kW], ["wkT"])
                if cfg.get("dn_dump") and bt == 0 and h == 0:
                    lim = P.limit
                    P.limit = None
                    for nm in ("Na", "Pa", "Nb", "Pb", "R", "Dm", "DTm", "aT", "rhsG", "kbg", "kdA", "kdB", "vb", "u", "wkT"):
                        o = dout("dump_" + nm, [128, 4, 128])
                        P.dma("sp", lambda e, o=o, nm=nm: e.dma_start(out=o[:, :, :], in_=M[nm][:]), [nm], [])
                    for nm, t_ in (("gg", gg), ("beta", beta), ("gc", gc), ("eg", eg), ("sdA", sdA), ("sbg", sbg)):
                        o = dout("dump_" + nm, [128, NT, 8])
                        P.dma("sp", lambda e, o=o, t_=t_: e.dma_start(out=o[:, :, :], in_=t_[:]), G, [])
                    o = dout("dump_glrep", [128, NT, 2, 8])
                    P.dma("sp", lambda e, o=o: e.dma_start(out=o[:, :, :, :], in_=glrep[:]), G, [])
                    P.limit = lim
                for i in range(4 if dn_stage >= 2 else 0):
                    t = t0 + i
                    for half, (lo, hi), kd in ((0, (0, 64), "kdA"), (1, (64, 128), "kdB")):
                        bk, bkk = bank()
                        P.pe(lambda e, bk=bk, i=i: e.matmul(bk[:, 0:128], lhsT=M["wkT"][:, i, :], rhs=Sst[:], start=True, stop=True),
                             ["wkT", "Sst"], [bkk])
                        qsl = qT[:, tk(i)]
                        P.pe(lambda e, bk=bk, qsl=qsl: e.matmul(bk[:, 128:256], lhsT=qsl, rhs=Sst[:], start=True, stop=True),
                             ["qT", "Sst"], [bkk])
                        P.dve(lambda e, bk=bk, i=i, lo=lo, hi=hi: e.tensor_sub(vn[lo:hi, :], M["u"][lo:hi, i, :], bk[lo:hi, 0:128]),
                              ["u", bkk], ["vn"])
                        P.dve(lambda e, bk=bk, lo=lo, hi=hi, t=t, h=h: e.tensor_scalar(o1[lo:hi, :], bk[lo:hi, 128:256], eg[lo:hi, t, h:h + 1],
                                                                                     None, ALU.mult), [bkk] + G, ["o1"])
                        bk2, bkk2 = bank()
                        P.pe(lambda e, bk2=bk2, i=i, kd=kd: e.matmul(bk2[:, 0:128], lhsT=M[kd][:, i, :], rhs=vn[:], start=True, stop=True),
                             [kd, "vn"], [bkk2])
                        P.dve(lambda e, bk2=bk2, t=t, half=half, h=h: e.scalar_tensor_tensor(
                            out=Sst[:], in0=Sst[:], scalar=glrep[:, t, half, h:h + 1], in1=bk2[:, 0:128], op0=ALU.mult, op1=ALU.add),
                            [bkk2, "Sst"] + G, ["Sst"])
                    bk3, bkk3 = bank()
                    P.pe(lambda e, bk3=bk3, i=i: e.matmul(bk3[:, 0:128], lhsT=M["aT"][:, i, :], rhs=vn[:], start=True, stop=True),
                         ["aT", "vn"], [bkk3])
                    P.dve(lambda e, bk3=bk3, i=i: e.tensor_add(M["o4"][:, i, :], o1[:], bk3[:, 0:128]), [bkk3, "o1"], ["o4"])
                if cfg.get("dn_dump") and dn_stage >= 2 and bt == 0 and h == 0:
                    o = dout("dump_o4", [128, 4, 128])
                    P.dma("sp", lambda e, o=o: e.dma_start(out=o[:, :, :], in_=M["o4"][:]), ["o4"], [])
                    o = dout("dump_S", [128, 128])
                    P.dma("sp", lambda e, o=o: e.dma_start(out=o[:, :], in_=Sst[:]), ["Sst"], [])
                if dn_stage < 3:
                    continue
                P.pool(lambda e: e.tensor_tensor(out=M["sq"][:], in0=M["o4"][:], in1=M["o4"][:], op=ALU.mult), ["o4"], ["sq"])
                P.dve(lambda e: e.tensor_reduce(out=ssum[:, 0:4], in_=M["sq"][:], axis=AX.X, op=ALU.add), ["sq"], ["ssum"])
                P.act(lambda e: e.activation(out=ssum[:, 4:8], in_=ssum[:, 0:4], func=AF.Sqrt, scale=1.0 / 128, bias=EPS), ["ssum"], ["ssum"])
                P.dve(lambda e: e.reciprocal(out=ssum[:, 4:8], in_=ssum[:, 4:8]), ["ssum"], ["ssum"])
                P.dve(lambda e: e.tensor_tensor(out=M["o4"][:], in0=M["o4"][:], in1=bc_last(ssum[:, 4:8], 128), op=ALU.mult), ["o4", "ssum"], ["o4"])
                P.dve(lambda e: e.tensor_tensor(out=M["o4"][:], in0=M["o4"][:], in1=bc_mid(onb[:], 4), op=ALU.mult), ["o4", "onb"], ["o4"])
                P.dve(lambda e, t0=t0: e.tensor_tensor(out=yb[:], in0=M["o4"][:], in1=gtm[:, t0:t0 + 4, :], op=ALU.mult), ["o4", "gtm"], ["yb"])
                if cfg.get("dn_dump") and bt == 0 and h == 0:
                    o = dout("dump_yb", [128, 4, 128], BF16)
                    P.dma("sp", lambda e, o=o: e.dma_start(out=o[:, :, :], in_=yb[:]), ["yb"], [])
                    o = dout("dump_ssum", [128, 8])
                    P.dma("sp", lambda e, o=o: e.dma_start(out=o[:, :], in_=ssum[:]), ["ssum"], [])
                bk, bkk = bank()
                bkb = bk[:, :].bitcast(BF16)
                for i in range(4):
                    P.pe(lambda e, bkb=bkb, i=i: e.transpose(out=bkb[:, i * 128:(i + 1) * 128], in_=yb[:, i, :], identity=ident_bf[:]),
                         ["yb", "ident_bf"], [bkk])
                P.act(lambda e, bkb=bkb, bt=bt: e.copy(out=yT[:, bt * 512:(bt + 1) * 512], in_=bkb[:, 0:512]), [bkk], ["yT"])
            if cfg.get("dn_stage", 9) >= 3:
                P.dma("sp", lambda e, h=h: e.dma_start(out=Yd[h], in_=yT[:]), ["yT"], ["Yd"])


    FILL = {}

    def fill_reg(e):
        if "r" not in FILL:
            FILL["r"] = e.to_reg(-30000.0)
        return FILL["r"]

    def phase_fox(l):
        new_phase()
        NT = S // 128
        A = lambda n, sh, dt=F32: AR.alloc(n, sh, dt)
        lf, cf, tot, incl = A("lf", [NT, 8]), A("cf", [NT, 8]), A("tot", [NT, 8]), A("incl", [NT, 8])
        fbb = A("fbb", [8])
        rrep = A("rrep", [8, 8])
        sel8 = A("sel8", [8, 128])
        cfT = A("cfT", [S])
        rowv = A("rowv", [512])
        cfq = A("cfq", [512])
        bkg = A("bkg", [NT])
        qT, kT = A("fqT", [S], BF16), A("fkT", [S], BF16)
        Vh = A("Vh", [NT, 128], BF16)
        yT = A("fyT", [S], BF16)
        tmps = [A("ftmp%d" % i, [512]) for i in range(2)]
        pTs = [A("fpT%d" % i, [512], BF16) for i in range(2)]
        rinv = A("rinv", [512])
        F = ["fox_gates"]
        P.pool(lambda e: e.memset(cfT[:], 0.0), [], ["cfT"])
        P.dma("sp", lambda e: e.dma_start(out=fbb[:], in_=f_bias_bc[l]), [], ["fbb"])
        P.dve(lambda e: e.tensor_tensor(out=lf[:], in0=sg[:, :, 16:24], in1=bc_mid(fbb[:], NT), op=ALU.add), ["sg", "fbb"], F)
        P.act(lambda e: e.activation(out=lf[:], in_=lf[:], func=AF.Exp, scale=-1.0), F, F)
        P.act(lambda e: e.activation(out=lf[:], in_=lf[:], func=AF.Ln, bias=1.0), F, F)
        P.dve(lambda e: e.tensor_scalar(lf[:], lf[:], -1.0, None, ALU.mult), F, F)
        lf2 = lf[:].rearrange("p a b -> p (a b)")
        for dst, cname in ((cf, "tri128"), (tot, "ones")):
            bk, bkk = bank()
            P.pe(lambda e, bk=bk, cname=cname: e.matmul(bk[:, 0:256], lhsT=cm(cname), rhs=lf2, start=True, stop=True), ["cst"] + F, [bkk])
            P.act(lambda e, bk=bk, dst=dst: e.copy(out=dst[:].rearrange("p a b -> p (a b)"), in_=bk[:, 0:256]), [bkk], F)
        for h in range(8):
            P.dve(lambda e, h=h: e.tensor_tensor_scan(out=incl[:, :, h], data0=cm("ones")[:, 0:NT], data1=tot[:, :, h], initial=0.0,
                                                     op0=ALU.mult, op1=ALU.add), ["cst"] + F, F)
        P.dve(lambda e: e.tensor_sub(incl[:], incl[:], tot[:]), F, F)
        P.dve(lambda e: e.tensor_add(cf[:], cf[:], incl[:]), F, F)
        bk, bkk = bank()
        cf4 = cf[:].rearrange("p (g f) h -> p g f h", f=4)[:, :, 0, :]
        P.pe(lambda e, bk=bk: e.matmul(bk[:, 0:64].rearrange("p (g h) -> p g h", g=8), lhsT=cm("selrow0"), rhs=cf4, start=True, stop=True),
             ["cst"] + F, [bkk])
        P.act(lambda e, bk=bk: e.copy(out=rrep[:].rearrange("p g h -> p (g h)"), in_=bk[:, 0:64]), [bkk], F)
        for q4 in range(8):
            bk, bkk = bank()
            for i in range(4):
                t = q4 * 4 + i
                P.pe(lambda e, bk=bk, i=i, t=t: e.matmul(bk[0:8, i * 128:(i + 1) * 128], lhsT=cf[:, t, :], rhs=cm("ident"), start=True, stop=True),
                     ["cst"] + F, [bkk])
            P.act(lambda e, bk=bk, q4=q4: e.copy(out=cfT[0:8, q4 * 512:(q4 + 1) * 512], in_=bk[0:8, :]), [bkk], ["cfT"])
        P.pool(lambda e: e.memset(sel8[:], 0.0), [], ["sel8"])
        P.pool(lambda e: e.memset(rowv[:], 0.0), [], ["rowv"])
        P.dve(lambda e: e.tensor_copy(sel8[0:8, :, :], cm("ident")[0:8, 0:8].unsqueeze(2).to_broadcast([8, 8, 128])), ["cst", "sel8"], ["sel8"])
        scale = 128.0 ** -0.5
        nheads = cfg.get("fox_heads", 8)
        for h in range(nheads):
            P.dma("sp", lambda e, h=h: e.dma_start(out=qT[:], in_=Qc[h]), ["QK0"], ["fqT"])
            P.dma("sp", lambda e, h=h: e.dma_start(out=kT[:], in_=Kc[h]), ["QK1"], ["fkT"])
            P.dma("sp", lambda e, h=h: e.dma_start(out=Vh[:], in_=Vc[:, :, h * 128:(h + 1) * 128]), ["Vc"], ["Vh"])
            for g in range(cfg.get("fox_groups", NG)):
                gs = slice(g * 512, (g + 1) * 512)
                nj = 4 * g + 4
                P.dve(lambda e, g=g, gs=gs: e.tensor_scalar(rowv[0:8, :], cfT[0:8, gs], cfT[0:8, g * 512:g * 512 + 1], None,
                                                            ALU.subtract), ["cfT"], ["rowv"])
                P.bank_lo, P.bank_n = 0, 4
                bk, bkk = bank()
                P.pe(lambda e, bk=bk, h=h: e.matmul(bk[:, :], lhsT=sel8[0:8, h, :], rhs=rowv[0:8, :], start=True, stop=True), ["sel8", "rowv"], [bkk])
                P.act(lambda e, bk=bk: e.copy(out=cfq[:], in_=bk[:, :]), [bkk], ["cfq"])
                P.dve(lambda e, g=g, h=h, nj=nj: e.tensor_scalar(bkg[:, 0:nj], cf[:, 0:nj, h], -1.0, rrep[:, g, h:h + 1], ALU.mult, ALU.add),
                      F, ["bkg"])
                if cfg.get("fox_dump") and g == 0 and h == 0:
                    for nm, t_, shp in (("cfq", cfq, [128, 512]), ("rowv", rowv, [128, 512]), ("cfT", cfT, [128, S]), ("cf", cf, [128, NT, 8]),
                                        ("rrep", rrep, [128, 8, 8]), ("sel8", sel8, [128, 8, 128]), ("bkg", bkg, [128, NT])):
                        o = dout("dump_" + nm, shp)
                        P.dma("sp", lambda e, o=o, t_=t_: e.dma_start(out=o, in_=t_[:]), ["cfq", "rowv", "cfT", "sel8", "bkg"] + F, [])
                    continue
                ai = 4 + 2 * (g % 2)
                accO, kO, accS, kS = banks[ai], "bank%d" % ai, banks[ai + 1], "bank%d" % (ai + 1)

                def qk(j):
                    c0 = 0
                    n = 512
                    bs, bsk = bank()
                    ka = kT[:, j * 128:(j + 1) * 128]
                    qa = qT[:, g * 512 + c0:(g + 1) * 512]
                    P.pe(lambda e: e.matmul(bs[:, 0:n], lhsT=ka, rhs=qa, start=True, stop=True), ["fkT", "fqT"], [bsk])
                    return bs, bsk, c0, n
                nxt = qk(0)
                for j in range(nj):
                    bs, bsk, c0, n = nxt
                    tmp, tmpk = tmps[j % 2], "ftmp%d" % (j % 2)
                    pT, pTk = pTs[j % 2], "fpT%d" % (j % 2)
                    P.dve(lambda e, bs=bs, tmp=tmp, c0=c0, n=n: e.scalar_tensor_tensor(
                        out=tmp[:, 0:n], in0=bs[:, 0:n], scalar=scale, in1=cfq[:, c0:512], op0=ALU.mult, op1=ALU.add), [bsk, "cfq"], [tmpk])
                    if j >= 4 * g:
                        jj = j - 4 * g
                        P.pool(lambda e, tmp=tmp, jj=jj: e.affine_select(out=tmp[:, :], in_=tmp[:, :], pattern=[[1, 512]], compare_op=ALU.is_ge,
                                                                         fill=fill_reg(e), base=-128 * jj, channel_multiplier=-1), [tmpk], [tmpk])
                    P.act(lambda e, tmp=tmp, pT=pT, n=n, j=j: e.activation(out=pT[:, 0:n], in_=tmp[:, 0:n], func=AF.Exp, bias=bkg[:, j:j + 1]),
                          [tmpk, "bkg"], [pTk])
                    if j + 1 < nj:
                        nxt = qk(j + 1)
                    va = Vh[:, j, :]
                    P.pe(lambda e, pT=pT, va=va, c0=c0, n=n, j=j, accO=accO, nj=nj: e.matmul(accO[:, c0:512], lhsT=va, rhs=pT[:, 0:n], start=(j == 0), stop=(j == nj - 1)),
                         ["Vh", pTk], [kO])
                    P.pe(lambda e, pT=pT, c0=c0, n=n, j=j, accS=accS, nj=nj: e.matmul(accS[:, c0:512], lhsT=ones_bf[:], rhs=pT[:, 0:n], start=(j == 0), stop=(j == nj - 1)),
                         ["ones_bf", pTk], [kS])
                P.dve(lambda e, accS=accS: e.reciprocal(out=rinv[:], in_=accS[:, :]), [kS], ["rinv"])
                P.dve(lambda e, gs=gs, accO=accO: e.tensor_tensor(out=yT[:, gs], in0=accO[:, :], in1=rinv[:], op=ALU.mult), [kO, "rinv"], ["fyT"])
            P.bank_lo, P.bank_n = 0, NBANK
            if cfg.get("fox_groups", NG) == NG:
                P.dma("sp", lambda e, h=h: e.dma_start(out=Yf[h], in_=yT[:]), ["fyT"], ["Yf"])


    def phase_merge(l, xsrc, xkey):
        new_phase()
        A = lambda n, sh, dt=F32: AR.alloc(n, sh, dt)
        hT = A("hT", [KC, TG], BF16)
        HT["hT"] = hT
        Ys = [A("Y%d" % b, [8, TG], BF16) for b in range(3)]
        wts = [A("wt%d" % i, [KC, 384], BF16) for i in range(2)]
        wbt = [A("wbt%d" % i, [3, 8, 128], BF16) for i in range(2)]
        gsb = A("gsb", [3, TG])
        macc, mtmp = A("macc", [TG]), A("mtmp", [TG])
        mergedT = A("mergedT", [KC, TG], BF16)
        xts = [A("xt0", [D])] * 2
        wos = [A("wo%d" % i, [KC, 512], BF16) for i in range(2)]
        rtmp = A("rtmp", [512])
        mod_vector(l, 2, modt[2], "mod2")
        Ysrc = (Yp, Yd, Yf)
        Ykey = ("Yp", "Yd", "Yf")
        for g in range(cfg.get("mg_groups", NG)):
            gs = slice(g * TG, (g + 1) * TG)
            P.dma("sp", lambda e, gs=gs: e.dma_start(out=hT[:], in_=Hs[:, :, gs].rearrange("k p t -> p k t")), ["Hs"], ["hT"])
            for b in range(3):
                P.dma("sp", lambda e, gs=gs, b=b: e.dma_start(out=Ys[b][:], in_=Ysrc[b][:, :, gs].rearrange("k p t -> p k t")), [Ykey[b]], ["Y%d" % b])
            for j in range(KC):
                wt, wtk = load_wt(wts, winb, OFF_G + j * 384, 384, "winb")
                wb, wbk = wbt[j % 2], "wbt%d" % (j % 2)
                for b in range(3):
                    P.dma("sp", lambda e, wb=wb, b=b, j=j: e.dma_start(
                        out=wb[:, b, :, :], in_=wbrb[b][:, j * 128:(j + 1) * 128].rearrange("(k p) c -> p k c", p=128)), ["wbrb"], [wbk])
                for b in range(3):
                    bk, bkk = proj_fm(wt, wtk, b * 128)
                    P.act(lambda e, bk=bk, b=b: e.activation(out=gsb[:, b, :], in_=bk[:, :], func=AF.Sigmoid), [bkk], ["gsb"])
                for b in range(3):
                    bz, bzk = bank()
                    for k in range(8):
                        P.pe(lambda e, bz=bz, wb=wb, b=b, k=k: e.matmul(bz[:, :], lhsT=wb[:, b, k, :], rhs=Ys[b][:, k, :],
                                                                        start=(k == 0), stop=(k == 7)), [wbk, "Y%d" % b], [bzk])
                    if b == 0:
                        P.dve(lambda e, bz=bz: e.tensor_tensor(out=macc[:], in0=bz[:, :], in1=gsb[:, 0, :], op=ALU.mult), [bzk, "gsb"], ["macc"])
                    else:
                        P.dve(lambda e, bz=bz, b=b: e.tensor_tensor(out=mtmp[:], in0=bz[:, :], in1=gsb[:, b, :], op=ALU.mult), [bzk, "gsb"], ["mtmp"])
                        if b == 1:
                            P.pool(lambda e: e.tensor_tensor(out=macc[:], in0=macc[:], in1=mtmp[:], op=ALU.add), ["macc", "mtmp"], ["macc"])
                        else:
                            P.pool(lambda e, j=j: e.tensor_tensor(out=mergedT[:, j, :], in0=macc[:], in1=mtmp[:], op=ALU.add),
                                   ["macc", "mtmp"], ["mergedT"])
            for t in range(4):
                ti = g * 4 + t
                xt, xtk = xts[0], "xt0"
                r0 = ti * 128
                P.dma("sp", lambda e, xt=xt, r0=r0: e.dma_start(out=xt[:], in_=xsrc[r0:r0 + 128, :]), [xkey + str(ti)], [xtk])
                for cb in range(4):
                    wo, wok = load_wt(wos, woutb, cb * 512, 512, "woutb", pref="wo")
                    bk, bkk = bank()
                    for k in range(KC):
                        P.pe(lambda e, bk=bk, wo=wo, k=k, t=t: e.matmul(bk[:, :], lhsT=mergedT[:, k, t * 128:(t + 1) * 128], rhs=wo[:, k, :],
                                                                        start=(k == 0), stop=(k == KC - 1)), [wok, "mergedT"], [bkk])
                    P.dve(lambda e, bk=bk, cb=cb: e.tensor_tensor(out=rtmp[:], in0=bk[:, :], in1=modt[2][:, cb * 512:(cb + 1) * 512], op=ALU.mult),
                          [bkk, "mod2"], ["rtmp"])
                    P.dve(lambda e, xt=xt, cb=cb: e.tensor_add(xt[:, cb * 512:(cb + 1) * 512], xt[:, cb * 512:(cb + 1) * 512], rtmp[:]),
                          ["rtmp", xtk], [xtk])
                P.dma("sp", lambda e, xt=xt, r0=r0: e.dma_start(out=xres[r0:r0 + 128, :], in_=xt[:]), [xtk], ["xres" + str(ti)])


    peer_u_flat = peer_u.rearrange("l e d -> (l e) d") if peer_u is not None else None
    peer_v_flat = peer_v.rearrange("l e d -> (l e) d") if peer_v is not None else None

    pubv = dscr("pubv", [NL * NEXP, 2 * D], BF16)

    def convert_tables():
        for src, c0, key in ((peer_u_flat, 0, "pub"), (peer_v_flat, D, "pvb")):
            for r0 in range(0, NL * NEXP, 8192):
                P.dma("pool", lambda e, src=src, c0=c0, r0=r0: e.dma_start(out=pubv[r0:r0 + 8192, c0:c0 + D], in_=src[r0:r0 + 8192, :]), [], [key + str(r0)])
    TABKEYS = [k + str(r0) for k in ("pub", "pvb") for r0 in range(0, NL * NEXP, 8192)]

    def phase_peer(l, last):
        new_phase()
        A = lambda n, sh, dt=F32: AR.alloc(n, sh, dt)
        hf = [A("hf%d" % i, [D]) for i in range(2)]
        hfT = A("hfT", [KC, 256])
        wq = A("wq", [KC, 128])
        qTc = A("qTc", [256])
        skt = A("skt", [16, 128])
        scs = A("scs", [2, 16, 128])
        cw = A("cw", [2048])
        cand = A("cand", [8, 16, 16])
        work = A("work", [2048])
        tv, tif = A("tv", [16, 16]), A("tif", [16, 16])
        ti = A("ti", [16, 16], U32)
        bs, bpf, i1, i2, sel1, sel2 = [A(n, [8, 16]) for n in ("bs", "bpf", "i1", "i2", "sel1", "sel2")]
        bp = A("bp", [8, 16], U32)
        idf = A("idf", [128])
        apre2 = [A("apre%d" % i, [128]) for i in range(4)]
        coef2 = [A("coef%d" % i, [128]) for i in range(4)]
        idi2 = [A("idi%d" % i, [128], I32) for i in range(4)]
        wsm2 = [A("wsm%d" % i, [8, 16]) for i in range(4)]
        wsum = A("wsum", [8])
        thr = A("thr", [16])
        NGB = 4
        Gs = [A("G%d" % i, [2 * D], BF16) for i in range(NGB)]
        gl = A("gl", [128])
        dgs = [A("dg%d" % i, [128], BF16) for i in range(4)]
        fnw = stg[:, 0:D] if last else None
        junk = A("pjunk", [D], BF16)
        cw16 = cw[:, :].rearrange("p (c n) -> p c n", c=16)
        cw8 = cw[:, :].rearrange("p (h n) -> p h n", h=8)
        tv4 = tv[:].rearrange("p (h t) k -> p h t k", t=2)
        tif4 = tif[:].rearrange("p (h t) k -> p h t k", t=2)
        iota16 = cst[:, C_IOTA16:C_IOTA16 + 16]
        mod_vector(l, 4, modt[0], "mod0", plus_one=True, mul_row=norm_ffn_w[l:l + 1, :])
        mod_vector(l, 3, modt[1], "mod1")
        mod_vector(l, 5, modt[2], "mod2")
        P.dma("sp", lambda e: e.dma_start(out=skt[:], in_=skT[l].rearrange("c d n -> d c n")), [], ["skt"])
        if last:
            P.dma("sp", lambda e: e.dma_start(out=fnw, in_=final_norm_w[0:1, :].to_broadcast([128, D])), ["stg"], ["stg"])
        P.dve(lambda e: e.tensor_scalar(thr[:, 0:15], iota16[:, 1:16], 16.0, None, ALU.mult), ["cst"], ["thr"])
        identf = cm("ident")
        print("peer arena words", AR.off, "of", AR.n)
        P.bank_lo, P.bank_n = 0, 4
        ngrp = cfg.get("peer_groups", (S // 512) if (last and SPLIT) else (S // 256))
        XALL = ["xres%d" % i_ for i_ in range(S // 128)]

        def load_x(dst, dstk, ti_, r0):
            if last and SPLIT:
                P.dma("pool", lambda e: e.indirect_dma_start(out=dst[:], out_offset=None, in_=xres,
                                                             in_offset=bass.IndirectOffsetOnAxis(ap=ridx[:, ti_:ti_ + 1], axis=0)),
                      XALL + ["ridx"], [dstk])
            else:
                P.dma("sp", lambda e: e.dma_start(out=dst[:], in_=xres[r0:r0 + 128, :]), ["xres%d" % ti_], [dstk])
        def front(g):
            for t in range(2):
                ti_ = g * 2 + t
                r0 = ti_ * 128
                hk = "hf%d" % t
                load_x(hf[t], hk, ti_, r0)
                rmsnorm_tile(hf[t], hk, modt[0], "mod0", modt[1], "mod1", junk)
                for q4 in range(4):
                    bk, bkk = bank()
                    for i in range(4):
                        k = q4 * 4 + i
                        P.pe(lambda e, bk=bk, i=i, k=k, t=t: e.matmul(bk[:, i * 128:(i + 1) * 128], lhsT=hf[t][:, k * 128:(k + 1) * 128], rhs=identf,
                                                                      start=True, stop=True), [hk, "cst"], [bkk])
                    P.act(lambda e, bk=bk, q4=q4, t=t: e.copy(out=hfT[:, q4 * 4:q4 * 4 + 4, t * 128:(t + 1) * 128], in_=v4(bk)), [bkk], ["hfT"])
            for c in range(16):
                P.dma("sp", lambda e, c=c: e.dma_start(out=wq[:], in_=peer_w_q[l, :, c * 128:(c + 1) * 128].rearrange("(k p) c -> p k c", p=128)),
                      [], ["wq"])
                bk, bkk = bank()
                for k in range(KC):
                    P.pe(lambda e, bk=bk, k=k: e.matmul(bk[:, 0:256], lhsT=wq[:, k, :], rhs=hfT[:, k, :], start=(k == 0), stop=(k == KC - 1)),
                         ["wq", "hfT"], [bkk])
                P.act(lambda e, bk=bk: e.copy(out=qTc[:], in_=bk[:, 0:256]), [bkk], ["qTc"])
                bk2, bkk2 = bank()
                for t in range(2):
                    P.pe(lambda e, bk2=bk2, t=t, c=c: e.matmul(bk2[:, t * 128:(t + 1) * 128], lhsT=qTc[:, t * 128:(t + 1) * 128], rhs=skt[:, c, :],
                                                               start=True, stop=True), ["qTc", "skt"], [bkk2])
                P.act(lambda e, bk2=bk2, c=c: e.copy(out=scs[:, :, c, :], in_=bk2[:, 0:256].rearrange("p (t n) -> p t n", t=2)), [bkk2], ["scs"])
            def stage_c(t):
                ti_ = g * 2 + t
                r0 = ti_ * 128
                hk = "hf%d" % t
                idi, wsm, apre, coef = idi2[(g % 2) * 2 + t], wsm2[(g % 2) * 2 + t], apre2[(g % 2) * 2 + t], coef2[(g % 2) * 2 + t]
                kI, kW, kA, kC = "idi%d" % ((g % 2) * 2 + t), "wsm%d" % ((g % 2) * 2 + t), "apre%d" % ((g % 2) * 2 + t), "coef%d" % ((g % 2) * 2 + t)
                nslot = cfg.get("peer_slots", 128)
                for c in range(16):
                    sv = scs[:, t, c, :]
                    P.dve(lambda e, c=c, sv=sv: e.max(out=tv[:, c, 0:8], in_=sv), ["scs"], ["tv"])
                    P.dve(lambda e, c=c, sv=sv: e.max_index(out=ti[:, c, 0:8], in_max=tv[:, c, 0:8], in_values=sv), ["scs", "tv"], ["ti"])
                    P.dve(lambda e, c=c, sv=sv: e.match_replace(out=cw16[:, c, :], in_to_replace=tv[:, c, 0:8], in_values=sv, imm_value=-1e30),
                          ["scs", "tv"], ["cw"])
                    P.dve(lambda e, c=c: e.max(out=tv[:, c, 8:16], in_=cw16[:, c, :]), ["cw"], ["tv"])
                    P.dve(lambda e, c=c: e.max_index(out=ti[:, c, 8:16], in_max=tv[:, c, 8:16], in_values=cw16[:, c, :]), ["cw", "tv"], ["ti"])
                P.dve(lambda e: e.tensor_copy(tif[:], ti[:]), ["ti"], ["tif"])
                P.dve(lambda e: e.tensor_tensor(out=cand[:], in0=tv4[:, :, 0, :].unsqueeze(3).to_broadcast([128, 8, 16, 16]),
                                                in1=tv4[:, :, 1, :].unsqueeze(2).to_broadcast([128, 8, 16, 16]), op=ALU.add), ["tv"], ["cand"])
                for h in range(8):
                    ch = cand[:, h, :, :].rearrange("p a b -> p (a b)")
                    P.dve(lambda e, h=h, ch=ch: e.max(out=bs[:, h, 0:8], in_=ch), ["cand"], ["bs"])
                    P.dve(lambda e, h=h, ch=ch: e.max_index(out=bp[:, h, 0:8], in_max=bs[:, h, 0:8], in_values=ch), ["cand", "bs"], ["bp"])
                    P.dve(lambda e, h=h, ch=ch: e.match_replace(out=cw8[:, h, :], in_to_replace=bs[:, h, 0:8], in_values=ch, imm_value=-1e30),
                          ["cand", "bs"], ["cw"])
                    P.dve(lambda e, h=h: e.max(out=bs[:, h, 8:16], in_=cw8[:, h, :]), ["cw"], ["bs"])
                    P.dve(lambda e, h=h: e.max_index(out=bp[:, h, 8:16], in_max=bs[:, h, 8:16], in_values=cw8[:, h, :]), ["cw", "bs"], ["bp"])
                P.dve(lambda e: e.tensor_copy(bpf[:], bp[:]), ["bp"], ["bpf"])
                w15 = work[:, 0:1920].rearrange("p (h k m) -> p h k m", h=8, k=16)
                P.dve(lambda e: e.tensor_tensor(out=w15, in0=bpf[:].unsqueeze(3).to_broadcast([128, 8, 16, 15]),
                                                in1=thr[:, 0:15].unsqueeze(1).unsqueeze(1).to_broadcast([128, 8, 16, 15]), op=ALU.is_ge),
                      ["bpf", "thr"], ["work"])
                P.dve(lambda e: e.tensor_reduce(out=i1[:], in_=w15, axis=AX.X, op=ALU.add), ["work"], ["i1"])
                P.dve(lambda e: e.scalar_tensor_tensor(out=i2[:].rearrange("p a b -> p (a b)"), in0=i1[:].rearrange("p a b -> p (a b)"), scalar=-16.0,
                                                       in1=bpf[:].rearrange("p a b -> p (a b)"), op0=ALU.mult, op1=ALU.add), ["i1", "bpf"], ["i2"])
                w16 = work[:, 0:2048].rearrange("p (h k m) -> p h k m", h=8, k=16)
                for ii, src_half, dsel in ((i1, 0, sel1), (i2, 1, sel2)):
                    iik = "i1" if src_half == 0 else "i2"
                    dk_ = "sel1" if src_half == 0 else "sel2"
                    P.dve(lambda e, ii=ii: e.tensor_tensor(out=w16, in0=ii[:].unsqueeze(3).to_broadcast([128, 8, 16, 16]),
                                                           in1=iota16.unsqueeze(1).unsqueeze(1).to_broadcast([128, 8, 16, 16]), op=ALU.is_equal),
                          [iik, "cst"], ["work"])
                    P.dve(lambda e, src_half=src_half: e.tensor_tensor(out=w16, in0=w16, in1=tif4[:, :, src_half, :].unsqueeze(2).to_broadcast([128, 8, 16, 16]),
                                                                       op=ALU.mult), ["work", "tif"], ["work"])
                    P.dve(lambda e, dsel=dsel: e.tensor_reduce(out=dsel[:], in_=w16, axis=AX.X, op=ALU.add), ["work"], [dk_])
                P.dve(lambda e: e.scalar_tensor_tensor(out=idf[:], in0=sel1[:].rearrange("p a b -> p (a b)"), scalar=128.0,
                                                       in1=sel2[:].rearrange("p a b -> p (a b)"), op0=ALU.mult, op1=ALU.add), ["sel1", "sel2"], ["idf"])
                if l > 0:
                    P.dve(lambda e: e.tensor_scalar(idf[:], idf[:], float(l * NEXP), None, ALU.add), ["idf"], ["idf"])
                P.dve(lambda e: e.tensor_copy(idi[:], idf[:]), ["idf"], [kI])
                P.dve(lambda e: e.tensor_tensor(out=wsm[:], in0=bs[:], in1=bs[:, :, 0:1].to_broadcast([128, 8, 16]), op=ALU.subtract), ["bs"], [kW])
                P.act(lambda e: e.activation(out=wsm[:], in_=wsm[:], func=AF.Exp), [kW], [kW])
                P.dve(lambda e: e.tensor_reduce(out=wsum[:], in_=wsm[:], axis=AX.X, op=ALU.add), [kW], ["wsum"])
                P.dve(lambda e: e.reciprocal(out=wsum[:], in_=wsum[:]), ["wsum"], ["wsum"])
                P.dve(lambda e: e.tensor_tensor(out=wsm[:], in0=wsm[:], in1=wsum[:].unsqueeze(2).to_broadcast([128, 8, 16]), op=ALU.mult),
                      [kW, "wsum"], [kW])
                if cfg.get("peer_dump") and g == 0 and t == 0:
                    for nm, t_, shp, dt_ in ((kI, idi, [128, 128], I32), (kW, wsm, [128, 8, 16], F32), ("bs", bs, [128, 8, 16], F32),
                                             ("tv", tv, [128, 16, 16], F32), ("tif", tif, [128, 16, 16], F32)):
                        o = dout("dump_" + nm, shp, dt_)
                        P.dma("sp", lambda e, o=o, t_=t_: e.dma_start(out=o, in_=t_[:]), [kI, kW, "bs", "tv", "tif"], [])
            for t in range(2):
                stage_c(t)
        def back(g):
            def stage_v(t):
                ti_ = g * 2 + t
                r0 = ti_ * 128
                hk = "hf%d" % t
                idi, wsm, apre, coef = idi2[(g % 2) * 2 + t], wsm2[(g % 2) * 2 + t], apre2[(g % 2) * 2 + t], coef2[(g % 2) * 2 + t]
                kI, kW, kA, kC = "idi%d" % ((g % 2) * 2 + t), "wsm%d" % ((g % 2) * 2 + t), "apre%d" % ((g % 2) * 2 + t), "coef%d" % ((g % 2) * 2 + t)
                nslot = cfg.get("peer_slots", 128)
                wflat = wsm[:].rearrange("p a b -> p (a b)")
                hft = hf[t]
                P.bank_lo, P.bank_n = 0, 4
                for s_ in range(nslot):
                    Gb, Gk = Gs[s_ % NGB], "G%d" % (s_ % NGB)
                    dg, dgk = dgs[s_ % 4], "dg%d" % (s_ % 4)
                    P.dma("pool", lambda e, Gb=Gb, s_=s_: e.indirect_dma_start(out=Gb[:], out_offset=None, in_=pubv,
                                                                               in_offset=bass.IndirectOffsetOnAxis(ap=idi[:, s_:s_ + 1], axis=0)),
                          [kI] + TABKEYS, [Gk])
                    P.dve(lambda e, Gb=Gb, s_=s_: e.scalar_tensor_tensor(out=junk[:], in0=Gb[:, 0:D], scalar=1.0, in1=hft[:], op0=ALU.mult, op1=ALU.mult,
                                                                         accum_out=apre[:, s_:s_ + 1]), [Gk, hk], ["pjunk", kA])
                    P.act(lambda e, s_=s_: e.activation(out=gl[:, s_:s_ + 1], in_=apre[:, s_:s_ + 1], func=AF.Gelu), [kA], ["gl"])
                    P.act(lambda e, s_=s_: e.activation(out=coef[:, s_:s_ + 1], in_=wflat[:, s_:s_ + 1], func=AF.Copy, scale=gl[:, s_:s_ + 1]),
                          ["gl", kW], [kC])
                    P.act(lambda e, dg=dg, s_=s_: e.activation(out=dg[:], in_=identf, func=AF.Copy, scale=coef[:, s_:s_ + 1]), [kC, "cst"], [dgk])
                    for cb in range(4):
                        P.pe(lambda e, Gb=Gb, dg=dg, cb=cb, s_=s_: e.matmul(banks[4 + cb][:, :], lhsT=dg[:], rhs=Gb[:, D + cb * 512:D + (cb + 1) * 512],
                                                                             start=(s_ == 0), stop=(s_ == nslot - 1)), [Gk, dgk], ["bank%d" % (4 + cb)])
                xt = hf[t]
                load_x(xt, hk, ti_, r0)
                for cb in range(4):
                    P.dve(lambda e, cb=cb: e.tensor_tensor(out=work[:, 0:512], in0=banks[4 + cb][:, :], in1=modt[2][:, cb * 512:(cb + 1) * 512], op=ALU.mult),
                          ["bank%d" % (4 + cb), "mod2"], ["work"])
                    P.dve(lambda e, xt=xt, cb=cb: e.tensor_add(xt[:, cb * 512:(cb + 1) * 512], xt[:, cb * 512:(cb + 1) * 512], work[:, 0:512]), ["work", hk], [hk])
                if last:
                    rmsnorm_tile(xt, hk, fnw, "stg", None, None, junk)
                    P.dma("sp", lambda e, xt=xt, r0=r0: e.dma_start(out=y_out[r0:r0 + 128, :], in_=xt[:]), [hk], ["y_out"])
                else:
                    P.dma("sp", lambda e, xt=xt, r0=r0: e.dma_start(out=xres[r0:r0 + 128, :], in_=xt[:]), [hk], ["xres%d" % ti_])
            for t in range(2):
                stage_v(t)
        for g in range(ngrp):
            front(g)
            back(g)

    dbg_outs = {}
    if phases in ("p1",):
        l = 0
        prepass(l)
        phase1(l, x_in, "x_in")
        for nm, src, shape, dt in (("d_Hs", Hs, [KC, 128, S], BF16), ("d_Yp", Yp, [8, 128, S], BF16),
                                   ("d_DNq", DNq, [8, 128, S], F32), ("d_DNk", DNk, [8, 128, S], F32), ("d_DNv", DNv, [8, 128, S], F32),
                                   ("d_Qc", Qc, [8, 128, S], BF16), ("d_Kc", Kc, [8, 128, S], BF16)):
            o = dout(nm, shape, dt)
            for a in range(shape[0]):
                P.dma("sp", lambda e, o=o, src=src, a=a: e.dma_start(out=o[a], in_=src[a]),
                      ["Hs", "Yp", "DN0", "DN1", "DN2", "QK0", "QK1"], [])
        o = dout("d_Gdn", [S, 1024], BF16)
        P.dma("sp", lambda e, o=o: e.dma_start(out=o[:, :], in_=Gdn[:, :]), ["Gdn"], [])
        o = dout("d_Vc", [128, 32, 1024], BF16)
        P.dma("sp", lambda e, o=o: e.dma_start(out=o[:, :, :], in_=Vc[:, :, :]), ["Vc"], [])
        o = dout("d_sg", [128, 32, 24], F32)
        P.dma("sp", lambda e, o=o: e.dma_start(out=o[:, :, :], in_=sg[:]), ["sg"], [])

    if phases == "dn":
        phase_dn(0)
        o = dout("d_Yd", [8, 128, S], BF16)
        for a in range(cfg.get("dn_heads", 8) if cfg.get("dn_stage", 9) >= 3 else 0):
            P.dma("sp", lambda e, o=o, a=a: e.dma_start(out=o[a], in_=Yd[a]), ["Yd"], [])

    if phases == "fox":
        phase_fox(0)
        o = dout("d_Yf", [8, 128, S], BF16)
        for a in range(cfg.get("fox_heads", 8)):
            P.dma("sp", lambda e, o=o, a=a: e.dma_start(out=o[a], in_=Yf[a]), ["Yf"], [])

    if phases == "merge":
        if "winb" not in feed:
            prepass(0)
        phase_merge(0, x_in, "x_in")
        o = dout("d_xres", [S, D])
        ng = cfg.get("mg_groups", NG)
        P.dma("sp", lambda e, o=o: e.dma_start(out=o[0:ng * TG, :], in_=xres[0:ng * TG, :]), ["xres%d" % i for i in range(ng * 4)], [])

    if phases == "peer":
        convert_tables()
        phase_peer(0, cfg.get("peer_last", False))
        if not cfg.get("peer_last", False):
            o = dout("d_xres", [S, D])
            ng = cfg.get("peer_groups", S // 256)
            P.dma("sp", lambda e, o=o: e.dma_start(out=o[0:ng * 256, :], in_=xres[0:ng * 256, :]), ["xres%d" % i for i in range(ng * 2)], [])

    if phases == "all":
        convert_tables()
        for l in layers:
            xsrc, xkey = (x_in, "x_in") if l == 0 else (xres, "xres")
            prepass(l)
            phase1(l, xsrc, xkey)
            phase_dn(l)
            phase_fox(l)
            phase_merge(l, xsrc, xkey)
            phase_peer(l, l == NL - 1)

    P.limit = None
    P.finish()
    print("ops", P.n_ops, "sbuf bytes/partition", P.sb_bytes)
    P.emit()
    return nc


def prep_shared(inputs):
    f = lambda a: np.ascontiguousarray(np.asarray(a, dtype=np.float32))
    m = {}
    for k in ("ada_w", "ada_b", "norm_mix_w", "norm_ffn_w", "w_in", "pool_w", "w_branch_pool", "w_branch_dn",
              "w_branch_fox", "w_out", "peer_w_q", "peer_u", "peer_v"):
        m[k] = f(inputs[k])
    m["final_norm_w"] = f(inputs["final_norm_w"]).reshape(1, D)
    m["pool_scale_t"] = f(np.asarray(inputs["pool_scale"]).reshape(NL, 8, 128).transpose(0, 2, 1))
    cw = np.asarray(inputs["dn_conv_w"]).reshape(NL, 4, 24, 128)
    m["conv_t"] = f(cw.transpose(0, 3, 2, 1).reshape(NL, 128, 96))
    bc = lambda a: f(np.broadcast_to(np.asarray(a)[:, None, :], (NL, 128, np.asarray(a).shape[-1])))
    m["a_log_bc"] = bc(inputs["dn_a_log"])
    m["dt_bias_bc"] = bc(inputs["dn_dt_bias"])
    m["f_bias_bc"] = bc(inputs["fox_f_bias"])
    m["onorm_bc"] = bc(inputs["dn_onorm_w"])
    sk = np.asarray(inputs["peer_sub_keys"])
    m["skT"] = f(sk.reshape(NL, 16, 128, 128).transpose(0, 1, 3, 2))
    m["consts"] = make_consts()
    return m


def prep_core(inputs, shared, b, core=0):
    m = dict(shared)
    half = 0 if core < 4 else 1
    tiles = (np.arange(S // 128) + half * (S // 256)) % (S // 128)
    m["rowidx"] = np.ascontiguousarray((tiles[None, :] * 128 + np.arange(128)[:, None]).astype(np.int32))
    m["x"] = np.ascontiguousarray(np.asarray(inputs["x"][b], dtype=np.float32))
    m["cT"] = np.ascontiguousarray(np.asarray(inputs["c"][b], dtype=np.float32).reshape(KC, 128).T)
    return m


_NC_CACHE = {}


def kernel(**inputs):
    if "nc" not in _NC_CACHE:
        _NC_CACHE["nc"] = build({"phases": "all"})
    nc = _NC_CACHE["nc"]
    shared = prep_shared(inputs)
    in_maps = [prep_core(inputs, shared, i % 4, core=i) for i in range(8)]
    res = run_bass_kernel_spmd(nc, in_maps, core_ids=list(range(8)))
    out = np.empty((4, S, D), np.float32)
    for b in range(4):
        out[b, :S // 2] = np.asarray(res.results[b]["y"], dtype=np.float32)
        out[b, S // 2:] = np.asarray(res.results[b + 4]["y"], dtype=np.float32)
    return out
```
